# Optimizing a Trainium2 kernel written in Bass

```python
import math, functools
import jax, jax.numpy as jnp
from jax import lax
import numpy as np

D_MODEL = 2048
BATCH = 4
SEQ = 2048
DEPTH = 1
DEC_BATCH = 128
DEC_SEQ = 4
PAST_LEN = 16384
PAGE_SIZE = 128

MLA_HEADS = D_MODEL // 256
V_HEAD = 128
MLA_WIDTH = MLA_HEADS * V_HEAD
LRU_WIDTH = D_MODEL - MLA_WIDTH
QK_NOPE = 128
QK_ROPE = 64
Q_LORA = 512
KV_LORA = 256
ROPE_THETA = 10000.0
SM_SCALE = (QK_NOPE + QK_ROPE) ** -0.5
Q_BLOCK = 128
NEG_INF = -1e30
LRU_BLOCKS = 8
LRU_BLOCK = LRU_WIDTH // LRU_BLOCKS
CONV_W = 4
LRU_C = 8.0
N_GROUPS = 4
EXPERTS_PER_GROUP = 8
N_EXPERTS = N_GROUPS * EXPERTS_PER_GROUP
TOP_K = 2
D_EXPERT = 512
PLE_DIM = 256
EPS = 1e-6
IN_COLS = Q_LORA + KV_LORA + QK_ROPE + 2 * LRU_WIDTH
SPLITS = (Q_LORA, Q_LORA + KV_LORA, Q_LORA + KV_LORA + QK_ROPE, Q_LORA + KV_LORA + QK_ROPE + LRU_WIDTH)

kernel_name = 'hymba_mla_rglru_hmoe_step'


def rmsnorm(x, g):
    xf = x.astype(jnp.float32)
    y = xf * lax.rsqrt(jnp.mean(jnp.square(xf), axis=-1, keepdims=True) + EPS)
    return (y * g.astype(jnp.float32)).astype(x.dtype)


def apply_rope(x, pos):
    half = x.shape[-1] // 2
    inv = ROPE_THETA ** (-jnp.arange(half, dtype=jnp.float32) / half)
    ang = pos.astype(jnp.float32)[:, None] * inv[None, :]
    shape = (1, pos.shape[0]) + (1,) * (x.ndim - 3) + (half,)
    cos = jnp.cos(ang).reshape(shape).astype(x.dtype)
    sin = jnp.sin(ang).reshape(shape).astype(x.dtype)
    x1, x2 = x[..., :half], x[..., half:]
    return jnp.concatenate([x1 * cos - x2 * sin, x1 * sin + x2 * cos], axis=-1)


def mixer_projections(u, pos, w_in, g_q, w_uq, g_kv, w_ukv):
    B, T, _ = u.shape
    z = u @ w_in
    c_q, c_kv, k_r, x_br, y_br = jnp.split(z, SPLITS, axis=-1)
    q = (rmsnorm(c_q, g_q) @ w_uq).reshape(B, T, MLA_HEADS, QK_NOPE + QK_ROPE)
    q_rope = apply_rope(q[..., QK_NOPE:], pos)
    q_lat = jnp.einsum('bthn,chn->bthc', q[..., :QK_NOPE], w_ukv[..., :QK_NOPE])
    lat = rmsnorm(c_kv, g_kv)
    k_rope = apply_rope(k_r, pos)
    return q_lat, q_rope, lat, k_rope, x_br, y_br


def _scores(q_lat, q_rope, lat, k_rope):
    s = jnp.einsum('bthc,bsc->bhts', q_lat, lat) + jnp.einsum('bthr,bsr->bhts', q_rope, k_rope)
    return s.astype(jnp.float32) * SM_SCALE


def mla_prompt_attention(q_lat, q_rope, lat, k_rope):
    B, S = q_lat.shape[:2]
    nb = S // Q_BLOCK
    key_pos = jnp.arange(S)

    def block(args):
        ql, qr, qpos = args
        s = _scores(ql, qr, lat, k_rope)
        s = jnp.where(key_pos[None, :] <= qpos[:, None], s, NEG_INF)
        p = jax.nn.softmax(s, axis=-1).astype(lat.dtype)
        return jnp.einsum('bhts,bsc->bthc', p, lat)

    qlb = q_lat.reshape(B, nb, Q_BLOCK, MLA_HEADS, KV_LORA).swapaxes(0, 1)
    qrb = q_rope.reshape(B, nb, Q_BLOCK, MLA_HEADS, QK_ROPE).swapaxes(0, 1)
    qpos = jnp.arange(S).reshape(nb, Q_BLOCK)
    o = lax.map(block, (qlb, qrb, qpos))
    return o.swapaxes(0, 1).reshape(B, S, MLA_HEADS, KV_LORA)


def mla_sample_attention(q_lat, q_rope, lat, k_rope, lat_past, rope_past):
    T = q_lat.shape[1]
    P = lat_past.shape[1]
    s_past = _scores(q_lat, q_rope, lat_past, rope_past)
    s_new = _scores(q_lat, q_rope, lat, k_rope)
    s_new = jnp.where(jnp.tril(jnp.ones((T, T), dtype=bool)), s_new, NEG_INF)
    p = jax.nn.softmax(jnp.concatenate([s_past, s_new], axis=-1), axis=-1).astype(lat.dtype)
    return (jnp.einsum('bhts,bsc->bthc', p[..., :P], lat_past)
            + jnp.einsum('bhts,bsc->bthc', p[..., P:], lat))


def causal_conv(x_br, conv_state, w_conv, b_conv):
    T = x_br.shape[1]
    xx = jnp.concatenate([conv_state.astype(x_br.dtype), x_br], axis=1)
    out = b_conv
    for k in range(CONV_W):
        out = out + xx[:, k:k + T] * w_conv[k]
    return out, xx[:, -(CONV_W - 1):]


def rglru(xc, h0, w_rg, b_rg, w_ig, b_ig, lru_lambda):
    B, T, W = xc.shape
    xb = xc.reshape(B, T, LRU_BLOCKS, LRU_BLOCK)
    r = jax.nn.sigmoid((jnp.einsum('btnk,nkj->btnj', xb, w_rg).reshape(B, T, W) + b_rg).astype(jnp.float32))
    i = jax.nn.sigmoid((jnp.einsum('btnk,nkj->btnj', xb, w_ig).reshape(B, T, W) + b_ig).astype(jnp.float32))
    log_a = -LRU_C * r * jax.nn.softplus(-lru_lambda.astype(jnp.float32))
    a = jnp.exp(log_a)
    u = jnp.sqrt(-jnp.expm1(2.0 * log_a)) * i * xc.astype(jnp.float32)

    def step(h, au):
        a_t, u_t = au
        h = a_t * h + u_t
        return h, h

    h_last, hs = lax.scan(step, h0.astype(jnp.float32), (a.swapaxes(0, 1), u.swapaxes(0, 1)))
    return hs.swapaxes(0, 1).astype(xc.dtype), h_last


def hier_moe(u, w_group, b_group, w_router, b_router, w_gate, w_up, w_down):
    shp = u.shape
    t = u.reshape(-1, D_MODEL)
    g_prob = jax.nn.softmax((t @ w_group).astype(jnp.float32) + b_group.astype(jnp.float32), axis=-1)
    g_idx = jnp.argmax(g_prob, axis=-1)
    g_w = jnp.take_along_axis(g_prob, g_idx[:, None], axis=-1)
    e_logits = ((t @ w_router).astype(jnp.float32) + b_router.astype(jnp.float32)).reshape(-1, N_GROUPS, EXPERTS_PER_GROUP)
    e_logits = jnp.take_along_axis(e_logits, g_idx[:, None, None], axis=1)[:, 0]
    top_v, top_i = lax.top_k(jax.nn.softmax(e_logits, axis=-1), TOP_K)
    w_k = g_w * top_v / jnp.sum(top_v, axis=-1, keepdims=True)
    eid = g_idx[:, None] * EXPERTS_PER_GROUP + top_i
    combine = jnp.sum(jax.nn.one_hot(eid, N_EXPERTS, dtype=jnp.float32) * w_k[..., None], axis=1)
    hg = jnp.einsum('nd,edf->nef', t, w_gate)
    hu = jnp.einsum('nd,edf->nef', t, w_up)
    act = jax.nn.silu(hg) * hu * combine[..., None].astype(t.dtype)
    return jnp.einsum('nef,efd->nd', act, w_down).reshape(shp)


def trunk_layer(h, p_l, pos, conv_state, lru_state, attend, w):
    B, T, _ = h.shape
    u = rmsnorm(h, w['g_mix'])
    q_lat, q_rope, lat, k_rope, x_br, y_br = mixer_projections(
        u, pos, w['w_in'], w['g_q'], w['w_uq'], w['g_kv'], w['w_ukv'])
    o_lat = attend(q_lat, q_rope, lat, k_rope)
    o_mla = jnp.einsum('bthc,chv->bthv', o_lat, w['w_ukv'][..., QK_NOPE:]).reshape(B, T, MLA_WIDTH)
    xc, new_conv = causal_conv(x_br, conv_state, w['w_conv'], w['b_conv'])
    o_rec, new_lru = rglru(xc, lru_state, w['w_rg'], w['b_rg'], w['w_ig'], w['b_ig'], w['lru_lambda'])
    o_lru = jax.nn.gelu(y_br) * o_rec
    mixed = jnp.concatenate([rmsnorm(o_mla, w['g_out_mla']), rmsnorm(o_lru, w['g_out_lru'])], axis=-1)
    h = h + mixed @ w['w_o']
    h = h + hier_moe(rmsnorm(h, w['g_ffn']), w['w_group'], w['b_group'], w['w_router'], w['b_router'],
                     w['w_gate'], w['w_up'], w['w_down'])
    gate = jax.nn.sigmoid(rmsnorm(h, w['g_ple']) @ w['w_ple_gate'] + w['b_ple_gate'])
    h = h + gate * (p_l @ w['w_ple_proj'])
    return h, (lat, k_rope, new_lru, new_conv)


def setup_inputs(seed: int = 0) -> dict:
    key = jax.random.key(seed)
    ks = jax.random.split(key, 64)
    cnt = [0]

    def nk():
        cnt[0] += 1
        return ks[cnt[0] - 1]

    def nrm(shape, scale=1.0):
        return jax.random.normal(nk(), shape, jnp.float32) * scale

    def gain(shape):
        return 1.0 + 0.05 * jax.random.normal(nk(), shape, jnp.float32)

    n_pages = PAST_LEN // PAGE_SIZE
    n_pool = (5 * DEC_BATCH * n_pages) // 4
    perm = jax.random.permutation(nk(), n_pool)[: DEC_BATCH * n_pages]
    page_table = perm.reshape(DEC_BATCH, n_pages).astype(jnp.int32)
    a_c = jax.random.uniform(nk(), (DEPTH, LRU_WIDTH), jnp.float32, 0.9, 0.999)
    a_base = a_c ** (1.0 / LRU_C)
    lru_lambda = jnp.log(a_base) - jnp.log1p(-a_base)
    return {
        'x_prompt': nrm((BATCH, SEQ, D_MODEL)),
        'x_sample': nrm((DEC_BATCH, DEC_SEQ, D_MODEL)),
        'p_prompt': nrm((DEPTH, BATCH, SEQ, PLE_DIM)),
        'p_sample': nrm((DEPTH, DEC_BATCH, DEC_SEQ, PLE_DIM)),
        'cache_latent': nrm((DEPTH, n_pool, PAGE_SIZE, KV_LORA)),
        'cache_krope': nrm((DEPTH, n_pool, PAGE_SIZE, QK_ROPE)),
        'state_lru': nrm((DEPTH, DEC_BATCH, LRU_WIDTH), 0.5),
        'state_conv': nrm((DEPTH, DEC_BATCH, CONV_W - 1, LRU_WIDTH)),
        'page_table': page_table,
        'g_mix': gain((DEPTH, D_MODEL)),
        'w_in': nrm((DEPTH, D_MODEL, IN_COLS), D_MODEL ** -0.5),
        'g_q': gain((DEPTH, Q_LORA)),
        'w_uq': nrm((DEPTH, Q_LORA, MLA_HEADS * (QK_NOPE + QK_ROPE)), Q_LORA ** -0.5),
        'g_kv': gain((DEPTH, KV_LORA)),
        'w_ukv': nrm((DEPTH, KV_LORA, MLA_HEADS, QK_NOPE + V_HEAD), KV_LORA ** -0.5),
        'w_conv': nrm((DEPTH, CONV_W, LRU_WIDTH), CONV_W ** -0.5),
        'b_conv': nrm((DEPTH, LRU_WIDTH), 0.02),
        'w_rg': nrm((DEPTH, LRU_BLOCKS, LRU_BLOCK, LRU_BLOCK), LRU_BLOCK ** -0.5),
        'b_rg': nrm((DEPTH, LRU_WIDTH), 0.1),
        'w_ig': nrm((DEPTH, LRU_BLOCKS, LRU_BLOCK, LRU_BLOCK), LRU_BLOCK ** -0.5),
        'b_ig': nrm((DEPTH, LRU_WIDTH), 0.1),
        'lru_lambda': lru_lambda,
        'g_out_mla': gain((DEPTH, MLA_WIDTH)),
        'g_out_lru': gain((DEPTH, LRU_WIDTH)),
        'w_o': nrm((DEPTH, D_MODEL, D_MODEL), D_MODEL ** -0.5),
        'g_ffn': gain((DEPTH, D_MODEL)),
        'w_group': nrm((DEPTH, D_MODEL, N_GROUPS), D_MODEL ** -0.5),
        'b_group': nrm((DEPTH, N_GROUPS), 0.01),
        'w_router': nrm((DEPTH, D_MODEL, N_EXPERTS), D_MODEL ** -0.5),
        'b_router': nrm((DEPTH, N_EXPERTS), 0.01),
        'w_gate': nrm((DEPTH, N_EXPERTS, D_MODEL, D_EXPERT), D_MODEL ** -0.5),
        'w_up': nrm((DEPTH, N_EXPERTS, D_MODEL, D_EXPERT), D_MODEL ** -0.5),
        'w_down': nrm((DEPTH, N_EXPERTS, D_EXPERT, D_MODEL), D_EXPERT ** -0.5),
        'g_ple': gain((DEPTH, D_MODEL)),
        'w_ple_gate': nrm((DEPTH, D_MODEL, D_MODEL), D_MODEL ** -0.5),
        'b_ple_gate': nrm((DEPTH, D_MODEL), 0.01),
        'w_ple_proj': nrm((DEPTH, PLE_DIM, D_MODEL), PLE_DIM ** -0.5),
        'g_final': gain((D_MODEL,)),
    }


def reference(x_prompt, x_sample, p_prompt, p_sample, cache_latent, cache_krope, state_lru, state_conv,
              page_table, g_mix, w_in, g_q, w_uq, g_kv, w_ukv, w_conv, b_conv, w_rg, b_rg, w_ig, b_ig,
              lru_lambda, g_out_mla, g_out_lru, w_o, g_ffn, w_group, b_group, w_router, b_router,
              w_gate, w_up, w_down, g_ple, w_ple_gate, b_ple_gate, w_ple_proj, g_final):
    B, S, _ = x_prompt.shape
    DB, T, _ = x_sample.shape
    past_len = page_table.shape[1] * cache_latent.shape[2]
    pos_p = jnp.arange(S)
    pos_s = past_len + jnp.arange(T)
    h_p, h_s = x_prompt, x_sample
    st_p, st_s = [], []
    for l in range(DEPTH):
        lw = dict(g_mix=g_mix[l], w_in=w_in[l], g_q=g_q[l], w_uq=w_uq[l], g_kv=g_kv[l], w_ukv=w_ukv[l],
                  w_conv=w_conv[l], b_conv=b_conv[l], w_rg=w_rg[l], b_rg=b_rg[l], w_ig=w_ig[l], b_ig=b_ig[l],
                  lru_lambda=lru_lambda[l], g_out_mla=g_out_mla[l], g_out_lru=g_out_lru[l], w_o=w_o[l],
                  g_ffn=g_ffn[l], w_group=w_group[l], b_group=b_group[l], w_router=w_router[l],
                  b_router=b_router[l], w_gate=w_gate[l], w_up=w_up[l], w_down=w_down[l], g_ple=g_ple[l],
                  w_ple_gate=w_ple_gate[l], b_ple_gate=b_ple_gate[l], w_ple_proj=w_ple_proj[l])
        zero_conv = jnp.zeros((B, CONV_W - 1, LRU_WIDTH), x_prompt.dtype)
        zero_h = jnp.zeros((B, LRU_WIDTH), jnp.float32)
        h_p, sp = trunk_layer(h_p, p_prompt[l], pos_p, zero_conv, zero_h, mla_prompt_attention, lw)
        lat_past = cache_latent[l][page_table].reshape(DB, past_len, KV_LORA)
        rope_past = cache_krope[l][page_table].reshape(DB, past_len, QK_ROPE)
        attend_s = functools.partial(mla_sample_attention, lat_past=lat_past, rope_past=rope_past)
        h_s, ss = trunk_layer(h_s, p_sample[l], pos_s, state_conv[l], state_lru[l], attend_s, lw)
        st_p.append(sp)
        st_s.append(ss)
    y_prompt = rmsnorm(h_p, g_final)
    y_sample = rmsnorm(h_s, g_final)
    new_latent_prompt = jnp.stack([s[0] for s in st_p])
    new_krope_prompt = jnp.stack([s[1] for s in st_p])
    new_lru_prompt = jnp.stack([s[2] for s in st_p])
    new_conv_prompt = jnp.stack([s[3] for s in st_p])
    new_latent_sample = jnp.stack([s[0] for s in st_s])
    new_krope_sample = jnp.stack([s[1] for s in st_s])
    new_lru_sample = jnp.stack([s[2] for s in st_s])
    new_conv_sample = jnp.stack([s[3] for s in st_s])
    return (y_prompt, y_sample, new_latent_prompt, new_krope_prompt, new_lru_prompt, new_conv_prompt,
            new_latent_sample, new_krope_sample, new_lru_sample, new_conv_sample)
```

```python
import numpy as np
from contextlib import ExitStack
import concourse.bass as bass
import concourse.mybir as mybir
from concourse.bass_utils import run_bass_kernel_spmd

F32 = mybir.dt.float32
BF16 = mybir.dt.bfloat16
I32 = mybir.dt.int32
AF = mybir.ActivationFunctionType
ALU = mybir.AluOpType
AX = mybir.AxisListType

import os
DBG = int(os.environ.get('KDBG', '0'))
STAGE = int(os.environ.get('KSTAGE', '3'))

D = 2048; NO = 1024; NPF = 1024; NS = 64; NT = NO + NS
NKEY = 2048 + NS
EPS = 1e-6
NEG = -1e30
SM_SCALE = 192.0 ** -0.5
ENG = ['pe', 'act', 'dve', 'pool', 'sp']
DECLARED = []


class Prog:
    def __init__(self, nc, es):
        self.nc = nc; self.es = es
        self.sem = {e: es.enter_context(nc.semaphore('sem_' + e)) for e in ENG}
        self.lanes = {}
        self.seq = {e: 0 for e in ENG}
        self.lcnt = {}
        self.waited = {e: {} for e in ENG}
        self.reset()
        self.barrier_vals = None

    def reset(self):
        self.ops = []; self.last_w = {}; self.readers = {}

    def lane(self, name):
        if name not in self.lanes:
            self.lanes[name] = self.es.enter_context(self.nc.semaphore('ln_' + name))
            self.lcnt[name] = 0
        return self.lanes[name]

    def op(self, eng, fn, r=(), w=(), lane=None):
        idx = len(self.ops)
        w = list(w) + [x for x in r if x.startswith('ps') and x[2:].isdigit() and x not in w]
        deps = set()
        for x in r:
            if x in self.last_w: deps.add(self.last_w[x])
        for x in w:
            if x in self.last_w: deps.add(self.last_w[x])
            deps.update(self.readers.get(x, ()))
        for x in w:
            self.last_w[x] = idx; self.readers[x] = []
        for x in r:
            self.readers.setdefault(x, []).append(idx)
        dv = {}
        for d in deps:
            p = self.ops[d]
            dv[d] = self.lcnt[p['lane']] if p['lane'] is not None else p['val']
        o = dict(eng=eng, fn=fn, deps=dv, lane=lane)
        if lane is None:
            self.seq[eng] += 1; o['sem'] = self.sem[eng]; o['val'] = self.seq[eng]; o['inc'] = 1
        else:
            s = self.lane(lane); self.lcnt[lane] += 16
            o['sem'] = s; o['val'] = self.lcnt[lane]; o['inc'] = 16
        self.ops.append(o)

    def emit(self, final=False):
        nc = self.nc
        bar = self.barrier_vals
        ops = self.ops
        with nc.Block() as block:
            decos = dict(pe=block.tensor, act=block.scalar, dve=block.vector, pool=block.gpsimd, sp=block.sync)
            for e in ENG:
                def body(eh, e=e):
                    wd = self.waited[e]
                    def wait(sem, val):
                        if wd.get(id(sem), 0) >= val: return
                        eh.wait_ge(sem, val); wd[id(sem)] = val
                    if bar is not None:
                        for sem, val in bar:
                            if val > 0: wait(sem, val)
                    for o in ops:
                        if o['eng'] != e: continue
                        for d in sorted(o['deps']):
                            p = ops[d]
                            if p['eng'] == 'pe' and e == 'pe' and p['lane'] is None: continue
                            wait(p['sem'], o['deps'][d])
                        ins = o['fn'](eh)
                        ins.then_inc(o['sem'], o['inc'])
                    if final and e == 'sp':
                        for en in ENG: wait(self.sem[en], self.seq[en])
                        for ln, s in self.lanes.items(): wait(s, self.lcnt[ln])
                decos[e](body)
        self.barrier_vals = [(self.sem[en], self.seq[en]) for en in ENG] + \
                            [(s, self.lcnt[ln]) for ln, s in self.lanes.items()]
        self.reset()


def build_nc(npool=20480):
    nc = bass.Bass("TRN2", target_bir_lowering=False)
    es = ExitStack()
    DECLARED.clear()
    def din(name, shape, dt=F32, stage=1):
        if STAGE < stage: return None
        DECLARED.append(name)
        return nc.dram_tensor(name, list(shape), dt, kind="ExternalInput").ap()
    def dout(name, shape, dt=F32): return nc.dram_tensor(name, list(shape), dt, kind="ExternalOutput").ap()
    xo = din('xo', [NO, D]); xp = din('xp', [NPF, D]); xs = din('xs', [NS, D])
    po = din('po', [NO, 256], stage=3); psm = din('psm', [NS, 256], stage=3)
    cl = din('cl', [npool, 128, 256], stage=2); ck = din('ck', [npool, 128, 64], stage=2); ptT = din('ptT', [128, 16], I32, stage=2)
    slT = din('slT', [1024, 16]); scT = din('scT', [1024, 48])
    flags = din('flags', [128, 2])
    gvh = din('gvh', [128, 104]); lruch = din('lruch', [128, 8, 8])
    ropeC = din('ropeC', [64, NKEY]); ropeS = din('ropeS', [64, NKEY])
    tri = din('tri', [128, 128], stage=2); smask = din('smask', [32, 4], stage=2)
    g_mix = din('g_mix', [D], stage=99); w_in = din('w_in', [D, 2880]); wkrp = din('wkrp', [D, 64])
    g_q = din('g_q', [512], stage=99); w_uq = din('w_uq', [512, 1536], stage=2); wqrp = din('wqrp', [512, 8, 64], stage=2)
    g_kv = din('g_kv', [256], stage=99); wukT = din('wukT', [8, 128, 256], stage=2); wuv = din('wuv', [8, 256, 128], stage=2)
    w_conv = din('w_conv', [4, 1024], stage=99); b_conv = din('b_conv', [1024], stage=99)
    w_rg = din('w_rg', [8, 128, 128]); b_rg = din('b_rg', [1024], stage=99); w_ig = din('w_ig', [8, 128, 128]); b_ig = din('b_ig', [1024], stage=99)
    lam = din('lam', [1024], stage=99); g_om = din('g_om', [1024], stage=99); g_ol = din('g_ol', [1024], stage=99)
    w_o = din('w_o', [D, D], stage=3); g_ffn = din('g_ffn', [D], stage=99)
    w_group = din('w_group', [D, 4], stage=3); b_group = din('b_group', [4], stage=3); w_router = din('w_router', [D, 32], stage=3); b_router = din('b_router', [32], stage=3)
    w_gate = din('w_gate', [32, D, 512], stage=3); w_up = din('w_up', [32, D, 512], stage=3); w_down = din('w_down', [32, 512, D], stage=3)
    g_ple = din('g_ple', [D], stage=99); w_pg = din('w_pg', [D, D], stage=3); b_pg = din('b_pg', [D], stage=3); w_pp = din('w_pp', [256, D], stage=3)
    g_fin = din('g_fin', [D], stage=99)
    y_o = dout('y_o', [NO, D]); y_s = dout('y_s', [NS, D])
    lat_o = dout('lat_o', [NO, 256]); kr_o = dout('kr_o', [NO, 64])
    lru_o = dout('lru_o', [128, 8]); conv_o = dout('conv_o', [128, 24])
    lat_s = dout('lat_s', [NS, 256]); kr_s = dout('kr_s', [NS, 64])
    lru_s = dout('lru_s', [1024, 16]); conv_s = dout('conv_s', [1024, 48])

    P = Prog(nc, es)
    def sb(name, shape, dt=F32, st=None): return (st or es).enter_context(nc.sbuf_tensor(name, list(shape), dt))
    hT = sb('hT', [128, 16, NT])
    mixT = sb('mixT', [128, 16, NT], BF16)
    identf = sb('identf', [128, 128]); identb = sb('identb', [128, 128], BF16)
    onesb = sb('onesb', [128, 128], BF16)
    epsT = sb('epsT', [128, 1]); oneT = sb('oneT', [128, 1])
    gv = sb('gv', [128, 88])
    GV_MIX, GV_Q, GV_KV, GV_OM, GV_OL, GV_FFN, GV_PLE, GV_BPG = 0, 16, 20, 22, 30, 38, 54, 70
    gv2 = sb('gv2', [128, 16])
    lruc = sb('lruc', [128, 8, 12])
    flg = sb('flg', [128, 2])
    hist = sb('hist', [128, 8, 3]); state = sb('state', [128, 8, 1])
    es_mid = ExitStack()
    cqT = sb('cqT', [128, 4, NT], BF16, es_mid)
    latT = sb('latT', [128, 2, NKEY], BF16, es_mid)
    krT = sb('krT', [64, NKEY], BF16, es_mid)
    lat_tok = sb('lat_tok', [128, 17, 260], BF16, es_mid)
    psall = es.enter_context(nc.psum_tensor('psall', [128, 8, 512], F32))
    ps = [psall[:, i, :] for i in range(8)]

    def dma(eng, lane, out, in_, r=(), w=()):
        P.op(eng, lambda e: e.dma_start(out=out, in_=in_), r=r, w=w, lane=lane)

    P.op('pool', lambda e: e.memset(identf[:], 0.0), w=['identf'])
    def c_ident(e):
        return e.affine_select(out=identf[:], in_=identf[:], pattern=[[-1, 128]], compare_op=ALU.not_equal,
                               fill=1.0, base=0, channel_multiplier=1)
    P.op('pool', c_ident, r=['identf'], w=['identf'])
    P.op('pool', lambda e: e.tensor_copy(out=identb[:], in_=identf[:]), r=['identf'], w=['identb'])
    P.op('pool', lambda e: e.memset(onesb[:], 1.0), w=['onesb'])
    P.op('pool', lambda e: e.memset(epsT[:], EPS), w=['epsT'])
    P.op('pool', lambda e: e.memset(oneT[:], 1.0), w=['oneT'])
    P.op('pool', lambda e: e.memset(lat_tok[:, :, 256:257], 1.0), w=['lat_tok_ones'])
    P.op('pool', lambda e: e.memset(hist[:], 0.0), w=['hist'])
    P.op('pool', lambda e: e.memset(state[:], 0.0), w=['state'])
    small = nc.allow_non_contiguous_dma(reason="small strided constant loads")
    es.enter_context(small)
    dma('sp', 'c0', gv[:, 0:70], gvh[:, 0:70], w=['gv'])
    dma('sp', 'c0', gv2[:, 0:16], gvh[:, 70:86], w=['gv'])
    dma('sp', 'c0', gv[:, 70:86], gvh[:, 86:102], w=['gv'])
    dma('sp', 'c0b', lruc[:, :, 0:8], lruch, w=['lruc'])
    dma('sp', 'c0', flg[:], flags, w=['flg'])
    P.op('act', lambda e: e.activation(out=lruc[:, :, 7:8], in_=lruc[:, :, 7:8], func=AF.Exp, scale=-1.0), r=['lruc'], w=['lruc'])
    P.op('act', lambda e: e.activation(out=lruc[:, :, 7:8], in_=lruc[:, :, 7:8], func=AF.Ln, bias=oneT[:], scale=1.0), r=['lruc', 'oneT'], w=['lruc'])
    P.op('dve', lambda e: e.tensor_scalar(out=lruc[:, :, 8:9], in0=lruc[:, :, 7:8], scalar1=-16.0, scalar2=None, op0=ALU.mult), r=['lruc'], w=['lruc'])
    P.op('dve', lambda e: e.tensor_scalar(out=lruc[:, :, 7:8], in0=lruc[:, :, 7:8], scalar1=-8.0, scalar2=None, op0=ALU.mult), r=['lruc'], w=['lruc'])
    P.emit(final=(DBG == 1))
    if DBG == 1:
        es.close(); return nc

    with ExitStack() as st1:
        xst = sb('xst', [128, D], F32, st1)
        xnb = sb('xnb', [128, D], BF16, st1)
        ssq = sb('ssq', [128, 2], F32, st1)
        xnT = sb('xnT', [128, 16, 256], BF16, st1)
        wst = sb('wst', [128, 2, 16, 128], BF16, st1)
        ropeT = sb('ropeT', [64, 2, 256], F32, st1)
        wg = sb('wg', [128, 2, 8, 128], BF16, st1)
        ckv_f = sb('ckv_f', [128, 2, 256], F32, st1)
        sqb = sb('sqb', [128, 256], BF16, st1)
        rstd_b = sb('rstd_b', [128, 256], F32, st1)
        kr_f = sb('kr_f', [64, 2, 256], F32, st1)
        xbes = [sb('xbe%d' % i, [128, 304], F32, st1) for i in range(2)]
        T = [[sb('lt%d_%d' % (j_, i), [128, 256], F32, st1) for i in range(5)] for j_ in range(2)]
        xcbs = [sb('xcb%d' % i, [128, 256], BF16, st1) for i in range(2)]
        stg = sb('stg', [128, 2, 320], F32, st1)
        shist = sb('shist', [128, 8, 48], F32, st1); sstate = sb('sstate', [128, 8, 16], F32, st1)

        dma('pool', 'c1p', wg[:, 0], w_rg.rearrange("n k j -> k n j"), w=['wg'])
        dma('pool', 'c1p', wg[:, 1], w_ig.rearrange("n k j -> k n j"), w=['wg'])
        dma('sp', 'shist', shist[:], scT.rearrange("(c p) k -> p c k", p=128), w=['shist'])
        dma('sp', 'sstate', sstate[:], slT.rearrange("(c p) k -> p c k", p=128), w=['sstate'])

        tiles = [('P', c_, 256) for c_ in range(0, 1024, 256)] + [('O', c_, 256) for c_ in range(0, 1024, 256)] + [('S', 0, 64)]
        wcnt = [0]; scnt = [0]; stgc = [0]
        def do_tile(kind, c0, n):
            src = dict(P=xp, O=xo, S=xs)[kind]
            pcol = dict(P=c0, O=NPF + c0, S=2048)[kind]
            hcol = dict(P=None, O=c0, S=NO)[kind]
            full = kind != 'P'
            nb = (n + 127) // 128
            for i in range(nb):
                rows = min(128, n - i * 128)
                dma('sp', 'xst', xst[0:rows, :], src[c0 + i * 128:c0 + i * 128 + rows, :], w=['xst'])
                P.op('act', lambda e, rows=rows: e.activation(out=xnb[0:rows, :], in_=xst[0:rows, :], func=AF.Square, accum_out=ssq[0:rows, 0:1]),
                     r=['xst'], w=['xnb', 'ssq'])
                P.op('act', lambda e, rows=rows: e.activation(out=ssq[0:rows, 1:2], in_=ssq[0:rows, 0:1], func=AF.Sqrt, bias=epsT[0:rows, :], scale=1.0 / D),
                     r=['ssq', 'epsT'], w=['ssq'])
                P.op('dve', lambda e, rows=rows: e.reciprocal(out=ssq[0:rows, 1:2], in_=ssq[0:rows, 1:2]), r=['ssq'], w=['ssq'])
                P.op('act', lambda e, rows=rows: e.activation(out=xnb[0:rows, :], in_=xst[0:rows, :], func=AF.Copy, scale=ssq[0:rows, 1:2]),
                     r=['xst', 'ssq'], w=['xnb'])
                for hb in range(2):
                    bank = ps[hb]
                    pv = bank[:].bitcast(BF16)
                    def tr(e, hb=hb, rows=rows, pv=pv):
                        ins = None
                        for k in range(8):
                            ins = e.transpose(pv[:, k * 128:k * 128 + rows], xnb[0:rows, (hb * 8 + k) * 128:(hb * 8 + k + 1) * 128], identb[0:rows, 0:rows])
                        return ins
                    P.op('pe', tr, r=['xnb', 'identb'], w=['ps%d' % hb])
                    def ev(e, hb=hb, rows=rows, pv=pv, i=i):
                        return e.tensor_tensor(out=xnT[:, hb * 8:hb * 8 + 8, i * 128:i * 128 + rows],
                                               in0=pv[:, 0:1024].rearrange("p (k t) -> p k t", k=8)[:, :, 0:rows],
                                               in1=gv[:, GV_MIX + hb * 8:GV_MIX + hb * 8 + 8].unsqueeze(2).to_broadcast([128, 8, rows]), op=ALU.mult)
                    P.op('dve', ev, r=['ps%d' % hb, 'gv'], w=['xnT'])
                if full:
                    for q4 in range(4):
                        bank = ps[2 + (q4 % 2)]
                        def trf(e, q4=q4, rows=rows, bank=bank):
                            ins = None
                            for k in range(4):
                                ch = q4 * 4 + k
                                ins = e.transpose(bank[:, k * 128:k * 128 + rows], xst[0:rows, ch * 128:(ch + 1) * 128], identf[0:rows, 0:rows])
                            return ins
                        P.op('pe', trf, r=['xst', 'identf'], w=['ps%d' % (2 + q4 % 2)])
                        def evf(e, q4=q4, rows=rows, bank=bank, i=i):
                            return e.activation(out=hT[:, q4 * 4:q4 * 4 + 4, hcol + i * 128:hcol + i * 128 + rows],
                                                in_=bank[:, 0:512].rearrange("p (k t) -> p k t", k=4)[:, :, 0:rows], func=AF.Copy)
                        P.op('act', evf, r=['ps%d' % (2 + q4 % 2)], w=['hT'])

            if DBG == 2: return
            def proj(col0, ncol, dst_bank, wsrc=None):
                b = wcnt[0] % 2; wcnt[0] += 1
                wt = wst[:, b, :, 0:ncol]; lane = 'wst%d' % b; res = 'wst%d' % b
                srcw = (wsrc if wsrc is not None else w_in[:, col0:col0 + ncol]).rearrange("(k p) c -> p k c", p=128)
                dma('pool', lane, wt, srcw, w=[res])
                def mm(e, wt=wt, ncol=ncol, dst_bank=dst_bank):
                    ins = None
                    for k in range(16):
                        ins = e.matmul(ps[dst_bank][0:ncol, 0:n], lhsT=wt[:, k, :], rhs=xnT[:, k, 0:n], start=(k == 0), stop=(k == 15))
                    return ins
                P.op('pe', mm, r=[res, 'xnT'], w=['ps%d' % dst_bank])

            def fm_norm(srcs, nfeat, gcol, outs, tag):
                tags = list(tag) if isinstance(tag, (list, tuple)) else [tag]
                for j, s_ in enumerate(srcs):
                    P.op('act', lambda e, s_=s_: e.activation(out=sqb[:, 0:n], in_=s_, func=AF.Square), r=tags, w=['sqb'])
                    P.op('pe', lambda e, j=j: e.matmul(ps[7][:, 0:n], lhsT=onesb[:], rhs=sqb[:, 0:n], start=(j == 0), stop=(j == len(srcs) - 1)),
                         r=['sqb', 'onesb'], w=['ps7'])
                P.op('act', lambda e: e.activation(out=rstd_b[:, 0:n], in_=ps[7][:, 0:n], func=AF.Sqrt, bias=epsT[:], scale=1.0 / nfeat),
                     r=['ps7', 'epsT'], w=['rstd_b'])
                P.op('dve', lambda e: e.reciprocal(out=rstd_b[:, 0:n], in_=rstd_b[:, 0:n]), r=['rstd_b'], w=['rstd_b'])
                for j, s_ in enumerate(srcs):
                    for (o_, ores) in outs[j]:
                        P.op('dve', lambda e, s_=s_, o_=o_, j=j: e.scalar_tensor_tensor(out=o_, in0=s_, scalar=gv[:, gcol + j:gcol + j + 1], in1=rstd_b[:, 0:n],
                                                                                       op0=ALU.mult, op1=ALU.mult), r=tags + ['gv', 'rstd_b'], w=[ores])

            if full:
                for j in range(4):
                    proj(j * 128, 128, 4 + j % 2)
                    P.op('act', lambda e, j=j: e.activation(out=T[0][j][:, 0:n], in_=ps[4 + j % 2][:, 0:n], func=AF.Copy), r=['ps%d' % (4 + j % 2)], w=['cqf', 'xc0', 'rr0', 'ii0', 'aa0'])
                fm_norm([T[0][j][:, 0:n] for j in range(4)], 512, GV_Q, [[(cqT[:, j, hcol:hcol + n], 'cqT')] for j in range(4)], ['cqf', 'xc0', 'rr0', 'ii0', 'aa0'])
            if DBG == 3: return
            for j in range(2):
                proj(512 + j * 128, 128, 4 + j % 2)
                P.op('act', lambda e, j=j: e.activation(out=ckv_f[:, j, 0:n], in_=ps[4 + j % 2][:, 0:n], func=AF.Copy), r=['ps%d' % (4 + j % 2)], w=['ckv'])
            fm_norm([ckv_f[:, j, 0:n] for j in range(2)], 256, GV_KV, [[(ckv_f[:, j, 0:n], 'ckv')] for j in range(2)], 'ckv')
            for j in range(2):
                P.op('pool', lambda e, j=j: e.tensor_copy(out=latT[:, j, pcol:pcol + n], in_=ckv_f[:, j, 0:n]), r=['ckv'], w=['latT'])
            if DBG == 4: return
            dma('sp', 'ropeT', ropeT[:, 0, 0:n], ropeC[:, pcol:pcol + n], w=['rope'])
            dma('sp', 'ropeT', ropeT[:, 1, 0:n], ropeS[:, pcol:pcol + n], w=['rope'])
            proj(768, 64, 4)
            P.op('act', lambda e: e.activation(out=kr_f[:, 0, 0:n], in_=ps[4][0:64, 0:n], func=AF.Copy), r=['ps4'], w=['krf0'])
            proj(0, 64, 5, wsrc=wkrp)
            P.op('dve', lambda e: e.tensor_tensor(out=kr_f[:, 1, 0:n], in0=ps[5][0:64, 0:n], in1=ropeT[:, 1, 0:n], op=ALU.mult), r=['ps5', 'rope'], w=['krf1'])
            P.op('dve', lambda e: e.tensor_tensor(out=kr_f[:, 0, 0:n], in0=kr_f[:, 0, 0:n], in1=ropeT[:, 0, 0:n], op=ALU.mult), r=['krf0', 'rope'], w=['krf0'])
            P.op('dve', lambda e: e.tensor_tensor(out=kr_f[:, 0, 0:n], in0=kr_f[:, 0, 0:n], in1=kr_f[:, 1, 0:n], op=ALU.add), r=['krf0', 'krf1'], w=['krf0'])
            P.op('pool', lambda e: e.tensor_copy(out=krT[:, pcol:pcol + n], in_=kr_f[:, 0, 0:n]), r=['krf0'], w=['krT'])
            if DBG == 5: return
            for i in range(nb):
                rows = min(128, n - i * 128)
                kb = (pcol + i * 128) // 128
                def trl(e, i=i, rows=rows):
                    e.transpose(ps[6][0:rows, 0:128], ckv_f[:, 0, i * 128:i * 128 + rows], identf[:])
                    e.transpose(ps[6][0:rows, 128:256], ckv_f[:, 1, i * 128:i * 128 + rows], identf[:])
                    return e.transpose(ps[6][0:rows, 256:320], kr_f[:, 0, i * 128:i * 128 + rows], identf[0:64, 0:64])
                P.op('pe', trl, r=['ckv', 'krf0', 'identf'], w=['ps6'])
                P.op('dve', lambda e, rows=rows, kb=kb: e.tensor_copy(out=lat_tok[0:rows, kb, 0:256], in_=ps[6][0:rows, 0:256]), r=['ps6'], w=['lat_tok'])
                if full and DBG != 7:
                    sb_ = stgc[0] % 2; stgc[0] += 1
                    P.op('act', lambda e, rows=rows, sb_=sb_: e.activation(out=stg[0:rows, sb_, :], in_=ps[6][0:rows, 0:320], func=AF.Copy), r=['ps6'], w=['stg%d' % sb_])
                    lo, ko = (lat_o, kr_o) if kind == 'O' else (lat_s, kr_s)
                    r0 = c0 + i * 128
                    if DBG != 8:
                        dma('sp', 'stg%d' % sb_, lo[r0:r0 + rows, :], stg[0:rows, sb_, 0:256], r=['stg%d' % sb_])
                    if DBG not in (8, 9):
                        dma('sp', 'stg%d' % sb_, ko[r0:r0 + rows, :], stg[0:rows, sb_, 256:320], r=['stg%d' % sb_])

            if DBG in (6, 7, 8, 9): return
            H = 48 if kind == 'S' else 3
            sh = 16 if kind == 'S' else 1
            if kind == 'O' and c0 == 0:
                P.op('dve', lambda e: e.tensor_scalar(out=hist[:], in0=hist[:], scalar1=flg[:, 0:1], scalar2=None, op0=ALU.mult), r=['hist', 'flg'], w=['hist'])
                P.op('dve', lambda e: e.tensor_scalar(out=state[:], in0=state[:], scalar1=flg[:, 0:1], scalar2=None, op0=ALU.mult), r=['state', 'flg'], w=['state'])
            def do_chunk(c):
                pp = c % 2
                xc, rr, ii, aa, uu = T[pp]; yy = aa; xbe = xbes[pp]; xcb = xcbs[pp]
                proj(832 + c * 128, 128, 4)
                hsrc = shist[:, c, :] if kind == 'S' else hist[:, c, :]
                P.op('pool', lambda e, hsrc=hsrc: e.tensor_copy(out=xbe[:, 0:H], in_=hsrc), r=['hist', 'shist'], w=['xbe%d' % pp])
                P.op('act', lambda e: e.activation(out=xbe[:, H:H + n], in_=ps[4][:, 0:n], func=AF.Copy), r=['ps4'], w=['xbe%d' % pp])
                P.op('dve', lambda e, c=c: e.tensor_scalar(out=xc[:, 0:n], in0=xbe[:, 0:n], scalar1=lruc[:, c, 0:1], scalar2=lruc[:, c, 4:5], op0=ALU.mult, op1=ALU.add),
                     r=['xbe%d' % pp, 'lruc'], w=['xc%d' % pp])
                for k in range(1, 4):
                    P.op('dve', lambda e, c=c, k=k: e.scalar_tensor_tensor(out=xc[:, 0:n], in0=xbe[:, k * sh:k * sh + n], scalar=lruc[:, c, k:k + 1], in1=xc[:, 0:n],
                                                                         op0=ALU.mult, op1=ALU.add), r=['xbe%d' % pp, 'lruc', 'xc%d' % pp], w=['xc%d' % pp])
                if kind != 'S':
                    P.op('pool', lambda e, c=c: e.tensor_copy(out=hist[:, c, :], in_=xbe[:, n:n + 3]), r=['xbe%d' % pp], w=['hist'])
                else:
                    P.op('pool', lambda e, c=c: e.tensor_copy(out=shist[:, c, :], in_=xbe[:, 64:112]), r=['xbe%d' % pp], w=['shist'])
                P.op('pool', lambda e: e.tensor_copy(out=xcb[:, 0:n], in_=xc[:, 0:n]), r=['xc%d' % pp], w=['xcb%d' % pp])
                P.op('pe', lambda e, c=c: e.matmul(ps[5][:, 0:n], lhsT=wg[:, 0, c, :], rhs=xcb[:, 0:n], start=True, stop=True), r=['wg', 'xcb%d' % pp], w=['ps5'])
                P.op('pe', lambda e, c=c: e.matmul(ps[6][:, 0:n], lhsT=wg[:, 1, c, :], rhs=xcb[:, 0:n], start=True, stop=True), r=['wg', 'xcb%d' % pp], w=['ps6'])
                P.op('act', lambda e, c=c: e.activation(out=rr[:, 0:n], in_=ps[5][:, 0:n], func=AF.Sigmoid, bias=lruc[:, c, 5:6], scale=1.0), r=['ps5', 'lruc'], w=['rr%d' % pp])
                P.op('act', lambda e, c=c: e.activation(out=ii[:, 0:n], in_=ps[6][:, 0:n], func=AF.Sigmoid, bias=lruc[:, c, 6:7], scale=1.0), r=['ps6', 'lruc'], w=['ii%d' % pp])
                P.op('act', lambda e, c=c: e.activation(out=aa[:, 0:n], in_=rr[:, 0:n], func=AF.Exp, scale=lruc[:, c, 7:8]), r=['rr%d' % pp, 'lruc'], w=['aa%d' % pp])
                P.op('act', lambda e, c=c: e.activation(out=rr[:, 0:n], in_=rr[:, 0:n], func=AF.Exp, scale=lruc[:, c, 8:9]), r=['rr%d' % pp, 'lruc'], w=['rr%d' % pp])
                P.op('act', lambda e: e.activation(out=rr[:, 0:n], in_=rr[:, 0:n], func=AF.Sqrt, bias=oneT[:], scale=-1.0), r=['rr%d' % pp, 'oneT'], w=['rr%d' % pp])
                P.op('dve', lambda e: e.tensor_tensor(out=uu[:, 0:n], in0=ii[:, 0:n], in1=xc[:, 0:n], op=ALU.mult), r=['ii%d' % pp, 'xc%d' % pp], w=['uu%d' % pp])
                P.op('dve', lambda e: e.tensor_tensor(out=uu[:, 0:n], in0=uu[:, 0:n], in1=rr[:, 0:n], op=ALU.mult), r=['uu%d' % pp, 'rr%d' % pp], w=['uu%d' % pp])
                if kind != 'S':
                    P.op('dve', lambda e, c=c: e.tensor_tensor_scan(out=uu[:, 0:n], data0=aa[:, 0:n], data1=uu[:, 0:n], initial=state[:, c, :], op0=ALU.mult, op1=ALU.add),
                         r=['aa%d' % pp, 'uu%d' % pp, 'state'], w=['uu%d' % pp])
                    P.op('dve', lambda e, c=c: e.tensor_copy(out=state[:, c, :], in_=uu[:, n - 1:n]), r=['uu%d' % pp], w=['state'])
                else:
                    for t in range(4):
                        prev = sstate[:, c, :] if t == 0 else uu[:, (t - 1) * 16:t * 16]
                        P.op('dve', lambda e, t=t, prev=prev: e.tensor_tensor(out=aa[:, t * 16:t * 16 + 16], in0=aa[:, t * 16:t * 16 + 16], in1=prev, op=ALU.mult),
                             r=['aa%d' % pp, 'uu%d' % pp, 'sstate'], w=['aa%d' % pp])
                        P.op('dve', lambda e, t=t: e.tensor_tensor(out=uu[:, t * 16:t * 16 + 16], in0=uu[:, t * 16:t * 16 + 16], in1=aa[:, t * 16:t * 16 + 16], op=ALU.add),
                             r=['aa%d' % pp, 'uu%d' % pp], w=['uu%d' % pp])
                    P.op('dve', lambda e, c=c: e.tensor_copy(out=sstate[:, c, :], in_=uu[:, 48:64]), r=['uu%d' % pp], w=['sstate'])
                if full:
                    proj(1856 + c * 128, 128, 7)
                    P.op('act', lambda e: e.activation(out=yy[:, 0:n], in_=ps[7][:, 0:n], func=AF.Copy), r=['ps7'], w=['aa%d' % pp])
                    P.op('act', lambda e: e.activation(out=ii[:, 0:n], in_=ps[7][:, 0:n], func=AF.Square), r=['ps7'], w=['ii%d' % pp])
                    P.op('dve', lambda e: e.tensor_scalar(out=ii[:, 0:n], in0=ii[:, 0:n], scalar1=0.044715, scalar2=1.0, op0=ALU.mult, op1=ALU.add), r=['ii%d' % pp], w=['ii%d' % pp])
                    P.op('dve', lambda e: e.tensor_tensor(out=ii[:, 0:n], in0=ii[:, 0:n], in1=yy[:, 0:n], op=ALU.mult), r=['ii%d' % pp, 'aa%d' % pp], w=['ii%d' % pp])
                    P.op('act', lambda e: e.activation(out=ii[:, 0:n], in_=ii[:, 0:n], func=AF.Sigmoid, scale=1.5957691216057308), r=['ii%d' % pp], w=['ii%d' % pp])
                    P.op('dve', lambda e: e.tensor_tensor(out=ii[:, 0:n], in0=ii[:, 0:n], in1=yy[:, 0:n], op=ALU.mult), r=['ii%d' % pp, 'aa%d' % pp], w=['ii%d' % pp])
                    P.op('dve', lambda e, c=c: e.tensor_tensor(out=mixT[:, 8 + c, hcol:hcol + n], in0=ii[:, 0:n], in1=uu[:, 0:n], op=ALU.mult), r=['ii%d' % pp, 'uu%d' % pp], w=['mixT'])
            for c_ in range(8):
                do_chunk(c_)
        for (kind_, c0_, n_) in tiles:
            do_tile(kind_, c0_, n_)
        dma('sp', 'o1', lru_o, state[:].rearrange("p c o -> p (c o)"), r=['state'])
        dma('sp', 'o2', conv_o, hist[:].rearrange("p c k -> p (c k)"), r=['hist'])
        dma('sp', 'o3', lru_s.rearrange("(c p) k -> p c k", p=128), sstate[:], r=['sstate'])
        dma('sp', 'o4', conv_s.rearrange("(c p) k -> p c k", p=128), shist[:], r=['shist'])
        P.emit(final=(STAGE == 1))
    if STAGE == 1:
        es_mid.close(); es.close(); return nc

    Sv = psall[:, 0:4, :].rearrange("p b c -> p (b c)")
    with ExitStack() as st2:
        wuvs = sb('wuvs', [128, 8, 2, 128], BF16, st2); smk = sb('smk', [32, 4], F32, st2)
        pts = sb('pts', [128, 16], I32, st2)
        qlS = sb('qlS', [128, 8, 2, 64], BF16, st2); qrSs = sb('qrSs', [64, 8, 64], BF16, st2)
        OTS = sb('OTS', [128, 2, 16, 32], BF16, st2)
        st2a = ExitStack()
        wqn = sb('wqn', [128, 4, 8, 128], BF16, st2a); wqr = sb('wqr', [128, 4, 8, 64], BF16, st2a); wqp = sb('wqp', [128, 4, 8, 64], BF16, st2a)
        wuk = sb('wuk', [128, 8, 256], BF16, st2a)
        trit = sb('trit', [128, 128], F32, st2a)
        qn = sb('qn', [128, 8, 128], BF16, st2a); qlT = sb('qlT', [128, 8, 2, 128], BF16, st2a); qrT = sb('qrT', [64, 8, 128], BF16, st2a)
        rtab = sb('rtab', [64, 2, 128], F32, st2a); qa = sb('qa', [64, 2, 4, 128], F32, st2a)
        Pm = sb('Pm', [128, 2048], BF16, st2a); PT = sb('PT', [128, 16, 128], BF16, st2a)
        stt = sb('stt', [128, 8], F32, st2a)
        On = sb('On', [128, 256], BF16, st2a); OT = sb('OT', [128, 8, 2, 128], BF16, st2a)
        w_uq3 = w_uq.rearrange("(k p) (h j) -> p k h j", p=128, h=8)
        wqrp3 = wqrp.rearrange("(k p) h j -> p k h j", p=128)
        for k in range(4):
            dma('pool', 'wq', wqn[:, k], w_uq3[:, k, :, 0:128], w=['wq'])
            dma('pool', 'wq', wqr[:, k], w_uq3[:, k, :, 128:192], w=['wq'])
            dma('pool', 'wq', wqp[:, k], wqrp3[:, k], w=['wq'])
        dma('pool', 'wq', wuk[:], wukT.rearrange("h n c -> n h c"), w=['wq'])
        for h in range(8):
            dma('pool', 'wuvs', wuvs[:, h], wuv[h].rearrange("(cc p) v -> p cc v", p=128), w=['wuvs'])
        dma('sp', 'trit', trit[:], tri, w=['trit']); dma('sp', 'smk', smk[:], smask, w=['smk'])

        def qproj(hc, nq, pcol):
            dma('sp', 'rtab', rtab[:, 0, 0:nq], ropeC[:, pcol:pcol + nq], w=['rtab'])
            dma('sp', 'rtab', rtab[:, 1, 0:nq], ropeS[:, pcol:pcol + nq], w=['rtab'])
            for hg in range(2):
                def mmn(e, hg=hg):
                    ins = None
                    for h4 in range(4):
                        for k in range(4):
                            ins = e.matmul(ps[6][:, h4 * 128:h4 * 128 + nq], lhsT=wqn[:, k, hg * 4 + h4, :], rhs=cqT[:, k, hc:hc + nq], start=(k == 0), stop=(k == 3))
                    return ins
                P.op('pe', mmn, r=['wq', 'cqT'], w=['ps6'])
                P.op('act', lambda e, hg=hg: e.activation(out=qn[:, hg * 4:hg * 4 + 4, 0:nq], in_=ps[6].rearrange("p (h t) -> p h t", h=4)[:, :, 0:nq], func=AF.Copy),
                     r=['ps6'], w=['qn'])
            for hp in range(4):
                def mml(e, hp=hp):
                    ins = None
                    for i2 in range(4):
                        h = hp * 2 + i2 // 2; cc = i2 % 2
                        ins = e.matmul(ps[7][:, i2 * 128:i2 * 128 + nq], lhsT=wuk[:, h, cc * 128:(cc + 1) * 128], rhs=qn[:, h, 0:nq], start=True, stop=True)
                    return ins
                P.op('pe', mml, r=['wq', 'qn'], w=['ps7'])
                P.op('dve', lambda e, hp=hp: e.tensor_copy(out=qlT[:, hp * 2:hp * 2 + 2, :, 0:nq],
                                                           in_=ps[7].rearrange("p (h c t) -> p h c t", h=2, c=2)[:, :, :, 0:nq]), r=['ps7'], w=['qlT'])
            for hg in range(2):
                for which, wt, bank in ((0, wqr, 6), (1, wqp, 7)):
                    def mmr(e, hg=hg, wt=wt, bank=bank):
                        ins = None
                        for h4 in range(4):
                            for k in range(4):
                                ins = e.matmul(ps[bank][0:64, h4 * 128:h4 * 128 + nq], lhsT=wt[:, k, hg * 4 + h4, :], rhs=cqT[:, k, hc:hc + nq], start=(k == 0), stop=(k == 3))
                        return ins
                    P.op('pe', mmr, r=['wq', 'cqT'], w=['ps%d' % bank])
                    P.op('dve', lambda e, which=which, bank=bank: e.tensor_tensor(
                        out=qa[:, which, :, 0:nq], in0=ps[bank][0:64, :].rearrange("p (h t) -> p h t", h=4)[:, :, 0:nq],
                        in1=rtab[:, which, 0:nq].unsqueeze(1).to_broadcast([64, 4, nq]), op=ALU.mult), r=['ps%d' % bank, 'rtab'], w=['qa%d' % which])
                P.op('dve', lambda e, hg=hg: e.tensor_tensor(out=qrT[:, hg * 4:hg * 4 + 4, 0:nq], in0=qa[:, 0, :, 0:nq], in1=qa[:, 1, :, 0:nq], op=ALU.add),
                     r=['qa0', 'qa1'], w=['qrT'])

        def do_block(j):
            hc = j * 128
            qproj(hc, 128, NPF + hc)
            nk = NPF + (j + 1) * 128
            nkc = nk // 128
            ngrp = (nk + 511) // 512
            for h in range(8):
                def sc(e, h=h):
                    ins = None
                    for g in range(ngrp):
                        k0 = g * 512; kw = min(512, nk - k0)
                        e.matmul(ps[g][:, 0:kw], lhsT=qlT[:, h, 0, :], rhs=latT[:, 0, k0:k0 + kw], start=True, stop=False)
                        e.matmul(ps[g][:, 0:kw], lhsT=qlT[:, h, 1, :], rhs=latT[:, 1, k0:k0 + kw], start=False, stop=False)
                        ins = e.matmul(ps[g][:, 0:kw], lhsT=qrT[:, h, :], rhs=krT[:, k0:k0 + kw], start=False, stop=True)
                    return ins
                SB = ['ps0', 'ps1', 'ps2', 'ps3']
                P.op('pe', sc, r=['qlT', 'qrT', 'latT', 'krT'], w=SB)
                P.op('dve', lambda e: e.tensor_scalar(out=Sv[:, 0:NPF], in0=Sv[:, 0:NPF], scalar1=flg[:, 1:2], scalar2=None, op0=ALU.add), r=SB + ['flg'], w=SB)
                P.op('dve', lambda e: e.tensor_tensor(out=Sv[:, nk - 128:nk], in0=Sv[:, nk - 128:nk], in1=trit[:], op=ALU.add), r=SB + ['trit'], w=SB)
                P.op('dve', lambda e: e.reduce_max(out=stt[:, 0:1], in_=Sv[:, 0:nk], axis=AX.X), r=SB, w=['stt0'])
                P.op('dve', lambda e: e.tensor_scalar(out=stt[:, 1:2], in0=stt[:, 0:1], scalar1=-SM_SCALE, scalar2=None, op0=ALU.mult), r=['stt0'], w=['stt1'])
                P.op('act', lambda e: e.activation(out=Pm[:, 0:nk], in_=Sv[:, 0:nk], func=AF.Exp, bias=stt[:, 1:2], scale=SM_SCALE), r=SB + ['stt1'], w=['Pm'])
                for half in range(2):
                    c_lo = half * 8; c_hi = min(nkc, c_lo + 8)
                    if c_hi <= c_lo: continue
                    pv4 = ps[4][:].bitcast(BF16)
                    def trp(e, c_lo=c_lo, c_hi=c_hi, pv4=pv4):
                        ins = None
                        for kc in range(c_lo, c_hi):
                            ins = e.transpose(pv4[:, (kc - c_lo) * 128:(kc - c_lo + 1) * 128], Pm[:, kc * 128:(kc + 1) * 128], identb[:])
                        return ins
                    P.op('pe', trp, r=['Pm', 'identb'], w=['ps4'])
                    P.op('dve', lambda e, c_lo=c_lo, c_hi=c_hi, pv4=pv4: e.tensor_copy(
                        out=PT[:, c_lo:c_hi, :], in_=pv4[:, 0:(c_hi - c_lo) * 128].rearrange("p (k t) -> p k t", t=128)), r=['ps4'], w=['PT'])
                def pvm(e):
                    ins = None
                    for kc in range(nkc):
                        ins = e.matmul(ps[5][:, 0:257], lhsT=PT[:, kc, :], rhs=lat_tok[:, kc, 0:257], start=(kc == 0), stop=(kc == nkc - 1))
                    return ins
                P.op('pe', pvm, r=['PT', 'lat_tok', 'lat_tok_ones'], w=['ps5'])
                P.op('dve', lambda e: e.reciprocal(out=stt[:, 2:3], in_=ps[5][:, 256:257]), r=['ps5'], w=['stt2'])
                P.op('act', lambda e: e.activation(out=On[:], in_=ps[5][:, 0:256], func=AF.Copy, scale=stt[:, 2:3]), r=['ps5', 'stt2'], w=['On'])
                pv6 = ps[6][:].bitcast(BF16)
                def tro(e, pv6=pv6):
                    e.transpose(pv6[:, 0:128], On[:, 0:128], identb[:])
                    return e.transpose(pv6[:, 128:256], On[:, 128:256], identb[:])
                P.op('pe', tro, r=['On', 'identb'], w=['ps6'])
                P.op('act', lambda e, h=h, pv6=pv6: e.activation(out=OT[:, h, :, :], in_=pv6[:, 0:256].rearrange("p (c t) -> p c t", c=2), func=AF.Copy), r=['ps6'], w=['OT'])
            for hg in range(2):
                def mmo(e, hg=hg):
                    ins = None
                    for h4 in range(4):
                        h = hg * 4 + h4
                        e.matmul(ps[7][:, h4 * 128:(h4 + 1) * 128], lhsT=wuvs[:, h, 0, :], rhs=OT[:, h, 0, :], start=True, stop=False)
                        ins = e.matmul(ps[7][:, h4 * 128:(h4 + 1) * 128], lhsT=wuvs[:, h, 1, :], rhs=OT[:, h, 1, :], start=False, stop=True)
                    return ins
                P.op('pe', mmo, r=['wuvs', 'OT'], w=['ps7'])
                P.op('act', lambda e, hg=hg, hc=hc: e.activation(out=mixT[:, hg * 4:hg * 4 + 4, hc:hc + 128], in_=ps[7].rearrange("p (h t) -> p h t", h=4), func=AF.Copy),
                     r=['ps7'], w=['mixT'])

        for j_ in range(8 if DBG != 21 else 1):
            do_block(j_)

        dma('sp', 'pts', pts[:], ptT, w=['pts'])
        P.op('dve', lambda e: e.tensor_single_scalar(out=pts[:], in_=pts[:], scalar=4, op=ALU.logical_shift_left), r=['pts'], w=['pts'])
        if DBG == 21:
            P.op('pool', lambda e: e.memset(OTS[:], 0.0), w=['OTS0', 'OTS1'])
        qproj(NO, 64, 2048)
        P.op('pool', lambda e: e.tensor_copy(out=qlS[:], in_=qlT[:, :, :, 0:64]), r=['qlT'], w=['qlS'])
        P.op('pool', lambda e: e.tensor_copy(out=qrSs[:], in_=qrT[:, :, 0:64]), r=['qrT'], w=['qrSs'])
        P.emit()
        st2a.close()
        st2b = st2
        NCH = 2
        Lg = sb('Lg', [128, NCH, 2, 8, 256], BF16, st2b); Kg = sb('Kg', [128, NCH, 2, 8, 64], BF16, st2b)
        KT = sb('KT', [128, NCH, 3, 512], BF16, st2b)
        qS = sb('qS', [128, NCH, 2, 32], BF16, st2b); qrS = sb('qrS', [64, NCH, 32], BF16, st2b)
        Ps = sb('Ps', [32, NCH, 512], BF16, st2b); PTs = sb('PTs', [128, NCH, 4, 32], BF16, st2b)
        sst = sb('sst', [32, NCH, 12], F32, st2b)
        Oa = sb('Oa', [32, NCH, 256], F32, st2b); Ob = sb('Ob', [32, NCH, 256], BF16, st2b)
        Ln = sb('Ln', [4, NCH, 256], BF16, st2b); PnT = sb('PnT', [4, NCH, 32], BF16, st2b)
        clv = cl.rearrange("n (a r) c -> (n a) (r c)", r=8)
        ckv = ck.rearrange("n (a r) c -> (n a) (r c)", r=8)

        def do_sample(s_, c):
            bk = 4 * c
            A, Bk, C_, D_ = bk, bk + 1, bk + 2, bk + 3
            rA, rB, rC, rD = 'ps%d' % A, 'ps%d' % Bk, 'ps%d' % C_, 'ps%d' % D_
            T_ = lambda n_: '%s_%d' % (n_, c)
            pvA = ps[A][:].bitcast(BF16); pvB = ps[Bk][:].bitcast(BF16)
            st_ = sst[:, c, :]
            def softmax_update(nkeys, srcS, first):
                P.op('dve', lambda e: e.reduce_max(out=st_[:, 1:2], in_=srcS, axis=AX.X), r=[rC], w=[T_('mx')])
                if first:
                    P.op('dve', lambda e: e.tensor_copy(out=st_[:, 2:3], in_=st_[:, 1:2]), r=[T_('mx')], w=[T_('mn')])
                else:
                    P.op('dve', lambda e: e.tensor_tensor(out=st_[:, 2:3], in0=st_[:, 0:1], in1=st_[:, 1:2], op=ALU.max), r=[T_('m'), T_('mx')], w=[T_('mn')])
                    P.op('dve', lambda e: e.tensor_tensor(out=st_[:, 3:4], in0=st_[:, 0:1], in1=st_[:, 2:3], op=ALU.subtract), r=[T_('m'), T_('mn')], w=[T_('d')])
                    P.op('act', lambda e: e.activation(out=st_[:, 4:5], in_=st_[:, 3:4], func=AF.Exp, scale=SM_SCALE), r=[T_('d')], w=[T_('corr')])
                P.op('dve', lambda e: e.tensor_scalar(out=st_[:, 5:6], in0=st_[:, 2:3], scalar1=-SM_SCALE, scalar2=None, op0=ALU.mult), r=[T_('mn')], w=[T_('nb')])
                P.op('dve', lambda e: e.tensor_copy(out=st_[:, 0:1], in_=st_[:, 2:3]), r=[T_('mn')], w=[T_('m')])
                P.op('act', lambda e: e.activation(out=Ps[:, c, 0:nkeys], in_=srcS, func=AF.Exp, bias=st_[:, 5:6], scale=SM_SCALE, accum_out=st_[:, 7:8]),
                     r=[rC, T_('nb')], w=[T_('Ps'), T_('lu')])
                if first:
                    P.op('dve', lambda e: e.tensor_copy(out=st_[:, 6:7], in_=st_[:, 7:8]), r=[T_('lu')], w=[T_('l')])
                else:
                    P.op('dve', lambda e: e.scalar_tensor_tensor(out=st_[:, 6:7], in0=st_[:, 6:7], scalar=st_[:, 4:5], in1=st_[:, 7:8], op0=ALU.mult, op1=ALU.add),
                         r=[T_('l'), T_('corr'), T_('lu')], w=[T_('l')])
            def acc_update(first):
                if first:
                    P.op('dve', lambda e: e.tensor_copy(out=Oa[:, c, :], in_=ps[D_][0:32, 0:256]), r=[rD], w=[T_('Oa')])
                else:
                    P.op('dve', lambda e: e.scalar_tensor_tensor(out=Oa[:, c, :], in0=Oa[:, c, :], scalar=st_[:, 4:5], in1=ps[D_][0:32, 0:256], op0=ALU.mult, op1=ALU.add),
                         r=[T_('Oa'), T_('corr'), rD], w=[T_('Oa')])
            for cc in range(2):
                P.op('pool', lambda e, cc=cc: e.tensor_copy(out=qS[:, c, cc, :].rearrange("p (h t) -> p h t", h=8), in_=qlS[:, :, cc, s_:64:16]), r=['qlS'], w=[T_('qS')])
            P.op('pool', lambda e: e.tensor_copy(out=qrS[:, c, :].rearrange("p (h t) -> p h t", h=8), in_=qrSs[:, :, s_:64:16]), r=['qrSs'], w=[T_('qrS')])
            yield
            for u in range(32):
                g, q4 = u // 2, u % 2
                gb = g % 2
                if q4 == 0:
                    P.op('pool', lambda e, g=g, gb=gb: e.indirect_dma_start(out=Lg[:, c, gb].rearrange("p r c -> p (r c)"), out_offset=None, in_=clv,
                         in_offset=bass.IndirectOffsetOnAxis(ap=pts[:, s_:s_ + 1], axis=0), element_offset=g * 2048), r=['pts'], w=[T_('Lg%d' % gb)], lane='Lg%d%d' % (c, gb))
                    P.op('pool', lambda e, g=g, gb=gb: e.indirect_dma_start(out=Kg[:, c, gb].rearrange("p r c -> p (r c)"), out_offset=None, in_=ckv,
                         in_offset=bass.IndirectOffsetOnAxis(ap=pts[:, s_:s_ + 1], axis=0), element_offset=g * 512), r=['pts'], w=[T_('Kg%d' % gb)], lane='Kg%d%d' % (c, gb))
                def trk(e, gb=gb, q4=q4):
                    ins = None
                    for r4 in range(4):
                        r_ = q4 * 4 + r4
                        e.transpose(pvA[:, r4 * 128:(r4 + 1) * 128], Lg[:, c, gb, r_, 0:128], identb[:])
                        e.transpose(pvA[:, 512 + r4 * 128:512 + (r4 + 1) * 128], Lg[:, c, gb, r_, 128:256], identb[:])
                        ins = e.transpose(pvB[0:64, r4 * 128:(r4 + 1) * 128], Kg[:, c, gb, r_, :], identb[:])
                    return ins
                P.op('pe', trk, r=[T_('Lg%d' % gb), T_('Kg%d' % gb), 'identb'], w=[rA, rB])
                yield
                P.op('dve', lambda e: e.tensor_copy(out=KT[:, c, 0:2, :], in_=pvA[:, 0:1024].rearrange("p (k t) -> p k t", k=2)), r=[rA], w=[T_('KT01')])
                P.op('act', lambda e: e.activation(out=KT[0:64, c, 2, :], in_=pvB[0:64, 0:512], func=AF.Copy), r=[rB], w=[T_('KT2')])
                def scs(e):
                    o_ = ps[C_][0:32, :]
                    e.matmul(o_, lhsT=qS[:, c, 0, :], rhs=KT[:, c, 0, :], start=True, stop=False)
                    e.matmul(o_, lhsT=qS[:, c, 1, :], rhs=KT[:, c, 1, :], start=False, stop=False)
                    return e.matmul(o_, lhsT=qrS[:, c, :], rhs=KT[0:64, c, 2, :], start=False, stop=True)
                P.op('pe', scs, r=[T_('qS'), T_('qrS'), T_('KT01'), T_('KT2')], w=[rC])
                yield
                softmax_update(512, ps[C_][0:32, :], first=(u == 0))
                def trps(e):
                    ins = None
                    for kc in range(4):
                        ins = e.transpose(pvB[:, 512 + kc * 32:512 + (kc + 1) * 32], Ps[:, c, kc * 128:(kc + 1) * 128], identb[0:32, 0:32])
                    return ins
                P.op('pe', trps, r=[T_('Ps'), 'identb'], w=[rB])
                yield
                P.op('act', lambda e: e.activation(out=PTs[:, c, :, :], in_=pvB[:, 512:640].rearrange("p (k q) -> p k q", q=32), func=AF.Copy), r=[rB], w=[T_('PTs')])
                def pvs(e, gb=gb, q4=q4):
                    ins = None
                    for kc in range(4):
                        ins = e.matmul(ps[D_][0:32, 0:256], lhsT=PTs[:, c, kc, :], rhs=Lg[:, c, gb, q4 * 4 + kc, :], start=(kc == 0), stop=(kc == 3))
                    return ins
                P.op('pe', pvs, r=[T_('PTs'), T_('Lg%d' % gb)], w=[rD])
                acc_update(first=(u == 0))
                yield
            def scn(e):
                o_ = ps[C_][0:32, 0:4]
                e.matmul(o_, lhsT=qS[:, c, 0, :], rhs=latT[:, 0, 2048 + s_:2048 + 64:16], start=True, stop=False)
                e.matmul(o_, lhsT=qS[:, c, 1, :], rhs=latT[:, 1, 2048 + s_:2048 + 64:16], start=False, stop=False)
                return e.matmul(o_, lhsT=qrS[:, c, :], rhs=krT[:, 2048 + s_:2048 + 64:16], start=False, stop=True)
            P.op('pe', scn, r=[T_('qS'), T_('qrS'), 'latT', 'krT'], w=[rC])
            P.op('dve', lambda e: e.tensor_tensor(out=ps[C_][0:32, 0:4], in0=ps[C_][0:32, 0:4], in1=smk[:], op=ALU.add), r=[rC, 'smk'], w=[rC])
            softmax_update(4, ps[C_][0:32, 0:4], first=False)
            yield
            P.op('pe', lambda e: e.matmul(ps[C_][0:4, 0:256], lhsT=identb[0:64, s_:64:16], rhs=lat_tok[0:64, 16, 0:256], start=True, stop=True),
                 r=['identb', 'lat_tok'], w=[rC])
            P.op('act', lambda e: e.activation(out=Ln[:, c, :], in_=ps[C_][0:4, 0:256], func=AF.Copy), r=[rC], w=[T_('Ln')])
            P.op('pe', lambda e: e.transpose(pvB[0:4, 704:736], Ps[:, c, 0:4], identb[0:32, 0:32]), r=[T_('Ps'), 'identb'], w=[rB])
            P.op('act', lambda e: e.activation(out=PnT[:, c, :], in_=pvB[0:4, 704:736], func=AF.Copy), r=[rB], w=[T_('PnT')])
            P.op('pe', lambda e: e.matmul(ps[D_][0:32, 0:256], lhsT=PnT[:, c, :], rhs=Ln[:, c, :], start=True, stop=True), r=[T_('PnT'), T_('Ln')], w=[rD])
            acc_update(first=False)
            yield
            P.op('dve', lambda e: e.reciprocal(out=st_[:, 8:9], in_=st_[:, 6:7]), r=[T_('l')], w=[T_('ri')])
            P.op('act', lambda e: e.activation(out=Ob[:, c, :], in_=Oa[:, c, :], func=AF.Copy, scale=st_[:, 8:9]), r=[T_('Oa'), T_('ri')], w=[T_('Ob')])
            def trob(e):
                e.transpose(pvB[:, 640:672], Ob[:, c, 0:128], identb[0:32, 0:32])
                return e.transpose(pvB[:, 672:704], Ob[:, c, 128:256], identb[0:32, 0:32])
            P.op('pe', trob, r=[T_('Ob'), 'identb'], w=[rB])
            P.op('act', lambda e: e.activation(out=OTS[:, :, s_, :], in_=pvB[:, 640:704].rearrange("p (c q) -> p c q", c=2), func=AF.Copy), r=[rB], w=['OTS%d' % c])
            yield

        NSAMP = 16 if DBG != 21 else 2
        for s0 in range(0, NSAMP, NCH):
            gens = [do_sample(s0 + c_, c_) for c_ in range(NCH)]
            alive = list(gens)
            while alive:
                for g_ in list(alive):
                    try:
                        next(g_)
                    except StopIteration:
                        alive.remove(g_)
        for h in range(8):
            def mmos(e, h=h):
                e.matmul(ps[7][:, 0:64], lhsT=wuvs[:, h, 0, :], rhs=OTS[:, 0, :, h * 4:(h + 1) * 4], start=True, stop=False)
                return e.matmul(ps[7][:, 0:64], lhsT=wuvs[:, h, 1, :], rhs=OTS[:, 1, :, h * 4:(h + 1) * 4], start=False, stop=True)
            P.op('pe', mmos, r=['wuvs', 'OTS0', 'OTS1'], w=['ps7'])
            P.op('act', lambda e, h=h: e.activation(out=mixT[:, h, NO:NO + 64].rearrange("p (t s) -> p s t", t=4),
                                                     in_=ps[7][:, 0:64].rearrange("p (s t) -> p s t", t=4), func=AF.Copy), r=['ps7'], w=['mixT'])
        P.emit(final=(STAGE == 2))
    es_mid.close()
    if STAGE == 2:
        es.close(); return nc

    TILES = [(0, 512), (512, 512), (1024, 64)]
    with ExitStack() as st3:
        sq2 = sb('sq2', [128, 512], BF16, st3)
        rsb = sb('rsb', [128, 512], F32, st3)
        tmpf = sb('tmpf', [128, 512], F32, st3); tmpg = sb('tmpg', [128, 512], F32, st3)
        wbuf = sb('wbuf', [128, 2, 16, 128], BF16, st3)
        wbc = [0]

        def norm_stats(srcs, nfeat, n, tag):
            for j, s_ in enumerate(srcs):
                P.op('act', lambda e, s_=s_: e.activation(out=sq2[:, 0:n], in_=s_, func=AF.Square), r=[tag], w=['sq2'])
                P.op('pe', lambda e, j=j: e.matmul(ps[7][:, 0:n], lhsT=onesb[:], rhs=sq2[:, 0:n], start=(j == 0), stop=(j == len(srcs) - 1)),
                     r=['sq2', 'onesb'], w=['ps7'])
            P.op('act', lambda e: e.activation(out=rsb[:, 0:n], in_=ps[7][:, 0:n], func=AF.Sqrt, bias=epsT[:], scale=1.0 / nfeat), r=['ps7', 'epsT'], w=['rsb'])
            P.op('dve', lambda e: e.reciprocal(out=rsb[:, 0:n], in_=rsb[:, 0:n]), r=['rsb'], w=['rsb'])

        def load_w(src_ap, nk, ncol=128):
            b = wbc[0] % 2; wbc[0] += 1
            wt = wbuf[:, b, 0:nk, 0:ncol]
            dma('pool', 'wbuf%d' % b, wt, src_ap.rearrange("(k p) c -> p k c", p=128), w=['wbuf%d' % b])
            return wt, 'wbuf%d' % b

        for (c0, n) in TILES:
            for (k0, gcol) in ((0, GV_OM), (8, GV_OL)):
                norm_stats([mixT[:, k0 + k, c0:c0 + n] for k in range(8)], 1024, n, 'mixT')
                for k in range(8):
                    P.op('dve', lambda e, k=k, k0=k0, gcol=gcol, c0=c0, n=n: e.scalar_tensor_tensor(
                        out=mixT[:, k0 + k, c0:c0 + n], in0=mixT[:, k0 + k, c0:c0 + n], scalar=gv[:, gcol + k:gcol + k + 1], in1=rsb[:, 0:n],
                        op0=ALU.mult, op1=ALU.mult), r=['mixT', 'gv', 'rsb'], w=['mixT'])
        for m in range(16):
            wt, wres = load_w(w_o[:, m * 128:(m + 1) * 128], 16)
            for ti, (c0, n) in enumerate(TILES):
                bank = 4 + ti % 2
                def mmw(e, wt=wt, c0=c0, n=n, bank=bank):
                    ins = None
                    for k in range(16):
                        ins = e.matmul(ps[bank][:, 0:n], lhsT=wt[:, k, :], rhs=mixT[:, k, c0:c0 + n], start=(k == 0), stop=(k == 15))
                    return ins
                P.op('pe', mmw, r=[wres, 'mixT'], w=['ps%d' % bank])
                P.op('dve', lambda e, m=m, c0=c0, n=n, bank=bank: e.tensor_tensor(out=hT[:, m, c0:c0 + n], in0=hT[:, m, c0:c0 + n], in1=ps[bank][:, 0:n], op=ALU.add),
                     r=['hT', 'ps%d' % bank], w=['hT'])

        def fm_rmsnorm_to_mix(gcol, extra=None):
            for (c0, n) in TILES:
                norm_stats([hT[:, k, c0:c0 + n] for k in range(16)], D, n, 'hT')
                for k in range(16):
                    P.op('dve', lambda e, k=k, c0=c0, n=n: e.scalar_tensor_tensor(out=tmpf[:, 0:n], in0=hT[:, k, c0:c0 + n], scalar=gv[:, gcol + k:gcol + k + 1],
                                                                                 in1=rsb[:, 0:n], op0=ALU.mult, op1=ALU.mult), r=['hT', 'gv', 'rsb'], w=['tmpf'])
                    P.op('pool', lambda e, k=k, c0=c0, n=n: e.tensor_copy(out=mixT[:, k, c0:c0 + n], in_=tmpf[:, 0:n]), r=['tmpf'], w=['mixT'])
                    if extra is not None: extra(k, c0, n)

        if DBG != 31:
          with ExitStack() as st4:
            wr32 = sb('wr32', [128, 16, 36], F32, st4)
            lgT = sb('lgT', [36, NT], F32, st4)
            brow = sb('brow', [128, 36], F32, st4)
            tl = sb('tl', [128, 36], F32, st4); ohg = sb('ohg', [128, 4], F32, st4); em = sb('em', [128, 32], F32, st4)
            m8 = sb('m8', [128, 8], F32, st4); rs_ = sb('rs_', [128, 8], F32, st4); cmb = sb('cmb', [128, 32], F32, st4)
            combT = sb('combT', [32, NT], F32, st4)
            esel = sb('esel', [32, 128], F32, st4); ones32 = sb('ones32', [32, 128], F32, st4)
            cbe = sb('cbe', [128, NT], F32, st4)
            gu = sb('gu', [128, 3, 2, 16, 128], BF16, st4)
            wd = sb('wd', [128, 2, 4, 2048], BF16, st4)
            actT = sb('actT', [128, 4, NT], BF16, st4)
            dma('sp', 'wr32', wr32[:, :, 0:4], w_group.rearrange("(k p) c -> p k c", p=128), w=['wr32'])
            dma('sp', 'wr32', wr32[:, :, 4:36], w_router.rearrange("(k p) c -> p k c", p=128), w=['wr32'])
            dma('sp', 'brow', brow[:, 0:4], b_group.partition_broadcast(128), w=['brow'])
            dma('sp', 'brow', brow[:, 4:36], b_router.partition_broadcast(128), w=['brow'])
            P.op('pool', lambda e: e.memset(ones32[:], 1.0), w=['ones32'])
            cur = {}
            def router_hook(k, c0, n):
                bank = 6
                P.op('pe', lambda e, k=k, n=n: e.matmul(ps[6][0:36, 0:n], lhsT=wr32[:, k, :], rhs=tmpf[:, 0:n], start=(k == 0), stop=(k == 15)),
                     r=['wr32', 'tmpf'], w=['ps6'])
                if k == 15:
                    P.op('act', lambda e, c0=c0, n=n: e.activation(out=lgT[:, c0:c0 + n], in_=ps[6][0:36, 0:n], func=AF.Copy), r=['ps6'], w=['lgT'])
            fm_rmsnorm_to_mix(GV_FFN, router_hook)
            for bi in range(9):
                t0_ = bi * 128; rows = min(128, NT - t0_)
                P.op('pe', lambda e, t0_=t0_, rows=rows: e.transpose(ps[6][0:rows, 0:36], lgT[:, t0_:t0_ + rows], identf[0:36, 0:36]), r=['lgT', 'identf'], w=['ps6'])
                R_ = slice(0, rows)
                P.op('dve', lambda e, R_=R_: e.tensor_tensor(out=tl[R_, :], in0=ps[6][R_, 0:36], in1=brow[R_, :], op=ALU.add), r=['ps6', 'brow'], w=['tl'])
                P.op('dve', lambda e, R_=R_: e.reduce_max(out=rs_[R_, 0:1], in_=tl[R_, 0:4], axis=AX.X), r=['tl'], w=['rs0'])
                P.op('dve', lambda e, R_=R_: e.tensor_scalar(out=ohg[R_, :], in0=tl[R_, 0:4], scalar1=rs_[R_, 0:1], scalar2=None, op0=ALU.is_ge), r=['tl', 'rs0'], w=['ohg'])
                P.op('dve', lambda e, R_=R_: e.tensor_scalar(out=rs_[R_, 1:2], in0=rs_[R_, 0:1], scalar1=-1.0, scalar2=None, op0=ALU.mult), r=['rs0'], w=['rs1'])
                P.op('act', lambda e, R_=R_: e.activation(out=tl[R_, 0:4], in_=tl[R_, 0:4], func=AF.Exp, bias=rs_[R_, 1:2], scale=1.0, accum_out=rs_[R_, 2:3]),
                     r=['tl', 'rs1'], w=['tl', 'rs2'])
                P.op('dve', lambda e, R_=R_: e.tensor_scalar(out=ohg[R_, :], in0=ohg[R_, :], scalar1=-1.0, scalar2=1e30, op0=ALU.add, op1=ALU.mult), r=['ohg'], w=['ohg'])
                P.op('dve', lambda e, R_=R_, rows=rows: e.tensor_tensor(out=em[R_, :].rearrange("p (g x) -> p g x", g=4), in0=tl[R_, 4:36].rearrange("p (g x) -> p g x", g=4),
                                                             in1=ohg[R_, :].unsqueeze(2).to_broadcast([rows, 4, 8]), op=ALU.add), r=['tl', 'ohg'], w=['em'])
                P.op('dve', lambda e, R_=R_: e.max(out=m8[R_, :], in_=em[R_, :]), r=['em'], w=['m8'])
                P.op('dve', lambda e, R_=R_: e.tensor_scalar(out=cmb[R_, :], in0=em[R_, :], scalar1=m8[R_, 1:2], scalar2=None, op0=ALU.is_ge), r=['em', 'm8'], w=['cmb'])
                P.op('dve', lambda e, R_=R_: e.tensor_scalar(out=rs_[R_, 3:4], in0=m8[R_, 0:1], scalar1=-1.0, scalar2=None, op0=ALU.mult), r=['m8'], w=['rs3'])
                P.op('act', lambda e, R_=R_: e.activation(out=em[R_, :], in_=em[R_, :], func=AF.Exp, bias=rs_[R_, 3:4], scale=1.0), r=['em', 'rs3'], w=['em'])
                P.op('act', lambda e, R_=R_: e.activation(out=rs_[R_, 4:5], in_=m8[R_, 1:2], func=AF.Exp, bias=rs_[R_, 3:4], scale=1.0), r=['m8', 'rs3'], w=['rs4'])
                P.op('dve', lambda e, R_=R_: e.tensor_scalar(out=rs_[R_, 4:5], in0=rs_[R_, 4:5], scalar1=1.0, scalar2=rs_[R_, 2:3], op0=ALU.add, op1=ALU.mult), r=['rs4', 'rs2'], w=['rs4'])
                P.op('dve', lambda e, R_=R_: e.reciprocal(out=rs_[R_, 5:6], in_=rs_[R_, 4:5]), r=['rs4'], w=['rs5'])
                P.op('dve', lambda e, R_=R_: e.scalar_tensor_tensor(out=cmb[R_, :], in0=em[R_, :], scalar=rs_[R_, 5:6], in1=cmb[R_, :], op0=ALU.mult, op1=ALU.mult),
                     r=['em', 'rs5', 'cmb'], w=['cmb'])
                P.op('pe', lambda e, rows=rows: e.transpose(ps[7][0:32, 0:rows], cmb[0:rows, :], identf[0:rows, 0:rows]), r=['cmb', 'identf'], w=['ps7'])
                P.op('act', lambda e, t0_=t0_, rows=rows: e.activation(out=combT[:, t0_:t0_ + rows], in_=ps[7][0:32, 0:rows], func=AF.Copy), r=['ps7'], w=['combT'])
            NEXP = 32 if DBG != 32 else 2
            guc = [0]; wdc = [0]
            for ex in range(NEXP):
                P.op('dve', lambda e, ex=ex: e.tensor_scalar(out=esel[:], in0=ones32[:], scalar1=identf[0:32, ex:ex + 1], scalar2=None, op0=ALU.mult), r=['ones32', 'identf'], w=['esel'])
                for ti, (c0, n) in enumerate(TILES):
                    P.op('pe', lambda e, c0=c0, n=n: e.matmul(ps[6][:, 0:n], lhsT=esel[:], rhs=combT[:, c0:c0 + n], start=True, stop=True), r=['esel', 'combT'], w=['ps6'])
                    P.op('act', lambda e, c0=c0, n=n: e.activation(out=cbe[:, c0:c0 + n], in_=ps[6][:, 0:n], func=AF.Copy), r=['ps6'], w=['cbe'])
                wb_ = wdc[0] % 2; wdc[0] += 1
                for fc in range(4):
                    dma('pool', 'wd%d' % wb_, wd[:, wb_, fc, :], w_down[ex, fc * 128:(fc + 1) * 128, :], w=['wd%d' % wb_])
                for fc in range(4):
                    gb = guc[0] % 3; guc[0] += 1
                    dma('pool', 'gu%d' % gb, gu[:, gb, 0], w_gate[ex, :, fc * 128:(fc + 1) * 128].rearrange("(k p) c -> p k c", p=128), w=['gu%d' % gb])
                    dma('pool', 'gu%d' % gb, gu[:, gb, 1], w_up[ex, :, fc * 128:(fc + 1) * 128].rearrange("(k p) c -> p k c", p=128), w=['gu%d' % gb])
                    for ti, (c0, n) in enumerate(TILES):
                        for which in range(2):
                            bank = 2 * (ti % 2) + which
                            def mmg(e, gb=gb, which=which, c0=c0, n=n, bank=bank):
                                ins = None
                                for k in range(16):
                                    ins = e.matmul(ps[bank][:, 0:n], lhsT=gu[:, gb, which, k, :], rhs=mixT[:, k, c0:c0 + n], start=(k == 0), stop=(k == 15))
                                return ins
                            P.op('pe', mmg, r=['gu%d' % gb, 'mixT'], w=['ps%d' % bank])
                        b0 = 2 * (ti % 2)
                        P.op('act', lambda e, n=n, b0=b0: e.activation(out=tmpf[:, 0:n], in_=ps[b0][:, 0:n], func=AF.Silu), r=['ps%d' % b0], w=['tmpf'])
                        P.op('dve', lambda e, n=n, b0=b0: e.tensor_tensor(out=tmpf[:, 0:n], in0=tmpf[:, 0:n], in1=ps[b0 + 1][:, 0:n], op=ALU.mult), r=['tmpf', 'ps%d' % (b0 + 1)], w=['tmpf'])
                        P.op('dve', lambda e, fc=fc, c0=c0, n=n: e.tensor_tensor(out=actT[:, fc, c0:c0 + n], in0=tmpf[:, 0:n], in1=cbe[:, c0:c0 + n], op=ALU.mult),
                             r=['tmpf', 'cbe'], w=['actT'])
                for m in range(16):
                    for ti, (c0, n) in enumerate(TILES):
                        bank = 4 + (m * 3 + ti) % 2
                        def mmd(e, wb_=wb_, m=m, c0=c0, n=n, bank=bank):
                            ins = None
                            for fc in range(4):
                                ins = e.matmul(ps[bank][:, 0:n], lhsT=wd[:, wb_, fc, m * 128:(m + 1) * 128], rhs=actT[:, fc, c0:c0 + n], start=(fc == 0), stop=(fc == 3))
                            return ins
                        P.op('pe', mmd, r=['wd%d' % wb_, 'actT'], w=['ps%d' % bank])
                        P.op('dve', lambda e, m=m, c0=c0, n=n, bank=bank: e.tensor_tensor(out=hT[:, m, c0:c0 + n], in0=hT[:, m, c0:c0 + n], in1=ps[bank][:, 0:n], op=ALU.add),
                             r=['hT', 'ps%d' % bank], w=['hT'])
            P.emit()

        with ExitStack() as st5:
            pT = sb('pT', [128, 2, NT], BF16, st5)
            pst = sb('pst', [128, 256], F32, st5)
            wpp = sb('wpp', [128, 2, 2, 128], BF16, st5)
            fm_rmsnorm_to_mix(GV_PLE)
            for bi in range(9):
                t0_ = bi * 128; rows = min(128, NT - t0_)
                srcp = po[t0_:t0_ + rows, :] if bi < 8 else psm[0:rows, :]
                dma('sp', 'pst', pst[0:rows, :], srcp, w=['pst'])
                def trp_(e, rows=rows):
                    e.transpose(ps[6][:, 0:rows], pst[0:rows, 0:128], identf[0:rows, 0:rows])
                    return e.transpose(ps[6][:, 128:128 + rows], pst[0:rows, 128:256], identf[0:rows, 0:rows])
                P.op('pe', trp_, r=['pst', 'identf'], w=['ps6'])
                P.op('act', lambda e, t0_=t0_, rows=rows: e.activation(out=pT[:, :, t0_:t0_ + rows], in_=ps[6][:, 0:256].rearrange("p (c t) -> p c t", c=2)[:, :, 0:rows], func=AF.Copy),
                     r=['ps6'], w=['pT'])
            for m in range(16):
                wt, wres = load_w(w_pg[:, m * 128:(m + 1) * 128], 16)
                pb_ = m % 2
                dma('pool', 'wpp%d' % pb_, wpp[:, pb_], w_pp[:, m * 128:(m + 1) * 128].rearrange("(k p) c -> p k c", p=128), w=['wpp%d' % pb_])
                for ti, (c0, n) in enumerate(TILES):
                    b0 = 2 * (ti % 2)
                    def mmg2(e, wt=wt, c0=c0, n=n, b0=b0):
                        ins = None
                        for k in range(16):
                            ins = e.matmul(ps[b0][:, 0:n], lhsT=wt[:, k, :], rhs=mixT[:, k, c0:c0 + n], start=(k == 0), stop=(k == 15))
                        return ins
                    P.op('pe', mmg2, r=[wres, 'mixT'], w=['ps%d' % b0])
                    def mmp2(e, pb_=pb_, c0=c0, n=n, b0=b0):
                        e.matmul(ps[b0 + 1][:, 0:n], lhsT=wpp[:, pb_, 0, :], rhs=pT[:, 0, c0:c0 + n], start=True, stop=False)
                        return e.matmul(ps[b0 + 1][:, 0:n], lhsT=wpp[:, pb_, 1, :], rhs=pT[:, 1, c0:c0 + n], start=False, stop=True)
                    P.op('pe', mmp2, r=['wpp%d' % pb_, 'pT'], w=['ps%d' % (b0 + 1)])
                    P.op('act', lambda e, m=m, n=n, b0=b0: e.activation(out=tmpg[:, 0:n], in_=ps[b0][:, 0:n], func=AF.Sigmoid, bias=gv[:, GV_BPG + m:GV_BPG + m + 1], scale=1.0),
                         r=['ps%d' % b0, 'gv'], w=['tmpg'])
                    P.op('dve', lambda e, n=n, b0=b0: e.tensor_tensor(out=tmpg[:, 0:n], in0=tmpg[:, 0:n], in1=ps[b0 + 1][:, 0:n], op=ALU.mult), r=['tmpg', 'ps%d' % (b0 + 1)], w=['tmpg'])
                    P.op('dve', lambda e, m=m, c0=c0, n=n: e.tensor_tensor(out=hT[:, m, c0:c0 + n], in0=hT[:, m, c0:c0 + n], in1=tmpg[:, 0:n], op=ALU.add), r=['hT', 'tmpg'], w=['hT'])
            P.emit()

        with ExitStack() as st6:
            yst = sb('yst', [128, 2, D], F32, st6)
            yc = [0]
            for (c0, n) in TILES:
                norm_stats([hT[:, k, c0:c0 + n] for k in range(16)], D, n, 'hT')
                nb_ = (n + 127) // 128
                for i in range(nb_):
                    rows = min(128, n - i * 128)
                    yb = yc[0] % 2; yc[0] += 1
                    for q4 in range(4):
                        for k4 in range(4):
                            k = q4 * 4 + k4
                            P.op('dve', lambda e, k=k, k4=k4, c0=c0, i=i, rows=rows: e.scalar_tensor_tensor(
                                out=tmpf[:, k4 * 128:k4 * 128 + rows], in0=hT[:, k, c0 + i * 128:c0 + i * 128 + rows], scalar=gv2[:, k:k + 1],
                                in1=rsb[:, i * 128:i * 128 + rows], op0=ALU.mult, op1=ALU.mult), r=['hT', 'gv', 'rsb'], w=['tmpf'])
                        bank = 4 + q4 % 2
                        def try_(e, rows=rows, bank=bank):
                            ins = None
                            for k4 in range(4):
                                ins = e.transpose(ps[bank][0:rows, k4 * 128:(k4 + 1) * 128], tmpf[:, k4 * 128:k4 * 128 + rows], identf[:])
                            return ins
                        P.op('pe', try_, r=['tmpf', 'identf'], w=['ps%d' % bank])
                        P.op('act', lambda e, q4=q4, rows=rows, bank=bank, yb=yb: e.activation(out=yst[0:rows, yb, q4 * 512:(q4 + 1) * 512], in_=ps[bank][0:rows, :], func=AF.Copy),
                             r=['ps%d' % bank], w=['yst%d' % yb])
                    r0 = c0 + i * 128
                    dst = y_o[r0:r0 + rows, :] if c0 < NO else y_s[0:rows, :]
                    dma('sp', 'yst%d' % yb, dst, yst[0:rows, yb, :], r=['yst%d' % yb])
            P.emit(final=True)
    es.close()
    return nc


def _rope_tables(pos):
    inv = 10000.0 ** (-np.arange(32, dtype=np.float32) / 32)
    ang = pos.astype(np.float32)[None, :] * inv[:, None]
    c = np.cos(ang).astype(np.float32); s = np.sin(ang).astype(np.float32)
    return np.concatenate([c, c], 0), np.concatenate([-s, s], 0)


def make_in_maps(inp):
    f = lambda a: np.ascontiguousarray(np.asarray(a, dtype=np.float32))
    x_prompt = np.asarray(inp['x_prompt']); x_sample = np.asarray(inp['x_sample'])
    perm = (np.arange(64) + 32) % 64
    w_in = np.asarray(inp['w_in'])[0]; w_uq = np.asarray(inp['w_uq'])[0]; w_ukv = np.asarray(inp['w_ukv'])[0]
    shared = dict(
        cl=f(inp['cache_latent'][0]), ck=f(inp['cache_krope'][0]),
        g_mix=f(inp['g_mix'][0]), w_in=f(w_in), wkrp=f(w_in[:, 768:832][:, perm]),
        g_q=f(inp['g_q'][0]), w_uq=f(w_uq), wqrp=f(w_uq.reshape(512, 8, 192)[:, :, 128:][:, :, perm]),
        g_kv=f(inp['g_kv'][0]), wukT=f(w_ukv[:, :, :128].transpose(1, 2, 0)), wuv=f(w_ukv[:, :, 128:].transpose(1, 0, 2)),
        w_conv=f(inp['w_conv'][0]), b_conv=f(inp['b_conv'][0]), w_rg=f(inp['w_rg'][0]), b_rg=f(inp['b_rg'][0]),
        w_ig=f(inp['w_ig'][0]), b_ig=f(inp['b_ig'][0]), lam=f(inp['lru_lambda'][0]),
        g_om=f(inp['g_out_mla'][0]), g_ol=f(inp['g_out_lru'][0]), w_o=f(inp['w_o'][0]), g_ffn=f(inp['g_ffn'][0]),
        w_group=f(inp['w_group'][0]), b_group=f(inp['b_group'][0]), w_router=f(inp['w_router'][0]), b_router=f(inp['b_router'][0]),
        w_gate=f(inp['w_gate'][0]), w_up=f(inp['w_up'][0]), w_down=f(inp['w_down'][0]),
        g_ple=f(inp['g_ple'][0]), w_pg=f(inp['w_ple_gate'][0]), b_pg=f(inp['b_ple_gate'][0]), w_pp=f(inp['w_ple_proj'][0]),
        g_fin=f(inp['g_final']),
        tri=np.where(np.arange(128)[None, :] <= np.arange(128)[:, None], 0.0, NEG).astype(np.float32),
        smask=np.where(np.arange(4)[None, :] <= (np.arange(32) % 4)[:, None], 0.0, NEG).astype(np.float32),
    )
    pm = lambda a: np.asarray(a, np.float32).reshape(-1, 128).T
    gvh = np.zeros((128, 104), np.float32)
    for off, key in [(0, 'g_mix'), (16, 'g_q'), (20, 'g_kv'), (22, 'g_out_mla'), (30, 'g_out_lru'), (38, 'g_ffn'), (54, 'g_ple')]:
        v = pm(np.asarray(inp[key])[0]); gvh[:, off:off + v.shape[1]] = v
    gvh[:, 70:86] = pm(inp['g_final'])
    gvh[:, 86:102] = pm(np.asarray(inp['b_ple_gate'])[0])
    lruch = np.zeros((128, 8, 8), np.float32)
    for k_ in range(4): lruch[:, :, k_] = pm(np.asarray(inp['w_conv'])[0, k_])
    for j_, key in enumerate(['b_conv', 'b_rg', 'b_ig', 'lru_lambda']): lruch[:, :, 4 + j_] = pm(np.asarray(inp[key])[0])
    past_len = inp['page_table'].shape[1] * inp['cache_latent'].shape[2]
    pos_s = past_len + np.repeat(np.arange(4), 16)
    in_maps = []
    for c in range(8):
        b, half = c // 2, c % 2
        own = slice(half * 1024, half * 1024 + 1024)
        ss = slice(16 * c, 16 * c + 16)
        tm = lambda a: np.ascontiguousarray(np.swapaxes(a, 0, 1).reshape((64,) + a.shape[2:]))
        pos = np.concatenate([np.arange(0, 1024), np.arange(half * 1024, half * 1024 + 1024), pos_s])
        rc, rs = _rope_tables(pos)
        m = dict(shared)
        m.update(
            xo=f(x_prompt[b, own]), xp=f(x_prompt[b, 0:1024]) if half else np.zeros((1024, D), np.float32),
            xs=f(tm(x_sample[ss])), po=f(inp['p_prompt'][0, b, own]), psm=f(tm(np.asarray(inp['p_sample'])[0, ss])),
            ptT=np.ascontiguousarray(np.asarray(inp['page_table'])[ss].T.astype(np.int32)),
            slT=f(np.asarray(inp['state_lru'])[0, ss].T), scT=f(np.asarray(inp['state_conv'])[0, ss].transpose(2, 1, 0).reshape(1024, 48)),
            gvh=gvh, lruch=lruch,
            flags=np.tile(np.array([[1.0 if half else 0.0, 0.0 if half else NEG]], np.float32), (128, 1)),
            ropeC=rc, ropeS=rs,
        )
        in_maps.append(m)
    return in_maps


def assemble(res):
    B, S, DB, T = 4, 2048, 128, 4
    y_p = np.zeros((B, S, D), np.float32); y_s = np.zeros((DB, T, D), np.float32)
    nl_p = np.zeros((1, B, S, 256), np.float32); nk_p = np.zeros((1, B, S, 64), np.float32)
    nlru_p = np.zeros((1, B, 1024), np.float32); nconv_p = np.zeros((1, B, 3, 1024), np.float32)
    nl_s = np.zeros((1, DB, T, 256), np.float32); nk_s = np.zeros((1, DB, T, 64), np.float32)
    nlru_s = np.zeros((1, DB, 1024), np.float32); nconv_s = np.zeros((1, DB, 3, 1024), np.float32)
    utm = lambda a: np.swapaxes(a.reshape((4, 16) + a.shape[1:]), 0, 1)
    for c in range(8):
        b, half = c // 2, c % 2
        own = slice(half * 1024, half * 1024 + 1024); ss = slice(16 * c, 16 * c + 16)
        r = res[c]
        y_p[b, own] = r['y_o']; y_s[ss] = utm(r['y_s'])
        nl_p[0, b, own] = r['lat_o']; nk_p[0, b, own] = r['kr_o']
        if half:
            nlru_p[0, b] = r['lru_o'].T.reshape(1024); nconv_p[0, b] = r['conv_o'].reshape(128, 8, 3).transpose(2, 1, 0).reshape(3, 1024)
        nl_s[0, ss] = utm(r['lat_s']); nk_s[0, ss] = utm(r['kr_s'])
        nlru_s[0, ss] = r['lru_s'].T
        nconv_s[0, ss] = r['conv_s'].reshape(1024, 3, 16).transpose(2, 1, 0)
    return (y_p, y_s, nl_p, nk_p, nlru_p, nconv_p, nl_s, nk_s, nlru_s, nconv_s)


def kernel(**inp):
    nc = build_nc(int(np.asarray(inp['cache_latent']).shape[1]))
    in_maps = make_in_maps(inp)
    names = set(DECLARED)
    in_maps = [{k: v for k, v in m.items() if k in names} for m in in_maps]
    res = run_bass_kernel_spmd(nc, in_maps, core_ids=list(range(8))).results
    return assemble(res)
```

```python
import numpy as np
from contextlib import ExitStack
import concourse.bass as bass
import concourse.mybir as mybir
from concourse.bass_utils import run_bass_kernel_spmd

F32 = mybir.dt.float32
BF16 = mybir.dt.bfloat16
I32 = mybir.dt.int32
AF = mybir.ActivationFunctionType
ALU = mybir.AluOpType
AX = mybir.AxisListType

import os
DBG = int(os.environ.get('KDBG', '0'))
STAGE = int(os.environ.get('KSTAGE', '3'))

D = 2048; NO = 1024; NPF = 1024; NS = 64; NT = NO + NS
NKEY = 2048 + NS
EPS = 1e-6
NEG = -1e30
SM_SCALE = 192.0 ** -0.5
ENG = ['pe', 'act', 'dve', 'pool', 'sp']
DECLARED = []


class Prog:
    def __init__(self, nc, es):
        self.nc = nc; self.es = es
        self.sem = {e: es.enter_context(nc.semaphore('sem_' + e)) for e in ENG}
        self.lanes = {}
        self.seq = {e: 0 for e in ENG}
        self.lcnt = {}
        self.waited = {e: {} for e in ENG}
        self.reset()
        self.barrier_vals = None

    def reset(self):
        self.ops = []; self.last_w = {}; self.readers = {}

    def lane(self, name):
        if name not in self.lanes:
            self.lanes[name] = self.es.enter_context(self.nc.semaphore('ln_' + name))
            self.lcnt[name] = 0
        return self.lanes[name]

    def op(self, eng, fn, r=(), w=(), lane=None):
        idx = len(self.ops)
        w = list(w) + [x for x in r if x.startswith('ps') and x[2:].isdigit() and x not in w]
        deps = set()
        for x in r:
            if x in self.last_w: deps.add(self.last_w[x])
        for x in w:
            if x in self.last_w: deps.add(self.last_w[x])
            deps.update(self.readers.get(x, ()))
        for x in w:
            self.last_w[x] = idx; self.readers[x] = []
        for x in r:
            self.readers.setdefault(x, []).append(idx)
        dv = {}
        for d in deps:
            p = self.ops[d]
            dv[d] = self.lcnt[p['lane']] if p['lane'] is not None else p['val']
        o = dict(eng=eng, fn=fn, deps=dv, lane=lane)
        if lane is None:
            self.seq[eng] += 1; o['sem'] = self.sem[eng]; o['val'] = self.seq[eng]; o['inc'] = 1
        else:
            s = self.lane(lane); self.lcnt[lane] += 16
            o['sem'] = s; o['val'] = self.lcnt[lane]; o['inc'] = 16
        self.ops.append(o)

    def emit(self, final=False):
        nc = self.nc
        bar = self.barrier_vals
        ops = self.ops
        with nc.Block() as block:
            decos = dict(pe=block.tensor, act=block.scalar, dve=block.vector, pool=block.gpsimd, sp=block.sync)
            for e in ENG:
                def body(eh, e=e):
                    wd = self.waited[e]
                    def wait(sem, val):
                        if wd.get(id(sem), 0) >= val: return
                        eh.wait_ge(sem, val); wd[id(sem)] = val
                    if bar is not None:
                        for sem, val in bar:
                            if val > 0: wait(sem, val)
                    for o in ops:
                        if o['eng'] != e: continue
                        for d in sorted(o['deps']):
                            p = ops[d]
                            if p['eng'] == 'pe' and e == 'pe' and p['lane'] is None: continue
                            wait(p['sem'], o['deps'][d])
                        ins = o['fn'](eh)
                        ins.then_inc(o['sem'], o['inc'])
                    if final and e == 'sp':
                        for en in ENG: wait(self.sem[en], self.seq[en])
                        for ln, s in self.lanes.items(): wait(s, self.lcnt[ln])
                decos[e](body)
        self.barrier_vals = [(self.sem[en], self.seq[en]) for en in ENG] + \
                            [(s, self.lcnt[ln]) for ln, s in self.lanes.items()]
        self.reset()


def build_nc(npool=20480):
    nc = bass.Bass("TRN2", target_bir_lowering=False)
    es = ExitStack()
    DECLARED.clear()
    def din(name, shape, dt=F32, stage=1):
        if STAGE < stage: return None
        DECLARED.append(name)
        return nc.dram_tensor(name, list(shape), dt, kind="ExternalInput").ap()
    def dout(name, shape, dt=F32): return nc.dram_tensor(name, list(shape), dt, kind="ExternalOutput").ap()
    xo = din('xo', [NO, D]); xp = din('xp', [NPF, D]); xs = din('xs', [NS, D])
    po = din('po', [NO, 256], stage=3); psm = din('psm', [NS, 256], stage=3)
    cl = din('cl', [npool, 128, 256], stage=2); ck = din('ck', [npool, 128, 64], stage=2); ptT = din('ptT', [128, 16], I32, stage=2)
    slT = din('slT', [1024, 16]); scT = din('scT', [1024, 48])
    flags = din('flags', [128, 2])
    gvh = din('gvh', [128, 104]); lruch = din('lruch', [128, 8, 8])
    ropeC = din('ropeC', [64, NKEY]); ropeS = din('ropeS', [64, NKEY])
    tri = din('tri', [128, 128], stage=2); smask = din('smask', [32, 4], stage=2)
    g_mix = din('g_mix', [D], stage=99); w_in = din('w_in', [D, 2880]); wkrp = din('wkrp', [D, 64])
    g_q = din('g_q', [512], stage=99); w_uq = din('w_uq', [512, 1536], stage=2); wqrp = din('wqrp', [512, 8, 64], stage=2)
    g_kv = din('g_kv', [256], stage=99); wukT = din('wukT', [8, 128, 256], stage=2); wuv = din('wuv', [8, 256, 128], stage=2)
    w_conv = din('w_conv', [4, 1024], stage=99); b_conv = din('b_conv', [1024], stage=99)
    w_rg = din('w_rg', [8, 128, 128]); b_rg = din('b_rg', [1024], stage=99); w_ig = din('w_ig', [8, 128, 128]); b_ig = din('b_ig', [1024], stage=99)
    lam = din('lam', [1024], stage=99); g_om = din('g_om', [1024], stage=99); g_ol = din('g_ol', [1024], stage=99)
    w_o = din('w_o', [D, D], stage=3); g_ffn = din('g_ffn', [D], stage=99)
    w_group = din('w_group', [D, 4], stage=3); b_group = din('b_group', [4], stage=3); w_router = din('w_router', [D, 32], stage=3); b_router = din('b_router', [32], stage=3)
    w_gate = din('w_gate', [32, D, 512], stage=3); w_up = din('w_up', [32, D, 512], stage=3); w_down = din('w_down', [32, 512, D], stage=3)
    g_ple = din('g_ple', [D], stage=99); w_pg = din('w_pg', [D, D], stage=3); b_pg = din('b_pg', [D], stage=3); w_pp = din('w_pp', [256, D], stage=3)
    g_fin = din('g_fin', [D], stage=99)
    y_o = dout('y_o', [NO, D]); y_s = dout('y_s', [NS, D])
    lat_o = dout('lat_o', [NO, 256]); kr_o = dout('kr_o', [NO, 64])
    lru_o = dout('lru_o', [128, 8]); conv_o = dout('conv_o', [128, 24])
    lat_s = dout('lat_s', [NS, 256]); kr_s = dout('kr_s', [NS, 64])
    lru_s = dout('lru_s', [1024, 16]); conv_s = dout('conv_s', [1024, 48])

    P = Prog(nc, es)
    def sb(name, shape, dt=F32, st=None): return (st or es).enter_context(nc.sbuf_tensor(name, list(shape), dt))
    hT = sb('hT', [128, 16, NT])
    mixT = sb('mixT', [128, 16, NT], BF16)
    identf = sb('identf', [128, 128]); identb = sb('identb', [128, 128], BF16)
    onesb = sb('onesb', [128, 128], BF16)
    epsT = sb('epsT', [128, 1]); oneT = sb('oneT', [128, 1])
    gv = sb('gv', [128, 88])
    GV_MIX, GV_Q, GV_KV, GV_OM, GV_OL, GV_FFN, GV_PLE, GV_BPG = 0, 16, 20, 22, 30, 38, 54, 70
    gv2 = sb('gv2', [128, 16])
    lruc = sb('lruc', [128, 8, 12])
    flg = sb('flg', [128, 2])
    hist = sb('hist', [128, 8, 3]); state = sb('state', [128, 8, 1])
    es_mid = ExitStack()
    cqT = sb('cqT', [128, 4, NT], BF16, es_mid)
    latT = sb('latT', [128, 2, NKEY], BF16, es_mid)
    krT = sb('krT', [64, NKEY], BF16, es_mid)
    lat_tok = sb('lat_tok', [128, 17, 260], BF16, es_mid)
    psall = es.enter_context(nc.psum_tensor('psall', [128, 8, 512], F32))
    ps = [psall[:, i, :] for i in range(8)]

    def dma(eng, lane, out, in_, r=(), w=()):
        P.op(eng, lambda e: e.dma_start(out=out, in_=in_), r=r, w=w, lane=lane)

    P.op('pool', lambda e: e.memset(identf[:], 0.0), w=['identf'])
    def c_ident(e):
        return e.affine_select(out=identf[:], in_=identf[:], pattern=[[-1, 128]], compare_op=ALU.not_equal,
                               fill=1.0, base=0, channel_multiplier=1)
    P.op('pool', c_ident, r=['identf'], w=['identf'])
    P.op('pool', lambda e: e.tensor_copy(out=identb[:], in_=identf[:]), r=['identf'], w=['identb'])
    P.op('pool', lambda e: e.memset(onesb[:], 1.0), w=['onesb'])
    P.op('pool', lambda e: e.memset(epsT[:], EPS), w=['epsT'])
    P.op('pool', lambda e: e.memset(oneT[:], 1.0), w=['oneT'])
    P.op('pool', lambda e: e.memset(lat_tok[:, :, 256:257], 1.0), w=['lat_tok_ones'])
    P.op('pool', lambda e: e.memset(hist[:], 0.0), w=['hist'])
    P.op('pool', lambda e: e.memset(state[:], 0.0), w=['state'])
    small = nc.allow_non_contiguous_dma(reason="small strided constant loads")
    es.enter_context(small)
    dma('sp', 'c0', gv[:, 0:70], gvh[:, 0:70], w=['gv'])
    dma('sp', 'c0', gv2[:, 0:16], gvh[:, 70:86], w=['gv'])
    dma('sp', 'c0', gv[:, 70:86], gvh[:, 86:102], w=['gv'])
    dma('sp', 'c0b', lruc[:, :, 0:8], lruch, w=['lruc'])
    dma('sp', 'c0', flg[:], flags, w=['flg'])
    P.op('act', lambda e: e.activation(out=lruc[:, :, 7:8], in_=lruc[:, :, 7:8], func=AF.Exp, scale=-1.0), r=['lruc'], w=['lruc'])
    P.op('act', lambda e: e.activation(out=lruc[:, :, 7:8], in_=lruc[:, :, 7:8], func=AF.Ln, bias=oneT[:], scale=1.0), r=['lruc', 'oneT'], w=['lruc'])
    P.op('dve', lambda e: e.tensor_scalar(out=lruc[:, :, 8:9], in0=lruc[:, :, 7:8], scalar1=-16.0, scalar2=None, op0=ALU.mult), r=['lruc'], w=['lruc'])
    P.op('dve', lambda e: e.tensor_scalar(out=lruc[:, :, 7:8], in0=lruc[:, :, 7:8], scalar1=-8.0, scalar2=None, op0=ALU.mult), r=['lruc'], w=['lruc'])
    P.emit(final=(DBG == 1))
    if DBG == 1:
        es.close(); return nc

    with ExitStack() as st1:
        xst = sb('xst', [128, D], F32, st1)
        xnb = sb('xnb', [128, D], BF16, st1)
        ssq = sb('ssq', [128, 2], F32, st1)
        xnT = sb('xnT', [128, 16, 256], BF16, st1)
        wst = sb('wst', [128, 4, 16, 128], BF16, st1)
        ropeT = sb('ropeT', [64, 2, 256], F32, st1)
        wg = sb('wg', [128, 2, 8, 128], BF16, st1)
        ckv_f = sb('ckv_f', [128, 2, 256], F32, st1)
        sqb = sb('sqb', [128, 256], BF16, st1)
        rstd_b = sb('rstd_b', [128, 256], F32, st1)
        kr_f = sb('kr_f', [64, 2, 256], F32, st1)
        xbes = [sb('xbe%d' % i, [128, 304], F32, st1) for i in range(2)]
        T = [[sb('lt%d_%d' % (j_, i), [128, 256], F32, st1) for i in range(5)] for j_ in range(2)]
        xcbs = [sb('xcb%d' % i, [128, 256], BF16, st1) for i in range(2)]
        stg = sb('stg', [128, 2, 320], F32, st1)
        shist = sb('shist', [128, 8, 48], F32, st1); sstate = sb('sstate', [128, 8, 16], F32, st1)

        dma('pool', 'c1p', wg[:, 0], w_rg.rearrange("n k j -> k n j"), w=['wg'])
        dma('pool', 'c1p', wg[:, 1], w_ig.rearrange("n k j -> k n j"), w=['wg'])
        dma('sp', 'shist', shist[:], scT.rearrange("(c p) k -> p c k", p=128), w=['shist'])
        dma('sp', 'sstate', sstate[:], slT.rearrange("(c p) k -> p c k", p=128), w=['sstate'])

        tiles = [('P', c_, 256) for c_ in range(0, 1024, 256)] + [('O', c_, 256) for c_ in range(0, 1024, 256)] + [('S', 0, 64)]
        wcnt = [0]; scnt = [0]; stgc = [0]
        def do_tile(kind, c0, n):
            src = dict(P=xp, O=xo, S=xs)[kind]
            pcol = dict(P=c0, O=NPF + c0, S=2048)[kind]
            hcol = dict(P=None, O=c0, S=NO)[kind]
            full = kind != 'P'
            nb = (n + 127) // 128
            for i in range(nb):
                rows = min(128, n - i * 128)
                dma('sp', 'xst', xst[0:rows, :], src[c0 + i * 128:c0 + i * 128 + rows, :], w=['xst'])
                P.op('act', lambda e, rows=rows: e.activation(out=xnb[0:rows, :], in_=xst[0:rows, :], func=AF.Square, accum_out=ssq[0:rows, 0:1]),
                     r=['xst'], w=['xnb', 'ssq'])
                P.op('act', lambda e, rows=rows: e.activation(out=ssq[0:rows, 1:2], in_=ssq[0:rows, 0:1], func=AF.Sqrt, bias=epsT[0:rows, :], scale=1.0 / D),
                     r=['ssq', 'epsT'], w=['ssq'])
                P.op('dve', lambda e, rows=rows: e.reciprocal(out=ssq[0:rows, 1:2], in_=ssq[0:rows, 1:2]), r=['ssq'], w=['ssq'])
                P.op('act', lambda e, rows=rows: e.activation(out=xnb[0:rows, :], in_=xst[0:rows, :], func=AF.Copy, scale=ssq[0:rows, 1:2]),
                     r=['xst', 'ssq'], w=['xnb'])
                for hb in range(2):
                    bank = ps[hb]
                    pv = bank[:].bitcast(BF16)
                    def tr(e, hb=hb, rows=rows, pv=pv):
                        ins = None
                        for k in range(8):
                            ins = e.transpose(pv[:, k * 128:k * 128 + rows], xnb[0:rows, (hb * 8 + k) * 128:(hb * 8 + k + 1) * 128], identb[0:rows, 0:rows])
                        return ins
                    P.op('pe', tr, r=['xnb', 'identb'], w=['ps%d' % hb])
                    def ev(e, hb=hb, rows=rows, pv=pv, i=i):
                        return e.tensor_tensor(out=xnT[:, hb * 8:hb * 8 + 8, i * 128:i * 128 + rows],
                                               in0=pv[:, 0:1024].rearrange("p (k t) -> p k t", k=8)[:, :, 0:rows],
                                               in1=gv[:, GV_MIX + hb * 8:GV_MIX + hb * 8 + 8].unsqueeze(2).to_broadcast([128, 8, rows]), op=ALU.mult)
                    P.op('dve', ev, r=['ps%d' % hb, 'gv'], w=['xnT'])
                if full:
                    for q4 in range(4):
                        bank = ps[2 + (q4 % 2)]
                        def trf(e, q4=q4, rows=rows, bank=bank):
                            ins = None
                            for k in range(4):
                                ch = q4 * 4 + k
                                ins = e.transpose(bank[:, k * 128:k * 128 + rows], xst[0:rows, ch * 128:(ch + 1) * 128], identf[0:rows, 0:rows])
                            return ins
                        P.op('pe', trf, r=['xst', 'identf'], w=['ps%d' % (2 + q4 % 2)])
                        def evf(e, q4=q4, rows=rows, bank=bank, i=i):
                            return e.activation(out=hT[:, q4 * 4:q4 * 4 + 4, hcol + i * 128:hcol + i * 128 + rows],
                                                in_=bank[:, 0:512].rearrange("p (k t) -> p k t", k=4)[:, :, 0:rows], func=AF.Copy)
                        P.op('act', evf, r=['ps%d' % (2 + q4 % 2)], w=['hT'])

            if DBG == 2: return
            def proj(col0, ncol, dst_bank, wsrc=None):
                b = wcnt[0] % 4; wcnt[0] += 1
                wt = wst[:, b, :, 0:ncol]; lane = 'wst%d' % b; res = 'wst%d' % b
                srcw = (wsrc if wsrc is not None else w_in[:, col0:col0 + ncol]).rearrange("(k p) c -> p k c", p=128)
                dma('pool', lane, wt, srcw, w=[res])
                def mm(e, wt=wt, ncol=ncol, dst_bank=dst_bank):
                    ins = None
                    for k in range(16):
                        ins = e.matmul(ps[dst_bank][0:ncol, 0:n], lhsT=wt[:, k, :], rhs=xnT[:, k, 0:n], start=(k == 0), stop=(k == 15))
                    return ins
                P.op('pe', mm, r=[res, 'xnT'], w=['ps%d' % dst_bank])

            def fm_norm(srcs, nfeat, gcol, outs, tag):
                tags = list(tag) if isinstance(tag, (list, tuple)) else [tag]
                for j, s_ in enumerate(srcs):
                    P.op('act', lambda e, s_=s_: e.activation(out=sqb[:, 0:n], in_=s_, func=AF.Square), r=tags, w=['sqb'])
                    P.op('pe', lambda e, j=j: e.matmul(ps[7][:, 0:n], lhsT=onesb[:], rhs=sqb[:, 0:n], start=(j == 0), stop=(j == len(srcs) - 1)),
                         r=['sqb', 'onesb'], w=['ps7'])
                P.op('act', lambda e: e.activation(out=rstd_b[:, 0:n], in_=ps[7][:, 0:n], func=AF.Sqrt, bias=epsT[:], scale=1.0 / nfeat),
                     r=['ps7', 'epsT'], w=['rstd_b'])
                P.op('dve', lambda e: e.reciprocal(out=rstd_b[:, 0:n], in_=rstd_b[:, 0:n]), r=['rstd_b'], w=['rstd_b'])
                for j, s_ in enumerate(srcs):
                    for (o_, ores) in outs[j]:
                        P.op('dve', lambda e, s_=s_, o_=o_, j=j: e.scalar_tensor_tensor(out=o_, in0=s_, scalar=gv[:, gcol + j:gcol + j + 1], in1=rstd_b[:, 0:n],
                                                                                       op0=ALU.mult, op1=ALU.mult), r=tags + ['gv', 'rstd_b'], w=[ores])

            if full:
                for j in range(4):
                    proj(j * 128, 128, 4 + j % 2)
                    P.op('act', lambda e, j=j: e.activation(out=T[0][j][:, 0:n], in_=ps[4 + j % 2][:, 0:n], func=AF.Copy), r=['ps%d' % (4 + j % 2)], w=['cqf', 'xc0', 'rr0', 'ii0', 'aa0'])
                fm_norm([T[0][j][:, 0:n] for j in range(4)], 512, GV_Q, [[(cqT[:, j, hcol:hcol + n], 'cqT')] for j in range(4)], ['cqf', 'xc0', 'rr0', 'ii0', 'aa0'])
            if DBG == 3: return
            for j in range(2):
                proj(512 + j * 128, 128, 4 + j % 2)
                P.op('act', lambda e, j=j: e.activation(out=ckv_f[:, j, 0:n], in_=ps[4 + j % 2][:, 0:n], func=AF.Copy), r=['ps%d' % (4 + j % 2)], w=['ckv'])
            fm_norm([ckv_f[:, j, 0:n] for j in range(2)], 256, GV_KV, [[(ckv_f[:, j, 0:n], 'ckv')] for j in range(2)], 'ckv')
            for j in range(2):
                P.op('dve', lambda e, j=j: e.tensor_copy(out=latT[:, j, pcol:pcol + n], in_=ckv_f[:, j, 0:n]), r=['ckv'], w=['latT'])
            if DBG == 4: return
            dma('sp', 'ropeT', ropeT[:, 0, 0:n], ropeC[:, pcol:pcol + n], w=['rope'])
            dma('sp', 'ropeT', ropeT[:, 1, 0:n], ropeS[:, pcol:pcol + n], w=['rope'])
            proj(768, 64, 4)
            P.op('act', lambda e: e.activation(out=kr_f[:, 0, 0:n], in_=ps[4][0:64, 0:n], func=AF.Copy), r=['ps4'], w=['krf0'])
            proj(0, 64, 5, wsrc=wkrp)
            P.op('dve', lambda e: e.tensor_tensor(out=kr_f[:, 1, 0:n], in0=ps[5][0:64, 0:n], in1=ropeT[:, 1, 0:n], op=ALU.mult), r=['ps5', 'rope'], w=['krf1'])
            P.op('dve', lambda e: e.tensor_tensor(out=kr_f[:, 0, 0:n], in0=kr_f[:, 0, 0:n], in1=ropeT[:, 0, 0:n], op=ALU.mult), r=['krf0', 'rope'], w=['krf0'])
            P.op('dve', lambda e: e.tensor_tensor(out=kr_f[:, 0, 0:n], in0=kr_f[:, 0, 0:n], in1=kr_f[:, 1, 0:n], op=ALU.add), r=['krf0', 'krf1'], w=['krf0'])
            P.op('dve', lambda e: e.tensor_copy(out=krT[:, pcol:pcol + n], in_=kr_f[:, 0, 0:n]), r=['krf0'], w=['krT'])
            if DBG == 5: return
            for i in range(nb):
                rows = min(128, n - i * 128)
                kb = (pcol + i * 128) // 128
                def trl(e, i=i, rows=rows):
                    e.transpose(ps[6][0:rows, 0:128], ckv_f[:, 0, i * 128:i * 128 + rows], identf[:])
                    e.transpose(ps[6][0:rows, 128:256], ckv_f[:, 1, i * 128:i * 128 + rows], identf[:])
                    return e.transpose(ps[6][0:rows, 256:320], kr_f[:, 0, i * 128:i * 128 + rows], identf[0:64, 0:64])
                P.op('pe', trl, r=['ckv', 'krf0', 'identf'], w=['ps6'])
                P.op('dve', lambda e, rows=rows, kb=kb: e.tensor_copy(out=lat_tok[0:rows, kb, 0:256], in_=ps[6][0:rows, 0:256]), r=['ps6'], w=['lat_tok'])
                if full and DBG != 7:
                    sb_ = stgc[0] % 2; stgc[0] += 1
                    P.op('act', lambda e, rows=rows, sb_=sb_: e.activation(out=stg[0:rows, sb_, :], in_=ps[6][0:rows, 0:320], func=AF.Copy), r=['ps6'], w=['stg%d' % sb_])
                    lo, ko = (lat_o, kr_o) if kind == 'O' else (lat_s, kr_s)
                    r0 = c0 + i * 128
                    if DBG != 8:
                        dma('sp', 'stg%d' % sb_, lo[r0:r0 + rows, :], stg[0:rows, sb_, 0:256], r=['stg%d' % sb_])
                    if DBG not in (8, 9):
                        dma('sp', 'stg%d' % sb_, ko[r0:r0 + rows, :], stg[0:rows, sb_, 256:320], r=['stg%d' % sb_])

            if DBG in (6, 7, 8, 9): return
            H = 48 if kind == 'S' else 3
            sh = 16 if kind == 'S' else 1
            if kind == 'O' and c0 == 0:
                P.op('dve', lambda e: e.tensor_scalar(out=hist[:], in0=hist[:], scalar1=flg[:, 0:1], scalar2=None, op0=ALU.mult), r=['hist', 'flg'], w=['hist'])
                P.op('dve', lambda e: e.tensor_scalar(out=state[:], in0=state[:], scalar1=flg[:, 0:1], scalar2=None, op0=ALU.mult), r=['state', 'flg'], w=['state'])
            def do_chunk(c):
                pp = c % 2
                xc, rr, ii, aa, uu = T[pp]; yy = aa; xbe = xbes[pp]; xcb = xcbs[pp]
                proj(832 + c * 128, 128, 4)
                hsrc = shist[:, c, :] if kind == 'S' else hist[:, c, :]
                P.op('dve', lambda e, hsrc=hsrc: e.tensor_copy(out=xbe[:, 0:H], in_=hsrc), r=['hist', 'shist'], w=['xbe%d' % pp])
                P.op('act', lambda e: e.activation(out=xbe[:, H:H + n], in_=ps[4][:, 0:n], func=AF.Copy), r=['ps4'], w=['xbe%d' % pp])
                P.op('dve', lambda e, c=c: e.tensor_scalar(out=xc[:, 0:n], in0=xbe[:, 0:n], scalar1=lruc[:, c, 0:1], scalar2=lruc[:, c, 4:5], op0=ALU.mult, op1=ALU.add),
                     r=['xbe%d' % pp, 'lruc'], w=['xc%d' % pp])
                for k in range(1, 4):
                    P.op('dve', lambda e, c=c, k=k: e.scalar_tensor_tensor(out=xc[:, 0:n], in0=xbe[:, k * sh:k * sh + n], scalar=lruc[:, c, k:k + 1], in1=xc[:, 0:n],
                                                                         op0=ALU.mult, op1=ALU.add), r=['xbe%d' % pp, 'lruc', 'xc%d' % pp], w=['xc%d' % pp])
                if kind != 'S':
                    P.op('dve', lambda e, c=c: e.tensor_copy(out=hist[:, c, :], in_=xbe[:, n:n + 3]), r=['xbe%d' % pp], w=['hist'])
                else:
                    P.op('dve', lambda e, c=c: e.tensor_copy(out=shist[:, c, :], in_=xbe[:, 64:112]), r=['xbe%d' % pp], w=['shist'])
                P.op('dve', lambda e: e.tensor_copy(out=xcb[:, 0:n], in_=xc[:, 0:n]), r=['xc%d' % pp], w=['xcb%d' % pp])
                P.op('pe', lambda e, c=c: e.matmul(ps[5][:, 0:n], lhsT=wg[:, 0, c, :], rhs=xcb[:, 0:n], start=True, stop=True), r=['wg', 'xcb%d' % pp], w=['ps5'])
                P.op('pe', lambda e, c=c: e.matmul(ps[6][:, 0:n], lhsT=wg[:, 1, c, :], rhs=xcb[:, 0:n], start=True, stop=True), r=['wg', 'xcb%d' % pp], w=['ps6'])
                P.op('act', lambda e, c=c: e.activation(out=rr[:, 0:n], in_=ps[5][:, 0:n], func=AF.Sigmoid, bias=lruc[:, c, 5:6], scale=1.0), r=['ps5', 'lruc'], w=['rr%d' % pp])
                P.op('act', lambda e, c=c: e.activation(out=ii[:, 0:n], in_=ps[6][:, 0:n], func=AF.Sigmoid, bias=lruc[:, c, 6:7], scale=1.0), r=['ps6', 'lruc'], w=['ii%d' % pp])
                P.op('act', lambda e, c=c: e.activation(out=aa[:, 0:n], in_=rr[:, 0:n], func=AF.Exp, scale=lruc[:, c, 7:8]), r=['rr%d' % pp, 'lruc'], w=['aa%d' % pp])
                P.op('act', lambda e, c=c: e.activation(out=rr[:, 0:n], in_=rr[:, 0:n], func=AF.Exp, scale=lruc[:, c, 8:9]), r=['rr%d' % pp, 'lruc'], w=['rr%d' % pp])
                P.op('act', lambda e: e.activation(out=rr[:, 0:n], in_=rr[:, 0:n], func=AF.Sqrt, bias=oneT[:], scale=-1.0), r=['rr%d' % pp, 'oneT'], w=['rr%d' % pp])
                P.op('dve', lambda e: e.tensor_tensor(out=uu[:, 0:n], in0=ii[:, 0:n], in1=xc[:, 0:n], op=ALU.mult), r=['ii%d' % pp, 'xc%d' % pp], w=['uu%d' % pp])
                P.op('dve', lambda e: e.tensor_tensor(out=uu[:, 0:n], in0=uu[:, 0:n], in1=rr[:, 0:n], op=ALU.mult), r=['uu%d' % pp, 'rr%d' % pp], w=['uu%d' % pp])
                if kind != 'S':
                    P.op('dve', lambda e, c=c: e.tensor_tensor_scan(out=uu[:, 0:n], data0=aa[:, 0:n], data1=uu[:, 0:n], initial=state[:, c, :], op0=ALU.mult, op1=ALU.add),
                         r=['aa%d' % pp, 'uu%d' % pp, 'state'], w=['uu%d' % pp])
                    P.op('dve', lambda e, c=c: e.tensor_copy(out=state[:, c, :], in_=uu[:, n - 1:n]), r=['uu%d' % pp], w=['state'])
                else:
                    for t in range(4):
                        prev = sstate[:, c, :] if t == 0 else uu[:, (t - 1) * 16:t * 16]
                        P.op('dve', lambda e, t=t, prev=prev: e.tensor_tensor(out=aa[:, t * 16:t * 16 + 16], in0=aa[:, t * 16:t * 16 + 16], in1=prev, op=ALU.mult),
                             r=['aa%d' % pp, 'uu%d' % pp, 'sstate'], w=['aa%d' % pp])
                        P.op('dve', lambda e, t=t: e.tensor_tensor(out=uu[:, t * 16:t * 16 + 16], in0=uu[:, t * 16:t * 16 + 16], in1=aa[:, t * 16:t * 16 + 16], op=ALU.add),
                             r=['aa%d' % pp, 'uu%d' % pp], w=['uu%d' % pp])
                    P.op('dve', lambda e, c=c: e.tensor_copy(out=sstate[:, c, :], in_=uu[:, 48:64]), r=['uu%d' % pp], w=['sstate'])
                if full:
                    proj(1856 + c * 128, 128, 7)
                    P.op('act', lambda e: e.activation(out=yy[:, 0:n], in_=ps[7][:, 0:n], func=AF.Copy), r=['ps7'], w=['aa%d' % pp])
                    P.op('act', lambda e: e.activation(out=ii[:, 0:n], in_=ps[7][:, 0:n], func=AF.Square), r=['ps7'], w=['ii%d' % pp])
                    P.op('dve', lambda e: e.tensor_scalar(out=ii[:, 0:n], in0=ii[:, 0:n], scalar1=0.044715, scalar2=1.0, op0=ALU.mult, op1=ALU.add), r=['ii%d' % pp], w=['ii%d' % pp])
                    P.op('dve', lambda e: e.tensor_tensor(out=ii[:, 0:n], in0=ii[:, 0:n], in1=yy[:, 0:n], op=ALU.mult), r=['ii%d' % pp, 'aa%d' % pp], w=['ii%d' % pp])
                    P.op('act', lambda e: e.activation(out=ii[:, 0:n], in_=ii[:, 0:n], func=AF.Sigmoid, scale=1.5957691216057308), r=['ii%d' % pp], w=['ii%d' % pp])
                    P.op('dve', lambda e: e.tensor_tensor(out=ii[:, 0:n], in0=ii[:, 0:n], in1=yy[:, 0:n], op=ALU.mult), r=['ii%d' % pp, 'aa%d' % pp], w=['ii%d' % pp])
                    P.op('dve', lambda e, c=c: e.tensor_tensor(out=mixT[:, 8 + c, hcol:hcol + n], in0=ii[:, 0:n], in1=uu[:, 0:n], op=ALU.mult), r=['ii%d' % pp, 'uu%d' % pp], w=['mixT'])
            for c_ in range(8):
                do_chunk(c_)
        for (kind_, c0_, n_) in tiles:
            do_tile(kind_, c0_, n_)
        dma('sp', 'o1', lru_o, state[:].rearrange("p c o -> p (c o)"), r=['state'])
        dma('sp', 'o2', conv_o, hist[:].rearrange("p c k -> p (c k)"), r=['hist'])
        dma('sp', 'o3', lru_s.rearrange("(c p) k -> p c k", p=128), sstate[:], r=['sstate'])
        dma('sp', 'o4', conv_s.rearrange("(c p) k -> p c k", p=128), shist[:], r=['shist'])
        P.emit(final=(STAGE == 1))
    if STAGE == 1:
        es_mid.close(); es.close(); return nc

    Sv = psall[:, 0:4, :].rearrange("p b c -> p (b c)")
    with ExitStack() as st2:
        wuvs = sb('wuvs', [128, 8, 2, 128], BF16, st2); smk = sb('smk', [32, 4], F32, st2)
        pts = sb('pts', [128, 16], I32, st2)
        qlS = sb('qlS', [128, 8, 2, 64], BF16, st2); qrSs = sb('qrSs', [64, 8, 64], BF16, st2)
        OTS = sb('OTS', [128, 2, 16, 32], BF16, st2)
        st2a = ExitStack()
        wqn = sb('wqn', [128, 4, 8, 128], BF16, st2a); wqr = sb('wqr', [128, 4, 8, 64], BF16, st2a); wqp = sb('wqp', [128, 4, 8, 64], BF16, st2a)
        wuk = sb('wuk', [128, 8, 256], BF16, st2a)
        trit = sb('trit', [128, 128], F32, st2a)
        qn = sb('qn', [128, 8, 128], BF16, st2a); qlT = sb('qlT', [128, 8, 2, 128], BF16, st2a); qrT = sb('qrT', [64, 8, 128], BF16, st2a)
        rtab = sb('rtab', [64, 2, 128], F32, st2a); qa = sb('qa', [64, 2, 4, 128], F32, st2a)
        Pm = sb('Pm', [128, 2048], BF16, st2a); PT = sb('PT', [128, 16, 128], BF16, st2a)
        stt = sb('stt', [128, 8], F32, st2a)
        On = sb('On', [128, 256], BF16, st2a); OT = sb('OT', [128, 8, 2, 128], BF16, st2a)
        w_uq3 = w_uq.rearrange("(k p) (h j) -> p k h j", p=128, h=8)
        wqrp3 = wqrp.rearrange("(k p) h j -> p k h j", p=128)
        for k in range(4):
            dma('pool', 'wq', wqn[:, k], w_uq3[:, k, :, 0:128], w=['wq'])
            dma('pool', 'wq', wqr[:, k], w_uq3[:, k, :, 128:192], w=['wq'])
            dma('pool', 'wq', wqp[:, k], wqrp3[:, k], w=['wq'])
        dma('pool', 'wq', wuk[:], wukT.rearrange("h n c -> n h c"), w=['wq'])
        for h in range(8):
            dma('pool', 'wuvs', wuvs[:, h], wuv[h].rearrange("(cc p) v -> p cc v", p=128), w=['wuvs'])
        dma('sp', 'trit', trit[:], tri, w=['trit']); dma('sp', 'smk', smk[:], smask, w=['smk'])

        def qproj(hc, nq, pcol):
            dma('sp', 'rtab', rtab[:, 0, 0:nq], ropeC[:, pcol:pcol + nq], w=['rtab'])
            dma('sp', 'rtab', rtab[:, 1, 0:nq], ropeS[:, pcol:pcol + nq], w=['rtab'])
            for hg in range(2):
                def mmn(e, hg=hg):
                    ins = None
                    for h4 in range(4):
                        for k in range(4):
                            ins = e.matmul(ps[6][:, h4 * 128:h4 * 128 + nq], lhsT=wqn[:, k, hg * 4 + h4, :], rhs=cqT[:, k, hc:hc + nq], start=(k == 0), stop=(k == 3))
                    return ins
                P.op('pe', mmn, r=['wq', 'cqT'], w=['ps6'])
                P.op('act', lambda e, hg=hg: e.activation(out=qn[:, hg * 4:hg * 4 + 4, 0:nq], in_=ps[6].rearrange("p (h t) -> p h t", h=4)[:, :, 0:nq], func=AF.Copy),
                     r=['ps6'], w=['qn'])
            for hp in range(4):
                def mml(e, hp=hp):
                    ins = None
                    for i2 in range(4):
                        h = hp * 2 + i2 // 2; cc = i2 % 2
                        ins = e.matmul(ps[7][:, i2 * 128:i2 * 128 + nq], lhsT=wuk[:, h, cc * 128:(cc + 1) * 128], rhs=qn[:, h, 0:nq], start=True, stop=True)
                    return ins
                P.op('pe', mml, r=['wq', 'qn'], w=['ps7'])
                P.op('dve', lambda e, hp=hp: e.tensor_copy(out=qlT[:, hp * 2:hp * 2 + 2, :, 0:nq],
                                                           in_=ps[7].rearrange("p (h c t) -> p h c t", h=2, c=2)[:, :, :, 0:nq]), r=['ps7'], w=['qlT'])
            for hg in range(2):
                for which, wt, bank in ((0, wqr, 6), (1, wqp, 7)):
                    def mmr(e, hg=hg, wt=wt, bank=bank):
                        ins = None
                        for h4 in range(4):
                            for k in range(4):
                                ins = e.matmul(ps[bank][0:64, h4 * 128:h4 * 128 + nq], lhsT=wt[:, k, hg * 4 + h4, :], rhs=cqT[:, k, hc:hc + nq], start=(k == 0), stop=(k == 3))
                        return ins
                    P.op('pe', mmr, r=['wq', 'cqT'], w=['ps%d' % bank])
                    P.op('dve', lambda e, which=which, bank=bank: e.tensor_tensor(
                        out=qa[:, which, :, 0:nq], in0=ps[bank][0:64, :].rearrange("p (h t) -> p h t", h=4)[:, :, 0:nq],
                        in1=rtab[:, which, 0:nq].unsqueeze(1).to_broadcast([64, 4, nq]), op=ALU.mult), r=['ps%d' % bank, 'rtab'], w=['qa%d' % which])
                P.op('dve', lambda e, hg=hg: e.tensor_tensor(out=qrT[:, hg * 4:hg * 4 + 4, 0:nq], in0=qa[:, 0, :, 0:nq], in1=qa[:, 1, :, 0:nq], op=ALU.add),
                     r=['qa0', 'qa1'], w=['qrT'])

        def do_block(j):
            hc = j * 128
            qproj(hc, 128, NPF + hc)
            nk = NPF + (j + 1) * 128
            nkc = nk // 128
            ngrp = (nk + 511) // 512
            for h in range(8):
                def sc(e, h=h):
                    ins = None
                    for g in range(ngrp):
                        k0 = g * 512; kw = min(512, nk - k0)
                        e.matmul(ps[g][:, 0:kw], lhsT=qlT[:, h, 0, :], rhs=latT[:, 0, k0:k0 + kw], start=True, stop=False)
                        e.matmul(ps[g][:, 0:kw], lhsT=qlT[:, h, 1, :], rhs=latT[:, 1, k0:k0 + kw], start=False, stop=False)
                        ins = e.matmul(ps[g][:, 0:kw], lhsT=qrT[:, h, :], rhs=krT[:, k0:k0 + kw], start=False, stop=True)
                    return ins
                SB = ['ps0', 'ps1', 'ps2', 'ps3']
                P.op('pe', sc, r=['qlT', 'qrT', 'latT', 'krT'], w=SB)
                P.op('dve', lambda e: e.tensor_scalar(out=Sv[:, 0:NPF], in0=Sv[:, 0:NPF], scalar1=flg[:, 1:2], scalar2=None, op0=ALU.add), r=SB + ['flg'], w=SB)
                P.op('dve', lambda e: e.tensor_tensor(out=Sv[:, nk - 128:nk], in0=Sv[:, nk - 128:nk], in1=trit[:], op=ALU.add), r=SB + ['trit'], w=SB)
                P.op('dve', lambda e: e.reduce_max(out=stt[:, 0:1], in_=Sv[:, 0:nk], axis=AX.X), r=SB, w=['stt0'])
                P.op('dve', lambda e: e.tensor_scalar(out=stt[:, 1:2], in0=stt[:, 0:1], scalar1=-SM_SCALE, scalar2=None, op0=ALU.mult), r=['stt0'], w=['stt1'])
                P.op('act', lambda e: e.activation(out=Pm[:, 0:nk], in_=Sv[:, 0:nk], func=AF.Exp, bias=stt[:, 1:2], scale=SM_SCALE), r=SB + ['stt1'], w=['Pm'])
                for half in range(2):
                    c_lo = half * 8; c_hi = min(nkc, c_lo + 8)
                    if c_hi <= c_lo: continue
                    pv4 = ps[4][:].bitcast(BF16)
                    def trp(e, c_lo=c_lo, c_hi=c_hi, pv4=pv4):
                        ins = None
                        for kc in range(c_lo, c_hi):
                            ins = e.transpose(pv4[:, (kc - c_lo) * 128:(kc - c_lo + 1) * 128], Pm[:, kc * 128:(kc + 1) * 128], identb[:])
                        return ins
                    P.op('pe', trp, r=['Pm', 'identb'], w=['ps4'])
                    P.op('dve', lambda e, c_lo=c_lo, c_hi=c_hi, pv4=pv4: e.tensor_copy(
                        out=PT[:, c_lo:c_hi, :], in_=pv4[:, 0:(c_hi - c_lo) * 128].rearrange("p (k t) -> p k t", t=128)), r=['ps4'], w=['PT'])
                def pvm(e):
                    ins = None
                    for kc in range(nkc):
                        ins = e.matmul(ps[5][:, 0:257], lhsT=PT[:, kc, :], rhs=lat_tok[:, kc, 0:257], start=(kc == 0), stop=(kc == nkc - 1))
                    return ins
                P.op('pe', pvm, r=['PT', 'lat_tok', 'lat_tok_ones'], w=['ps5'])
                P.op('dve', lambda e: e.reciprocal(out=stt[:, 2:3], in_=ps[5][:, 256:257]), r=['ps5'], w=['stt2'])
                P.op('act', lambda e: e.activation(out=On[:], in_=ps[5][:, 0:256], func=AF.Copy, scale=stt[:, 2:3]), r=['ps5', 'stt2'], w=['On'])
                pv6 = ps[6][:].bitcast(BF16)
                def tro(e, pv6=pv6):
                    e.transpose(pv6[:, 0:128], On[:, 0:128], identb[:])
                    return e.transpose(pv6[:, 128:256], On[:, 128:256], identb[:])
                P.op('pe', tro, r=['On', 'identb'], w=['ps6'])
                P.op('act', lambda e, h=h, pv6=pv6: e.activation(out=OT[:, h, :, :], in_=pv6[:, 0:256].rearrange("p (c t) -> p c t", c=2), func=AF.Copy), r=['ps6'], w=['OT'])
            for hg in range(2):
                def mmo(e, hg=hg):
                    ins = None
                    for h4 in range(4):
                        h = hg * 4 + h4
                        e.matmul(ps[7][:, h4 * 128:(h4 + 1) * 128], lhsT=wuvs[:, h, 0, :], rhs=OT[:, h, 0, :], start=True, stop=False)
                        ins = e.matmul(ps[7][:, h4 * 128:(h4 + 1) * 128], lhsT=wuvs[:, h, 1, :], rhs=OT[:, h, 1, :], start=False, stop=True)
                    return ins
                P.op('pe', mmo, r=['wuvs', 'OT'], w=['ps7'])
                P.op('act', lambda e, hg=hg, hc=hc: e.activation(out=mixT[:, hg * 4:hg * 4 + 4, hc:hc + 128], in_=ps[7].rearrange("p (h t) -> p h t", h=4), func=AF.Copy),
                     r=['ps7'], w=['mixT'])

        for j_ in range(8 if DBG != 21 else 1):
            do_block(j_)

        dma('sp', 'pts', pts[:], ptT, w=['pts'])
        P.op('dve', lambda e: e.tensor_single_scalar(out=pts[:], in_=pts[:], scalar=4, op=ALU.logical_shift_left), r=['pts'], w=['pts'])
        if DBG == 21:
            P.op('pool', lambda e: e.memset(OTS[:], 0.0), w=['OTS0', 'OTS1'])
        qproj(NO, 64, 2048)
        P.op('pool', lambda e: e.tensor_copy(out=qlS[:], in_=qlT[:, :, :, 0:64]), r=['qlT'], w=['qlS'])
        P.op('pool', lambda e: e.tensor_copy(out=qrSs[:], in_=qrT[:, :, 0:64]), r=['qrT'], w=['qrSs'])
        P.emit()
        st2a.close()
        st2b = st2
        NCH = 2
        Lg = sb('Lg', [128, NCH, 2, 8, 256], BF16, st2b); Kg = sb('Kg', [128, NCH, 2, 8, 64], BF16, st2b)
        KT = sb('KT', [128, NCH, 3, 512], BF16, st2b)
        qS = sb('qS', [128, NCH, 2, 32], BF16, st2b); qrS = sb('qrS', [64, NCH, 32], BF16, st2b)
        Ps = sb('Ps', [32, NCH, 512], BF16, st2b); PTs = sb('PTs', [128, NCH, 4, 32], BF16, st2b)
        sst = sb('sst', [32, NCH, 12], F32, st2b)
        Oa = sb('Oa', [32, NCH, 256], F32, st2b); Ob = sb('Ob', [32, NCH, 256], BF16, st2b)
        Ln = sb('Ln', [4, NCH, 256], BF16, st2b); PnT = sb('PnT', [4, NCH, 32], BF16, st2b)
        clv = cl.rearrange("n (a r) c -> (n a) (r c)", r=8)
        ckv = ck.rearrange("n (a r) c -> (n a) (r c)", r=8)

        def do_sample(s_, c):
            bk = 4 * c
            A, Bk, C_, D_ = bk, bk + 1, bk + 2, bk + 3
            rA, rB, rC, rD = 'ps%d' % A, 'ps%d' % Bk, 'ps%d' % C_, 'ps%d' % D_
            T_ = lambda n_: '%s_%d' % (n_, c)
            pvA = ps[A][:].bitcast(BF16); pvB = ps[Bk][:].bitcast(BF16)
            st_ = sst[:, c, :]
            def softmax_update(nkeys, srcS, first):
                P.op('dve', lambda e: e.reduce_max(out=st_[:, 1:2], in_=srcS, axis=AX.X), r=[rC], w=[T_('mx')])
                if first:
                    P.op('dve', lambda e: e.tensor_copy(out=st_[:, 2:3], in_=st_[:, 1:2]), r=[T_('mx')], w=[T_('mn')])
                else:
                    P.op('dve', lambda e: e.tensor_tensor(out=st_[:, 2:3], in0=st_[:, 0:1], in1=st_[:, 1:2], op=ALU.max), r=[T_('m'), T_('mx')], w=[T_('mn')])
                    P.op('dve', lambda e: e.tensor_tensor(out=st_[:, 3:4], in0=st_[:, 0:1], in1=st_[:, 2:3], op=ALU.subtract), r=[T_('m'), T_('mn')], w=[T_('d')])
                    P.op('act', lambda e: e.activation(out=st_[:, 4:5], in_=st_[:, 3:4], func=AF.Exp, scale=SM_SCALE), r=[T_('d')], w=[T_('corr')])
                P.op('dve', lambda e: e.tensor_scalar(out=st_[:, 5:6], in0=st_[:, 2:3], scalar1=-SM_SCALE, scalar2=None, op0=ALU.mult), r=[T_('mn')], w=[T_('nb')])
                P.op('dve', lambda e: e.tensor_copy(out=st_[:, 0:1], in_=st_[:, 2:3]), r=[T_('mn')], w=[T_('m')])
                P.op('act', lambda e: e.activation(out=Ps[:, c, 0:nkeys], in_=srcS, func=AF.Exp, bias=st_[:, 5:6], scale=SM_SCALE, accum_out=st_[:, 7:8]),
                     r=[rC, T_('nb')], w=[T_('Ps'), T_('lu')])
                if first:
                    P.op('dve', lambda e: e.tensor_copy(out=st_[:, 6:7], in_=st_[:, 7:8]), r=[T_('lu')], w=[T_('l')])
                else:
                    P.op('dve', lambda e: e.scalar_tensor_tensor(out=st_[:, 6:7], in0=st_[:, 6:7], scalar=st_[:, 4:5], in1=st_[:, 7:8], op0=ALU.mult, op1=ALU.add),
                         r=[T_('l'), T_('corr'), T_('lu')], w=[T_('l')])
            def acc_update(first):
                if first:
                    P.op('dve', lambda e: e.tensor_copy(out=Oa[:, c, :], in_=ps[D_][0:32, 0:256]), r=[rD], w=[T_('Oa')])
                else:
                    P.op('dve', lambda e: e.scalar_tensor_tensor(out=Oa[:, c, :], in0=Oa[:, c, :], scalar=st_[:, 4:5], in1=ps[D_][0:32, 0:256], op0=ALU.mult, op1=ALU.add),
                         r=[T_('Oa'), T_('corr'), rD], w=[T_('Oa')])
            for cc in range(2):
                P.op('dve', lambda e, cc=cc: e.tensor_copy(out=qS[:, c, cc, :].rearrange("p (h t) -> p h t", h=8), in_=qlS[:, :, cc, s_:64:16]), r=['qlS'], w=[T_('qS')])
            P.op('dve', lambda e: e.tensor_copy(out=qrS[:, c, :].rearrange("p (h t) -> p h t", h=8), in_=qrSs[:, :, s_:64:16]), r=['qrSs'], w=[T_('qrS')])
            yield
            for u in range(32):
                g, q4 = u // 2, u % 2
                gb = g % 2
                if q4 == 0:
                    P.op('pool', lambda e, g=g, gb=gb: e.indirect_dma_start(out=Lg[:, c, gb].rearrange("p r c -> p (r c)"), out_offset=None, in_=clv,
                         in_offset=bass.IndirectOffsetOnAxis(ap=pts[:, s_:s_ + 1], axis=0), element_offset=g * 2048), r=['pts'], w=[T_('Lg%d' % gb)], lane='Lg%d%d' % (c, gb))
                    P.op('pool', lambda e, g=g, gb=gb: e.indirect_dma_start(out=Kg[:, c, gb].rearrange("p r c -> p (r c)"), out_offset=None, in_=ckv,
                         in_offset=bass.IndirectOffsetOnAxis(ap=pts[:, s_:s_ + 1], axis=0), element_offset=g * 512), r=['pts'], w=[T_('Kg%d' % gb)], lane='Kg%d%d' % (c, gb))
                def trk(e, gb=gb, q4=q4):
                    ins = None
                    for r4 in range(4):
                        r_ = q4 * 4 + r4
                        e.transpose(pvA[:, r4 * 128:(r4 + 1) * 128], Lg[:, c, gb, r_, 0:128], identb[:])
                        e.transpose(pvA[:, 512 + r4 * 128:512 + (r4 + 1) * 128], Lg[:, c, gb, r_, 128:256], identb[:])
                        ins = e.transpose(pvB[0:64, r4 * 128:(r4 + 1) * 128], Kg[:, c, gb, r_, :], identb[:])
                    return ins
                P.op('pe', trk, r=[T_('Lg%d' % gb), T_('Kg%d' % gb), 'identb'], w=[rA, rB])
                yield
                P.op('dve', lambda e: e.tensor_copy(out=KT[:, c, 0:2, :], in_=pvA[:, 0:1024].rearrange("p (k t) -> p k t", k=2)), r=[rA], w=[T_('KT01')])
                P.op('act', lambda e: e.activation(out=KT[0:64, c, 2, :], in_=pvB[0:64, 0:512], func=AF.Copy), r=[rB], w=[T_('KT2')])
                def scs(e):
                    o_ = ps[C_][0:32, :]
                    e.matmul(o_, lhsT=qS[:, c, 0, :], rhs=KT[:, c, 0, :], start=True, stop=False)
                    e.matmul(o_, lhsT=qS[:, c, 1, :], rhs=KT[:, c, 1, :], start=False, stop=False)
                    return e.matmul(o_, lhsT=qrS[:, c, :], rhs=KT[0:64, c, 2, :], start=False, stop=True)
                P.op('pe', scs, r=[T_('qS'), T_('qrS'), T_('KT01'), T_('KT2')], w=[rC])
                yield
                softmax_update(512, ps[C_][0:32, :], first=(u == 0))
                def trps(e):
                    ins = None
                    for kc in range(4):
                        ins = e.transpose(pvB[:, 512 + kc * 32:512 + (kc + 1) * 32], Ps[:, c, kc * 128:(kc + 1) * 128], identb[0:32, 0:32])
                    return ins
                P.op('pe', trps, r=[T_('Ps'), 'identb'], w=[rB])
                yield
                P.op('act', lambda e: e.activation(out=PTs[:, c, :, :], in_=pvB[:, 512:640].rearrange("p (k q) -> p k q", q=32), func=AF.Copy), r=[rB], w=[T_('PTs')])
                def pvs(e, gb=gb, q4=q4):
                    ins = None
                    for kc in range(4):
                        ins = e.matmul(ps[D_][0:32, 0:256], lhsT=PTs[:, c, kc, :], rhs=Lg[:, c, gb, q4 * 4 + kc, :], start=(kc == 0), stop=(kc == 3))
                    return ins
                P.op('pe', pvs, r=[T_('PTs'), T_('Lg%d' % gb)], w=[rD])
                acc_update(first=(u == 0))
                yield
            def scn(e):
                o_ = ps[C_][0:32, 0:4]
                e.matmul(o_, lhsT=qS[:, c, 0, :], rhs=latT[:, 0, 2048 + s_:2048 + 64:16], start=True, stop=False)
                e.matmul(o_, lhsT=qS[:, c, 1, :], rhs=latT[:, 1, 2048 + s_:2048 + 64:16], start=False, stop=False)
                return e.matmul(o_, lhsT=qrS[:, c, :], rhs=krT[:, 2048 + s_:2048 + 64:16], start=False, stop=True)
            P.op('pe', scn, r=[T_('qS'), T_('qrS'), 'latT', 'krT'], w=[rC])
            P.op('dve', lambda e: e.tensor_tensor(out=ps[C_][0:32, 0:4], in0=ps[C_][0:32, 0:4], in1=smk[:], op=ALU.add), r=[rC, 'smk'], w=[rC])
            softmax_update(4, ps[C_][0:32, 0:4], first=False)
            yield
            P.op('pe', lambda e: e.matmul(ps[C_][0:4, 0:256], lhsT=identb[0:64, s_:64:16], rhs=lat_tok[0:64, 16, 0:256], start=True, stop=True),
                 r=['identb', 'lat_tok'], w=[rC])
            P.op('act', lambda e: e.activation(out=Ln[:, c, :], in_=ps[C_][0:4, 0:256], func=AF.Copy), r=[rC], w=[T_('Ln')])
            P.op('pe', lambda e: e.transpose(pvB[0:4, 704:736], Ps[:, c, 0:4], identb[0:32, 0:32]), r=[T_('Ps'), 'identb'], w=[rB])
            P.op('act', lambda e: e.activation(out=PnT[:, c, :], in_=pvB[0:4, 704:736], func=AF.Copy), r=[rB], w=[T_('PnT')])
            P.op('pe', lambda e: e.matmul(ps[D_][0:32, 0:256], lhsT=PnT[:, c, :], rhs=Ln[:, c, :], start=True, stop=True), r=[T_('PnT'), T_('Ln')], w=[rD])
            acc_update(first=False)
            yield
            P.op('dve', lambda e: e.reciprocal(out=st_[:, 8:9], in_=st_[:, 6:7]), r=[T_('l')], w=[T_('ri')])
            P.op('act', lambda e: e.activation(out=Ob[:, c, :], in_=Oa[:, c, :], func=AF.Copy, scale=st_[:, 8:9]), r=[T_('Oa'), T_('ri')], w=[T_('Ob')])
            def trob(e):
                e.transpose(pvB[:, 640:672], Ob[:, c, 0:128], identb[0:32, 0:32])
                return e.transpose(pvB[:, 672:704], Ob[:, c, 128:256], identb[0:32, 0:32])
            P.op('pe', trob, r=[T_('Ob'), 'identb'], w=[rB])
            P.op('act', lambda e: e.activation(out=OTS[:, :, s_, :], in_=pvB[:, 640:704].rearrange("p (c q) -> p c q", c=2), func=AF.Copy), r=[rB], w=['OTS%d' % c])
            yield

        NSAMP = 16 if DBG != 21 else 2
        for s0 in range(0, NSAMP, NCH):
            gens = [do_sample(s0 + c_, c_) for c_ in range(NCH)]
            alive = list(gens)
            while alive:
                for g_ in list(alive):
                    try:
                        next(g_)
                    except StopIteration:
                        alive.remove(g_)
        for h in range(8):
            def mmos(e, h=h):
                e.matmul(ps[7][:, 0:64], lhsT=wuvs[:, h, 0, :], rhs=OTS[:, 0, :, h * 4:(h + 1) * 4], start=True, stop=False)
                return e.matmul(ps[7][:, 0:64], lhsT=wuvs[:, h, 1, :], rhs=OTS[:, 1, :, h * 4:(h + 1) * 4], start=False, stop=True)
            P.op('pe', mmos, r=['wuvs', 'OTS0', 'OTS1'], w=['ps7'])
            P.op('act', lambda e, h=h: e.activation(out=mixT[:, h, NO:NO + 64].rearrange("p (t s) -> p s t", t=4),
                                                     in_=ps[7][:, 0:64].rearrange("p (s t) -> p s t", t=4), func=AF.Copy), r=['ps7'], w=['mixT'])
        P.emit(final=(STAGE == 2))
    es_mid.close()
    if STAGE == 2:
        es.close(); return nc

    TILES = [(0, 512), (512, 512), (1024, 64)]
    with ExitStack() as st3:
        sq2 = sb('sq2', [128, 512], BF16, st3)
        rsb = sb('rsb', [128, 512], F32, st3)
        tmpf = sb('tmpf', [128, 512], F32, st3); tmpg = sb('tmpg', [128, 512], F32, st3)
        wbuf = sb('wbuf', [128, 3, 16, 128], BF16, st3)
        wbc = [0]

        def norm_stats(srcs, nfeat, n, tag):
            for j, s_ in enumerate(srcs):
                P.op('act', lambda e, s_=s_: e.activation(out=sq2[:, 0:n], in_=s_, func=AF.Square), r=[tag], w=['sq2'])
                P.op('pe', lambda e, j=j: e.matmul(ps[7][:, 0:n], lhsT=onesb[:], rhs=sq2[:, 0:n], start=(j == 0), stop=(j == len(srcs) - 1)),
                     r=['sq2', 'onesb'], w=['ps7'])
            P.op('act', lambda e: e.activation(out=rsb[:, 0:n], in_=ps[7][:, 0:n], func=AF.Sqrt, bias=epsT[:], scale=1.0 / nfeat), r=['ps7', 'epsT'], w=['rsb'])
            P.op('dve', lambda e: e.reciprocal(out=rsb[:, 0:n], in_=rsb[:, 0:n]), r=['rsb'], w=['rsb'])

        def load_w(src_ap, nk, ncol=128):
            b = wbc[0] % 3; wbc[0] += 1
            wt = wbuf[:, b, 0:nk, 0:ncol]
            dma('pool', 'wbuf%d' % b, wt, src_ap.rearrange("(k p) c -> p k c", p=128), w=['wbuf%d' % b])
            return wt, 'wbuf%d' % b

        for (c0, n) in TILES:
            for (k0, gcol) in ((0, GV_OM), (8, GV_OL)):
                norm_stats([mixT[:, k0 + k, c0:c0 + n] for k in range(8)], 1024, n, 'mixT')
                for k in range(8):
                    P.op('dve', lambda e, k=k, k0=k0, gcol=gcol, c0=c0, n=n: e.scalar_tensor_tensor(
                        out=mixT[:, k0 + k, c0:c0 + n], in0=mixT[:, k0 + k, c0:c0 + n], scalar=gv[:, gcol + k:gcol + k + 1], in1=rsb[:, 0:n],
                        op0=ALU.mult, op1=ALU.mult), r=['mixT', 'gv', 'rsb'], w=['mixT'])
        for m in range(16):
            wt, wres = load_w(w_o[:, m * 128:(m + 1) * 128], 16)
            for ti, (c0, n) in enumerate(TILES):
                bank = 4 + ti % 2
                def mmw(e, wt=wt, c0=c0, n=n, bank=bank):
                    ins = None
                    for k in range(16):
                        ins = e.matmul(ps[bank][:, 0:n], lhsT=wt[:, k, :], rhs=mixT[:, k, c0:c0 + n], start=(k == 0), stop=(k == 15))
                    return ins
                P.op('pe', mmw, r=[wres, 'mixT'], w=['ps%d' % bank])
                P.op('dve', lambda e, m=m, c0=c0, n=n, bank=bank: e.tensor_tensor(out=hT[:, m, c0:c0 + n], in0=hT[:, m, c0:c0 + n], in1=ps[bank][:, 0:n], op=ALU.add),
                     r=['hT', 'ps%d' % bank], w=['hT'])

        def fm_rmsnorm_to_mix(gcol, extra=None):
            for (c0, n) in TILES:
                norm_stats([hT[:, k, c0:c0 + n] for k in range(16)], D, n, 'hT')
                for k in range(16):
                    P.op('dve', lambda e, k=k, c0=c0, n=n: e.scalar_tensor_tensor(out=tmpf[:, 0:n], in0=hT[:, k, c0:c0 + n], scalar=gv[:, gcol + k:gcol + k + 1],
                                                                                 in1=rsb[:, 0:n], op0=ALU.mult, op1=ALU.mult), r=['hT', 'gv', 'rsb'], w=['tmpf'])
                    P.op('pool', lambda e, k=k, c0=c0, n=n: e.tensor_copy(out=mixT[:, k, c0:c0 + n], in_=tmpf[:, 0:n]), r=['tmpf'], w=['mixT'])
                    if extra is not None: extra(k, c0, n)

        if DBG != 31:
          with ExitStack() as st4:
            wr32 = sb('wr32', [128, 16, 36], F32, st4)
            lgT = sb('lgT', [36, NT], F32, st4)
            brow = sb('brow', [128, 36], F32, st4)
            tl = sb('tl', [128, 36], F32, st4); ohg = sb('ohg', [128, 4], F32, st4); em = sb('em', [128, 32], F32, st4)
            m8 = sb('m8', [128, 8], F32, st4); rs_ = sb('rs_', [128, 8], F32, st4); cmb = sb('cmb', [128, 32], F32, st4)
            combT = sb('combT', [32, NT], F32, st4)
            esel = sb('esel', [32, 128], F32, st4); ones32 = sb('ones32', [32, 128], F32, st4)
            cbe = sb('cbe', [128, NT], F32, st4)
            gu = sb('gu', [128, 3, 2, 16, 128], BF16, st4)
            wd = sb('wd', [128, 2, 4, 2048], BF16, st4)
            actT = sb('actT', [128, 4, NT], BF16, st4)
            dma('sp', 'wr32', wr32[:, :, 0:4], w_group.rearrange("(k p) c -> p k c", p=128), w=['wr32'])
            dma('sp', 'wr32', wr32[:, :, 4:36], w_router.rearrange("(k p) c -> p k c", p=128), w=['wr32'])
            dma('sp', 'brow', brow[:, 0:4], b_group.partition_broadcast(128), w=['brow'])
            dma('sp', 'brow', brow[:, 4:36], b_router.partition_broadcast(128), w=['brow'])
            P.op('pool', lambda e: e.memset(ones32[:], 1.0), w=['ones32'])
            cur = {}
            def router_hook(k, c0, n):
                bank = 6
                P.op('pe', lambda e, k=k, n=n: e.matmul(ps[6][0:36, 0:n], lhsT=wr32[:, k, :], rhs=tmpf[:, 0:n], start=(k == 0), stop=(k == 15)),
                     r=['wr32', 'tmpf'], w=['ps6'])
                if k == 15:
                    P.op('act', lambda e, c0=c0, n=n: e.activation(out=lgT[:, c0:c0 + n], in_=ps[6][0:36, 0:n], func=AF.Copy), r=['ps6'], w=['lgT'])
            fm_rmsnorm_to_mix(GV_FFN, router_hook)
            for bi in range(9):
                t0_ = bi * 128; rows = min(128, NT - t0_)
                P.op('pe', lambda e, t0_=t0_, rows=rows: e.transpose(ps[6][0:rows, 0:36], lgT[:, t0_:t0_ + rows], identf[0:36, 0:36]), r=['lgT', 'identf'], w=['ps6'])
                R_ = slice(0, rows)
                P.op('dve', lambda e, R_=R_: e.tensor_tensor(out=tl[R_, :], in0=ps[6][R_, 0:36], in1=brow[R_, :], op=ALU.add), r=['ps6', 'brow'], w=['tl'])
                P.op('dve', lambda e, R_=R_: e.reduce_max(out=rs_[R_, 0:1], in_=tl[R_, 0:4], axis=AX.X), r=['tl'], w=['rs0'])
                P.op('dve', lambda e, R_=R_: e.tensor_scalar(out=ohg[R_, :], in0=tl[R_, 0:4], scalar1=rs_[R_, 0:1], scalar2=None, op0=ALU.is_ge), r=['tl', 'rs0'], w=['ohg'])
                P.op('dve', lambda e, R_=R_: e.tensor_scalar(out=rs_[R_, 1:2], in0=rs_[R_, 0:1], scalar1=-1.0, scalar2=None, op0=ALU.mult), r=['rs0'], w=['rs1'])
                P.op('act', lambda e, R_=R_: e.activation(out=tl[R_, 0:4], in_=tl[R_, 0:4], func=AF.Exp, bias=rs_[R_, 1:2], scale=1.0, accum_out=rs_[R_, 2:3]),
                     r=['tl', 'rs1'], w=['tl', 'rs2'])
                P.op('dve', lambda e, R_=R_: e.tensor_scalar(out=ohg[R_, :], in0=ohg[R_, :], scalar1=-1.0, scalar2=1e30, op0=ALU.add, op1=ALU.mult), r=['ohg'], w=['ohg'])
                P.op('dve', lambda e, R_=R_, rows=rows: e.tensor_tensor(out=em[R_, :].rearrange("p (g x) -> p g x", g=4), in0=tl[R_, 4:36].rearrange("p (g x) -> p g x", g=4),
                                                             in1=ohg[R_, :].unsqueeze(2).to_broadcast([rows, 4, 8]), op=ALU.add), r=['tl', 'ohg'], w=['em'])
                P.op('dve', lambda e, R_=R_: e.max(out=m8[R_, :], in_=em[R_, :]), r=['em'], w=['m8'])
                P.op('dve', lambda e, R_=R_: e.tensor_scalar(out=cmb[R_, :], in0=em[R_, :], scalar1=m8[R_, 1:2], scalar2=None, op0=ALU.is_ge), r=['em', 'm8'], w=['cmb'])
                P.op('dve', lambda e, R_=R_: e.tensor_scalar(out=rs_[R_, 3:4], in0=m8[R_, 0:1], scalar1=-1.0, scalar2=None, op0=ALU.mult), r=['m8'], w=['rs3'])
                P.op('act', lambda e, R_=R_: e.activation(out=em[R_, :], in_=em[R_, :], func=AF.Exp, bias=rs_[R_, 3:4], scale=1.0), r=['em', 'rs3'], w=['em'])
                P.op('act', lambda e, R_=R_: e.activation(out=rs_[R_, 4:5], in_=m8[R_, 1:2], func=AF.Exp, bias=rs_[R_, 3:4], scale=1.0), r=['m8', 'rs3'], w=['rs4'])
                P.op('dve', lambda e, R_=R_: e.tensor_scalar(out=rs_[R_, 4:5], in0=rs_[R_, 4:5], scalar1=1.0, scalar2=rs_[R_, 2:3], op0=ALU.add, op1=ALU.mult), r=['rs4', 'rs2'], w=['rs4'])
                P.op('dve', lambda e, R_=R_: e.reciprocal(out=rs_[R_, 5:6], in_=rs_[R_, 4:5]), r=['rs4'], w=['rs5'])
                P.op('dve', lambda e, R_=R_: e.scalar_tensor_tensor(out=cmb[R_, :], in0=em[R_, :], scalar=rs_[R_, 5:6], in1=cmb[R_, :], op0=ALU.mult, op1=ALU.mult),
                     r=['em', 'rs5', 'cmb'], w=['cmb'])
                P.op('pe', lambda e, rows=rows: e.transpose(ps[7][0:32, 0:rows], cmb[0:rows, :], identf[0:rows, 0:rows]), r=['cmb', 'identf'], w=['ps7'])
                P.op('act', lambda e, t0_=t0_, rows=rows: e.activation(out=combT[:, t0_:t0_ + rows], in_=ps[7][0:32, 0:rows], func=AF.Copy), r=['ps7'], w=['combT'])
            NEXP = 32 if DBG != 32 else 2
            guc = [0]; wdc = [0]
            for ex in range(NEXP):
                P.op('dve', lambda e, ex=ex: e.tensor_scalar(out=esel[:], in0=ones32[:], scalar1=identf[0:32, ex:ex + 1], scalar2=None, op0=ALU.mult), r=['ones32', 'identf'], w=['esel'])
                for ti, (c0, n) in enumerate(TILES):
                    P.op('pe', lambda e, c0=c0, n=n: e.matmul(ps[6][:, 0:n], lhsT=esel[:], rhs=combT[:, c0:c0 + n], start=True, stop=True), r=['esel', 'combT'], w=['ps6'])
                    P.op('act', lambda e, c0=c0, n=n: e.activation(out=cbe[:, c0:c0 + n], in_=ps[6][:, 0:n], func=AF.Copy), r=['ps6'], w=['cbe'])
                wb_ = wdc[0] % 2; wdc[0] += 1
                for fc in range(4):
                    dma('pool', 'wd%d' % wb_, wd[:, wb_, fc, :], w_down[ex, fc * 128:(fc + 1) * 128, :], w=['wd%d' % wb_])
                for fc in range(4):
                    gb = guc[0] % 3; guc[0] += 1
                    dma('pool', 'gu%d' % gb, gu[:, gb, 0], w_gate[ex, :, fc * 128:(fc + 1) * 128].rearrange("(k p) c -> p k c", p=128), w=['gu%d' % gb])
                    dma('pool', 'gu%d' % gb, gu[:, gb, 1], w_up[ex, :, fc * 128:(fc + 1) * 128].rearrange("(k p) c -> p k c", p=128), w=['gu%d' % gb])
                    for ti, (c0, n) in enumerate(TILES):
                        for which in range(2):
                            bank = 2 * (ti % 2) + which
                            def mmg(e, gb=gb, which=which, c0=c0, n=n, bank=bank):
                                ins = None
                                for k in range(16):
                                    ins = e.matmul(ps[bank][:, 0:n], lhsT=gu[:, gb, which, k, :], rhs=mixT[:, k, c0:c0 + n], start=(k == 0), stop=(k == 15))
                                return ins
                            P.op('pe', mmg, r=['gu%d' % gb, 'mixT'], w=['ps%d' % bank])
                        b0 = 2 * (ti % 2)
                        P.op('act', lambda e, n=n, b0=b0: e.activation(out=tmpf[:, 0:n], in_=ps[b0][:, 0:n], func=AF.Silu), r=['ps%d' % b0], w=['tmpf'])
                        P.op('dve', lambda e, n=n, b0=b0: e.tensor_tensor(out=tmpf[:, 0:n], in0=tmpf[:, 0:n], in1=ps[b0 + 1][:, 0:n], op=ALU.mult), r=['tmpf', 'ps%d' % (b0 + 1)], w=['tmpf'])
                        P.op('dve', lambda e, fc=fc, c0=c0, n=n: e.tensor_tensor(out=actT[:, fc, c0:c0 + n], in0=tmpf[:, 0:n], in1=cbe[:, c0:c0 + n], op=ALU.mult),
                             r=['tmpf', 'cbe'], w=['actT'])
                for m in range(16):
                    for ti, (c0, n) in enumerate(TILES):
                        bank = 4 + (m * 3 + ti) % 2
                        def mmd(e, wb_=wb_, m=m, c0=c0, n=n, bank=bank):
                            ins = None
                            for fc in range(4):
                                ins = e.matmul(ps[bank][:, 0:n], lhsT=wd[:, wb_, fc, m * 128:(m + 1) * 128], rhs=actT[:, fc, c0:c0 + n], start=(fc == 0), stop=(fc == 3))
                            return ins
                        P.op('pe', mmd, r=['wd%d' % wb_, 'actT'], w=['ps%d' % bank])
                        P.op('dve', lambda e, m=m, c0=c0, n=n, bank=bank: e.tensor_tensor(out=hT[:, m, c0:c0 + n], in0=hT[:, m, c0:c0 + n], in1=ps[bank][:, 0:n], op=ALU.add),
                             r=['hT', 'ps%d' % bank], w=['hT'])
            P.emit()

        with ExitStack() as st5:
            pT = sb('pT', [128, 2, NT], BF16, st5)
            pst = sb('pst', [128, 256], F32, st5)
            wpp = sb('wpp', [128, 2, 2, 128], BF16, st5)
            fm_rmsnorm_to_mix(GV_PLE)
            for bi in range(9):
                t0_ = bi * 128; rows = min(128, NT - t0_)
                srcp = po[t0_:t0_ + rows, :] if bi < 8 else psm[0:rows, :]
                dma('sp', 'pst', pst[0:rows, :], srcp, w=['pst'])
                def trp_(e, rows=rows):
                    e.transpose(ps[6][:, 0:rows], pst[0:rows, 0:128], identf[0:rows, 0:rows])
                    return e.transpose(ps[6][:, 128:128 + rows], pst[0:rows, 128:256], identf[0:rows, 0:rows])
                P.op('pe', trp_, r=['pst', 'identf'], w=['ps6'])
                P.op('act', lambda e, t0_=t0_, rows=rows: e.activation(out=pT[:, :, t0_:t0_ + rows], in_=ps[6][:, 0:256].rearrange("p (c t) -> p c t", c=2)[:, :, 0:rows], func=AF.Copy),
                     r=['ps6'], w=['pT'])
            for m in range(16):
                wt, wres = load_w(w_pg[:, m * 128:(m + 1) * 128], 16)
                pb_ = m % 2
                dma('pool', 'wpp%d' % pb_, wpp[:, pb_], w_pp[:, m * 128:(m + 1) * 128].rearrange("(k p) c -> p k c", p=128), w=['wpp%d' % pb_])
                for ti, (c0, n) in enumerate(TILES):
                    b0 = 2 * (ti % 2)
                    def mmg2(e, wt=wt, c0=c0, n=n, b0=b0):
                        ins = None
                        for k in range(16):
                            ins = e.matmul(ps[b0][:, 0:n], lhsT=wt[:, k, :], rhs=mixT[:, k, c0:c0 + n], start=(k == 0), stop=(k == 15))
                        return ins
                    P.op('pe', mmg2, r=[wres, 'mixT'], w=['ps%d' % b0])
                    def mmp2(e, pb_=pb_, c0=c0, n=n, b0=b0):
                        e.matmul(ps[b0 + 1][:, 0:n], lhsT=wpp[:, pb_, 0, :], rhs=pT[:, 0, c0:c0 + n], start=True, stop=False)
                        return e.matmul(ps[b0 + 1][:, 0:n], lhsT=wpp[:, pb_, 1, :], rhs=pT[:, 1, c0:c0 + n], start=False, stop=True)
                    P.op('pe', mmp2, r=['wpp%d' % pb_, 'pT'], w=['ps%d' % (b0 + 1)])
                    P.op('act', lambda e, m=m, n=n, b0=b0: e.activation(out=tmpg[:, 0:n], in_=ps[b0][:, 0:n], func=AF.Sigmoid, bias=gv[:, GV_BPG + m:GV_BPG + m + 1], scale=1.0),
                         r=['ps%d' % b0, 'gv'], w=['tmpg'])
                    P.op('dve', lambda e, n=n, b0=b0: e.tensor_tensor(out=tmpg[:, 0:n], in0=tmpg[:, 0:n], in1=ps[b0 + 1][:, 0:n], op=ALU.mult), r=['tmpg', 'ps%d' % (b0 + 1)], w=['tmpg'])
                    P.op('dve', lambda e, m=m, c0=c0, n=n: e.tensor_tensor(out=hT[:, m, c0:c0 + n], in0=hT[:, m, c0:c0 + n], in1=tmpg[:, 0:n], op=ALU.add), r=['hT', 'tmpg'], w=['hT'])
            P.emit()

        with ExitStack() as st6:
            yst = sb('yst', [128, 2, D], F32, st6)
            yc = [0]
            for (c0, n) in TILES:
                norm_stats([hT[:, k, c0:c0 + n] for k in range(16)], D, n, 'hT')
                nb_ = (n + 127) // 128
                for i in range(nb_):
                    rows = min(128, n - i * 128)
                    yb = yc[0] % 2; yc[0] += 1
                    for q4 in range(4):
                        for k4 in range(4):
                            k = q4 * 4 + k4
                            P.op('dve', lambda e, k=k, k4=k4, c0=c0, i=i, rows=rows: e.scalar_tensor_tensor(
                                out=tmpf[:, k4 * 128:k4 * 128 + rows], in0=hT[:, k, c0 + i * 128:c0 + i * 128 + rows], scalar=gv2[:, k:k + 1],
                                in1=rsb[:, i * 128:i * 128 + rows], op0=ALU.mult, op1=ALU.mult), r=['hT', 'gv', 'rsb'], w=['tmpf'])
                        bank = 4 + q4 % 2
                        def try_(e, rows=rows, bank=bank):
                            ins = None
                            for k4 in range(4):
                                ins = e.transpose(ps[bank][0:rows, k4 * 128:(k4 + 1) * 128], tmpf[:, k4 * 128:k4 * 128 + rows], identf[:])
                            return ins
                        P.op('pe', try_, r=['tmpf', 'identf'], w=['ps%d' % bank])
                        P.op('act', lambda e, q4=q4, rows=rows, bank=bank, yb=yb: e.activation(out=yst[0:rows, yb, q4 * 512:(q4 + 1) * 512], in_=ps[bank][0:rows, :], func=AF.Copy),
                             r=['ps%d' % bank], w=['yst%d' % yb])
                    r0 = c0 + i * 128
                    dst = y_o[r0:r0 + rows, :] if c0 < NO else y_s[0:rows, :]
                    dma('sp', 'yst%d' % yb, dst, yst[0:rows, yb, :], r=['yst%d' % yb])
            P.emit(final=True)
    es.close()
    return nc


def _rope_tables(pos):
    inv = 10000.0 ** (-np.arange(32, dtype=np.float32) / 32)
    ang = pos.astype(np.float32)[None, :] * inv[:, None]
    c = np.cos(ang).astype(np.float32); s = np.sin(ang).astype(np.float32)
    return np.concatenate([c, c], 0), np.concatenate([-s, s], 0)


def make_in_maps(inp):
    f = lambda a: np.ascontiguousarray(np.asarray(a, dtype=np.float32))
    x_prompt = np.asarray(inp['x_prompt']); x_sample = np.asarray(inp['x_sample'])
    perm = (np.arange(64) + 32) % 64
    w_in = np.asarray(inp['w_in'])[0]; w_uq = np.asarray(inp['w_uq'])[0]; w_ukv = np.asarray(inp['w_ukv'])[0]
    shared = dict(
        cl=f(inp['cache_latent'][0]), ck=f(inp['cache_krope'][0]),
        g_mix=f(inp['g_mix'][0]), w_in=f(w_in), wkrp=f(w_in[:, 768:832][:, perm]),
        g_q=f(inp['g_q'][0]), w_uq=f(w_uq), wqrp=f(w_uq.reshape(512, 8, 192)[:, :, 128:][:, :, perm]),
        g_kv=f(inp['g_kv'][0]), wukT=f(w_ukv[:, :, :128].transpose(1, 2, 0)), wuv=f(w_ukv[:, :, 128:].transpose(1, 0, 2)),
        w_conv=f(inp['w_conv'][0]), b_conv=f(inp['b_conv'][0]), w_rg=f(inp['w_rg'][0]), b_rg=f(inp['b_rg'][0]),
        w_ig=f(inp['w_ig'][0]), b_ig=f(inp['b_ig'][0]), lam=f(inp['lru_lambda'][0]),
        g_om=f(inp['g_out_mla'][0]), g_ol=f(inp['g_out_lru'][0]), w_o=f(inp['w_o'][0]), g_ffn=f(inp['g_ffn'][0]),
        w_group=f(inp['w_group'][0]), b_group=f(inp['b_group'][0]), w_router=f(inp['w_router'][0]), b_router=f(inp['b_router'][0]),
        w_gate=f(inp['w_gate'][0]), w_up=f(inp['w_up'][0]), w_down=f(inp['w_down'][0]),
        g_ple=f(inp['g_ple'][0]), w_pg=f(inp['w_ple_gate'][0]), b_pg=f(inp['b_ple_gate'][0]), w_pp=f(inp['w_ple_proj'][0]),
        g_fin=f(inp['g_final']),
        tri=np.where(np.arange(128)[None, :] <= np.arange(128)[:, None], 0.0, NEG).astype(np.float32),
        smask=np.where(np.arange(4)[None, :] <= (np.arange(32) % 4)[:, None], 0.0, NEG).astype(np.float32),
    )
    pm = lambda a: np.asarray(a, np.float32).reshape(-1, 128).T
    gvh = np.zeros((128, 104), np.float32)
    for off, key in [(0, 'g_mix'), (16, 'g_q'), (20, 'g_kv'), (22, 'g_out_mla'), (30, 'g_out_lru'), (38, 'g_ffn'), (54, 'g_ple')]:
        v = pm(np.asarray(inp[key])[0]); gvh[:, off:off + v.shape[1]] = v
    gvh[:, 70:86] = pm(inp['g_final'])
    gvh[:, 86:102] = pm(np.asarray(inp['b_ple_gate'])[0])
    lruch = np.zeros((128, 8, 8), np.float32)
    for k_ in range(4): lruch[:, :, k_] = pm(np.asarray(inp['w_conv'])[0, k_])
    for j_, key in enumerate(['b_conv', 'b_rg', 'b_ig', 'lru_lambda']): lruch[:, :, 4 + j_] = pm(np.asarray(inp[key])[0])
    past_len = inp['page_table'].shape[1] * inp['cache_latent'].shape[2]
    pos_s = past_len + np.repeat(np.arange(4), 16)
    in_maps = []
    for c in range(8):
        b, half = c // 2, c % 2
        own = slice(half * 1024, half * 1024 + 1024)
        ss = slice(16 * c, 16 * c + 16)
        tm = lambda a: np.ascontiguousarray(np.swapaxes(a, 0, 1).reshape((64,) + a.shape[2:]))
        pos = np.concatenate([np.arange(0, 1024), np.arange(half * 1024, half * 1024 + 1024), pos_s])
        rc, rs = _rope_tables(pos)
        m = dict(shared)
        m.update(
            xo=f(x_prompt[b, own]), xp=f(x_prompt[b, 0:1024]) if half else np.zeros((1024, D), np.float32),
            xs=f(tm(x_sample[ss])), po=f(inp['p_prompt'][0, b, own]), psm=f(tm(np.asarray(inp['p_sample'])[0, ss])),
            ptT=np.ascontiguousarray(np.asarray(inp['page_table'])[ss].T.astype(np.int32)),
            slT=f(np.asarray(inp['state_lru'])[0, ss].T), scT=f(np.asarray(inp['state_conv'])[0, ss].transpose(2, 1, 0).reshape(1024, 48)),
            gvh=gvh, lruch=lruch,
            flags=np.tile(np.array([[1.0 if half else 0.0, 0.0 if half else NEG]], np.float32), (128, 1)),
            ropeC=rc, ropeS=rs,
        )
        in_maps.append(m)
    return in_maps


def assemble(res):
    B, S, DB, T = 4, 2048, 128, 4
    y_p = np.zeros((B, S, D), np.float32); y_s = np.zeros((DB, T, D), np.float32)
    nl_p = np.zeros((1, B, S, 256), np.float32); nk_p = np.zeros((1, B, S, 64), np.float32)
    nlru_p = np.zeros((1, B, 1024), np.float32); nconv_p = np.zeros((1, B, 3, 1024), np.float32)
    nl_s = np.zeros((1, DB, T, 256), np.float32); nk_s = np.zeros((1, DB, T, 64), np.float32)
    nlru_s = np.zeros((1, DB, 1024), np.float32); nconv_s = np.zeros((1, DB, 3, 1024), np.float32)
    utm = lambda a: np.swapaxes(a.reshape((4, 16) + a.shape[1:]), 0, 1)
    for c in range(8):
        b, half = c // 2, c % 2
        own = slice(half * 1024, half * 1024 + 1024); ss = slice(16 * c, 16 * c + 16)
        r = res[c]
        y_p[b, own] = r['y_o']; y_s[ss] = utm(r['y_s'])
        nl_p[0, b, own] = r['lat_o']; nk_p[0, b, own] = r['kr_o']
        if half:
            nlru_p[0, b] = r['lru_o'].T.reshape(1024); nconv_p[0, b] = r['conv_o'].reshape(128, 8, 3).transpose(2, 1, 0).reshape(3, 1024)
        nl_s[0, ss] = utm(r['lat_s']); nk_s[0, ss] = utm(r['kr_s'])
        nlru_s[0, ss] = r['lru_s'].T
        nconv_s[0, ss] = r['conv_s'].reshape(1024, 3, 16).transpose(2, 1, 0)
    return (y_p, y_s, nl_p, nk_p, nlru_p, nconv_p, nl_s, nk_s, nlru_s, nconv_s)


def kernel(**inp):
    nc = build_nc(int(np.asarray(inp['cache_latent']).shape[1]))
    in_maps = make_in_maps(inp)
    names = set(DECLARED)
    in_maps = [{k: v for k, v in m.items() if k in names} for m in in_maps]
    res = run_bass_kernel_spmd(nc, in_maps, core_ids=list(range(8))).results
    return assemble(res)
```

```python
import numpy as np
from contextlib import ExitStack
import concourse.bass as bass
import concourse.mybir as mybir
from concourse.bass_utils import run_bass_kernel_spmd

F32 = mybir.dt.float32
BF16 = mybir.dt.bfloat16
I32 = mybir.dt.int32
AF = mybir.ActivationFunctionType
ALU = mybir.AluOpType
AX = mybir.AxisListType

import os
DBG = int(os.environ.get('KDBG', '0'))
STAGE = int(os.environ.get('KSTAGE', '3'))

D = 2048; NO = 1024; NPF = 1024; NS = 64; NT = NO + NS
NKEY = 2048 + NS
EPS = 1e-6
NEG = -1e30
SM_SCALE = 192.0 ** -0.5
ENG = ['pe', 'act', 'dve', 'pool', 'sp']
DECLARED = []


class Prog:
    def __init__(self, nc, es):
        self.nc = nc; self.es = es
        self.sem = {e: es.enter_context(nc.semaphore('sem_' + e)) for e in ENG}
        self.lanes = {}
        self.seq = {e: 0 for e in ENG}
        self.lcnt = {}
        self.waited = {e: {} for e in ENG}
        self.reset()
        self.barrier_vals = None

    def reset(self):
        self.ops = []; self.last_w = {}; self.readers = {}

    def lane(self, name):
        if name not in self.lanes:
            self.lanes[name] = self.es.enter_context(self.nc.semaphore('ln_' + name))
            self.lcnt[name] = 0
        return self.lanes[name]

    def op(self, eng, fn, r=(), w=(), lane=None):
        idx = len(self.ops)
        w = list(w) + [x for x in r if x.startswith('ps') and x[2:].isdigit() and x not in w]
        deps = set()
        for x in r:
            if x in self.last_w: deps.add(self.last_w[x])
        for x in w:
            if x in self.last_w: deps.add(self.last_w[x])
            deps.update(self.readers.get(x, ()))
        for x in w:
            self.last_w[x] = idx; self.readers[x] = []
        for x in r:
            self.readers.setdefault(x, []).append(idx)
        dv = {}
        for d in deps:
            p = self.ops[d]
            dv[d] = self.lcnt[p['lane']] if p['lane'] is not None else p['val']
        o = dict(eng=eng, fn=fn, deps=dv, lane=lane)
        if lane is None:
            self.seq[eng] += 1; o['sem'] = self.sem[eng]; o['val'] = self.seq[eng]; o['inc'] = 1
        else:
            s = self.lane(lane); self.lcnt[lane] += 16
            o['sem'] = s; o['val'] = self.lcnt[lane]; o['inc'] = 16
        self.ops.append(o)

    def emit(self, final=False):
        nc = self.nc
        bar = self.barrier_vals
        ops = self.ops
        with nc.Block() as block:
            decos = dict(pe=block.tensor, act=block.scalar, dve=block.vector, pool=block.gpsimd, sp=block.sync)
            for e in ENG:
                def body(eh, e=e):
                    wd = self.waited[e]
                    def wait(sem, val):
                        if wd.get(id(sem), 0) >= val: return
                        eh.wait_ge(sem, val); wd[id(sem)] = val
                    if bar is not None:
                        for sem, val in bar:
                            if val > 0: wait(sem, val)
                    for o in ops:
                        if o['eng'] != e: continue
                        for d in sorted(o['deps']):
                            p = ops[d]
                            if p['eng'] == 'pe' and e == 'pe' and p['lane'] is None: continue
                            wait(p['sem'], o['deps'][d])
                        ins = o['fn'](eh)
                        ins.then_inc(o['sem'], o['inc'])
                    if final and e == 'sp':
                        for en in ENG: wait(self.sem[en], self.seq[en])
                        for ln, s in self.lanes.items(): wait(s, self.lcnt[ln])
                decos[e](body)
        self.barrier_vals = [(self.sem[en], self.seq[en]) for en in ENG] + \
                            [(s, self.lcnt[ln]) for ln, s in self.lanes.items()]
        self.reset()


def build_nc(npool=20480):
    nc = bass.Bass("TRN2", target_bir_lowering=False)
    es = ExitStack()
    DECLARED.clear()
    def din(name, shape, dt=F32, stage=1):
        if STAGE < stage: return None
        DECLARED.append(name)
        return nc.dram_tensor(name, list(shape), dt, kind="ExternalInput").ap()
    def dout(name, shape, dt=F32): return nc.dram_tensor(name, list(shape), dt, kind="ExternalOutput").ap()
    xo = din('xo', [NO, D]); xp = din('xp', [NPF, D]); xs = din('xs', [NS, D])
    po = din('po', [NO, 256], stage=3); psm = din('psm', [NS, 256], stage=3)
    cl = din('cl', [npool, 128, 256], stage=2); ck = din('ck', [npool, 128, 64], stage=2); ptT = din('ptT', [128, 16], I32, stage=2)
    slT = din('slT', [1024, 16]); scT = din('scT', [1024, 48])
    flags = din('flags', [128, 2])
    gvh = din('gvh', [128, 104]); lruch = din('lruch', [128, 8, 8])
    ropeC = din('ropeC', [64, NKEY]); ropeS = din('ropeS', [64, NKEY])
    tri = din('tri', [128, 128], stage=2); smask = din('smask', [32, 4], stage=2)
    g_mix = din('g_mix', [D], stage=99); w_in = din('w_in', [D, 2880]); wkrp = din('wkrp', [D, 64])
    g_q = din('g_q', [512], stage=99); w_uq = din('w_uq', [512, 1536], stage=2); wqrp = din('wqrp', [512, 8, 64], stage=2)
    g_kv = din('g_kv', [256], stage=99); wukT = din('wukT', [8, 128, 256], stage=2); wuv = din('wuv', [8, 256, 128], stage=2)
    w_conv = din('w_conv', [4, 1024], stage=99); b_conv = din('b_conv', [1024], stage=99)
    w_rg = din('w_rg', [8, 128, 128]); b_rg = din('b_rg', [1024], stage=99); w_ig = din('w_ig', [8, 128, 128]); b_ig = din('b_ig', [1024], stage=99)
    lam = din('lam', [1024], stage=99); g_om = din('g_om', [1024], stage=99); g_ol = din('g_ol', [1024], stage=99)
    w_o = din('w_o', [D, D], stage=3); g_ffn = din('g_ffn', [D], stage=99)
    w_group = din('w_group', [D, 4], stage=3); b_group = din('b_group', [4], stage=3); w_router = din('w_router', [D, 32], stage=3); b_router = din('b_router', [32], stage=3)
    w_gate = din('w_gate', [32, D, 512], stage=3); w_up = din('w_up', [32, D, 512], stage=3); w_down = din('w_down', [32, 512, D], stage=3)
    g_ple = din('g_ple', [D], stage=99); w_pg = din('w_pg', [D, D], stage=3); b_pg = din('b_pg', [D], stage=3); w_pp = din('w_pp', [256, D], stage=3)
    g_fin = din('g_fin', [D], stage=99)
    y_o = dout('y_o', [NO, D]); y_s = dout('y_s', [NS, D])
    lat_o = dout('lat_o', [NO, 256]); kr_o = dout('kr_o', [NO, 64])
    lru_o = dout('lru_o', [128, 8]); conv_o = dout('conv_o', [128, 24])
    lat_s = dout('lat_s', [NS, 256]); kr_s = dout('kr_s', [NS, 64])
    lru_s = dout('lru_s', [1024, 16]); conv_s = dout('conv_s', [1024, 48])

    P = Prog(nc, es)
    def sb(name, shape, dt=F32, st=None): return (st or es).enter_context(nc.sbuf_tensor(name, list(shape), dt))
    hT = sb('hT', [128, 16, NT])
    mixT = sb('mixT', [128, 16, NT], BF16)
    identf = sb('identf', [128, 128]); identb = sb('identb', [128, 128], BF16)
    onesb = sb('onesb', [128, 128], BF16)
    epsT = sb('epsT', [128, 1]); oneT = sb('oneT', [128, 1])
    gv = sb('gv', [128, 88])
    GV_MIX, GV_Q, GV_KV, GV_OM, GV_OL, GV_FFN, GV_PLE, GV_BPG = 0, 16, 20, 22, 30, 38, 54, 70
    gv2 = sb('gv2', [128, 16])
    lruc = sb('lruc', [128, 8, 12])
    flg = sb('flg', [128, 2])
    hist = sb('hist', [128, 8, 3]); state = sb('state', [128, 8, 1])
    es_mid = ExitStack()
    cqT = sb('cqT', [128, 4, NT], BF16, es_mid)
    latT = sb('latT', [128, 2, NKEY], BF16, es_mid)
    krT = sb('krT', [64, NKEY], BF16, es_mid)
    lat_tok = sb('lat_tok', [128, 17, 260], BF16, es_mid)
    psall = es.enter_context(nc.psum_tensor('psall', [128, 8, 512], F32))
    ps = [psall[:, i, :] for i in range(8)]

    def dma(eng, lane, out, in_, r=(), w=()):
        P.op(eng, lambda e: e.dma_start(out=out, in_=in_), r=r, w=w, lane=lane)

    P.op('pool', lambda e: e.memset(identf[:], 0.0), w=['identf'])
    def c_ident(e):
        return e.affine_select(out=identf[:], in_=identf[:], pattern=[[-1, 128]], compare_op=ALU.not_equal,
                               fill=1.0, base=0, channel_multiplier=1)
    P.op('pool', c_ident, r=['identf'], w=['identf'])
    P.op('pool', lambda e: e.tensor_copy(out=identb[:], in_=identf[:]), r=['identf'], w=['identb'])
    P.op('pool', lambda e: e.memset(onesb[:], 1.0), w=['onesb'])
    P.op('pool', lambda e: e.memset(epsT[:], EPS), w=['epsT'])
    P.op('pool', lambda e: e.memset(oneT[:], 1.0), w=['oneT'])
    P.op('pool', lambda e: e.memset(lat_tok[:, :, 256:257], 1.0), w=['lat_tok_ones'])
    P.op('pool', lambda e: e.memset(hist[:], 0.0), w=['hist'])
    P.op('pool', lambda e: e.memset(state[:], 0.0), w=['state'])
    small = nc.allow_non_contiguous_dma(reason="small strided constant loads")
    es.enter_context(small)
    dma('sp', 'c0', gv[:, 0:70], gvh[:, 0:70], w=['gv'])
    dma('sp', 'c0', gv2[:, 0:16], gvh[:, 70:86], w=['gv'])
    dma('sp', 'c0', gv[:, 70:86], gvh[:, 86:102], w=['gv'])
    dma('sp', 'c0b', lruc[:, :, 0:8], lruch, w=['lruc'])
    dma('sp', 'c0', flg[:], flags, w=['flg'])
    P.op('act', lambda e: e.activation(out=lruc[:, :, 7:8], in_=lruc[:, :, 7:8], func=AF.Exp, scale=-1.0), r=['lruc'], w=['lruc'])
    P.op('act', lambda e: e.activation(out=lruc[:, :, 7:8], in_=lruc[:, :, 7:8], func=AF.Ln, bias=oneT[:], scale=1.0), r=['lruc', 'oneT'], w=['lruc'])
    P.op('dve', lambda e: e.tensor_scalar(out=lruc[:, :, 8:9], in0=lruc[:, :, 7:8], scalar1=-16.0, scalar2=None, op0=ALU.mult), r=['lruc'], w=['lruc'])
    P.op('dve', lambda e: e.tensor_scalar(out=lruc[:, :, 7:8], in0=lruc[:, :, 7:8], scalar1=-8.0, scalar2=None, op0=ALU.mult), r=['lruc'], w=['lruc'])
    P.emit(final=(DBG == 1))
    if DBG == 1:
        es.close(); return nc

    with ExitStack() as st1:
        xst = sb('xst', [128, D], F32, st1)
        xnb = sb('xnb', [128, D], BF16, st1)
        ssq = sb('ssq', [128, 2], F32, st1)
        xnT = sb('xnT', [128, 16, 256], BF16, st1)
        wst = sb('wst', [128, 4, 16, 128], BF16, st1)
        ropeT = sb('ropeT', [64, 2, 256], F32, st1)
        wg = sb('wg', [128, 2, 8, 128], BF16, st1)
        ckv_f = sb('ckv_f', [128, 2, 256], F32, st1)
        sqb = sb('sqb', [128, 256], BF16, st1)
        rstd_b = sb('rstd_b', [128, 256], F32, st1)
        kr_f = sb('kr_f', [64, 2, 256], F32, st1)
        xbes = [sb('xbe%d' % i, [128, 304], F32, st1) for i in range(2)]
        T = [[sb('lt%d_%d' % (j_, i), [128, 256], F32, st1) for i in range(5)] for j_ in range(2)]
        xcbs = [sb('xcb%d' % i, [128, 256], BF16, st1) for i in range(2)]
        stg = sb('stg', [128, 2, 320], F32, st1)
        shist = sb('shist', [128, 8, 48], F32, st1); sstate = sb('sstate', [128, 8, 16], F32, st1)

        dma('pool', 'c1p', wg[:, 0], w_rg.rearrange("n k j -> k n j"), w=['wg'])
        dma('pool', 'c1p', wg[:, 1], w_ig.rearrange("n k j -> k n j"), w=['wg'])
        dma('sp', 'shist', shist[:], scT.rearrange("(c p) k -> p c k", p=128), w=['shist'])
        dma('sp', 'sstate', sstate[:], slT.rearrange("(c p) k -> p c k", p=128), w=['sstate'])

        tiles = [('P', c_, 256) for c_ in range(0, 1024, 256)] + [('O', c_, 256) for c_ in range(0, 1024, 256)] + [('S', 0, 64)]
        wcnt = [0]; scnt = [0]; stgc = [0]
        def do_tile(kind, c0, n):
            src = dict(P=xp, O=xo, S=xs)[kind]
            pcol = dict(P=c0, O=NPF + c0, S=2048)[kind]
            hcol = dict(P=None, O=c0, S=NO)[kind]
            full = kind != 'P'
            nb = (n + 127) // 128
            for i in range(nb):
                rows = min(128, n - i * 128)
                dma('sp', 'xst', xst[0:rows, :], src[c0 + i * 128:c0 + i * 128 + rows, :], w=['xst'])
                P.op('act', lambda e, rows=rows: e.activation(out=xnb[0:rows, :], in_=xst[0:rows, :], func=AF.Square, accum_out=ssq[0:rows, 0:1]),
                     r=['xst'], w=['xnb', 'ssq'])
                P.op('act', lambda e, rows=rows: e.activation(out=ssq[0:rows, 1:2], in_=ssq[0:rows, 0:1], func=AF.Sqrt, bias=epsT[0:rows, :], scale=1.0 / D),
                     r=['ssq', 'epsT'], w=['ssq'])
                P.op('dve', lambda e, rows=rows: e.reciprocal(out=ssq[0:rows, 1:2], in_=ssq[0:rows, 1:2]), r=['ssq'], w=['ssq'])
                P.op('act', lambda e, rows=rows: e.activation(out=xnb[0:rows, :], in_=xst[0:rows, :], func=AF.Copy, scale=ssq[0:rows, 1:2]),
                     r=['xst', 'ssq'], w=['xnb'])
                for hb in range(2):
                    bank = ps[hb]
                    pv = bank[:].bitcast(BF16)
                    def tr(e, hb=hb, rows=rows, pv=pv):
                        ins = None
                        for k in range(8):
                            ins = e.transpose(pv[:, k * 128:k * 128 + rows], xnb[0:rows, (hb * 8 + k) * 128:(hb * 8 + k + 1) * 128], identb[0:rows, 0:rows])
                        return ins
                    P.op('pe', tr, r=['xnb', 'identb'], w=['ps%d' % hb])
                    def ev(e, hb=hb, rows=rows, pv=pv, i=i):
                        return e.tensor_tensor(out=xnT[:, hb * 8:hb * 8 + 8, i * 128:i * 128 + rows],
                                               in0=pv[:, 0:1024].rearrange("p (k t) -> p k t", k=8)[:, :, 0:rows],
                                               in1=gv[:, GV_MIX + hb * 8:GV_MIX + hb * 8 + 8].unsqueeze(2).to_broadcast([128, 8, rows]), op=ALU.mult)
                    P.op('dve', ev, r=['ps%d' % hb, 'gv'], w=['xnT'])
                if full:
                    for q4 in range(4):
                        bank = ps[2 + (q4 % 2)]
                        def trf(e, q4=q4, rows=rows, bank=bank):
                            ins = None
                            for k in range(4):
                                ch = q4 * 4 + k
                                ins = e.transpose(bank[:, k * 128:k * 128 + rows], xst[0:rows, ch * 128:(ch + 1) * 128], identf[0:rows, 0:rows])
                            return ins
                        P.op('pe', trf, r=['xst', 'identf'], w=['ps%d' % (2 + q4 % 2)])
                        def evf(e, q4=q4, rows=rows, bank=bank, i=i):
                            return e.activation(out=hT[:, q4 * 4:q4 * 4 + 4, hcol + i * 128:hcol + i * 128 + rows],
                                                in_=bank[:, 0:512].rearrange("p (k t) -> p k t", k=4)[:, :, 0:rows], func=AF.Copy)
                        P.op('act', evf, r=['ps%d' % (2 + q4 % 2)], w=['hT'])

            if DBG == 2: return
            def proj(col0, ncol, dst_bank, wsrc=None):
                b = wcnt[0] % 4; wcnt[0] += 1
                wt = wst[:, b, :, 0:ncol]; lane = 'wst%d' % b; res = 'wst%d' % b
                srcw = (wsrc if wsrc is not None else w_in[:, col0:col0 + ncol]).rearrange("(k p) c -> p k c", p=128)
                dma('pool', lane, wt, srcw, w=[res])
                def mm(e, wt=wt, ncol=ncol, dst_bank=dst_bank):
                    ins = None
                    for k in range(16):
                        ins = e.matmul(ps[dst_bank][0:ncol, 0:n], lhsT=wt[:, k, :], rhs=xnT[:, k, 0:n], start=(k == 0), stop=(k == 15))
                    return ins
                P.op('pe', mm, r=[res, 'xnT'], w=['ps%d' % dst_bank])

            def fm_norm(srcs, nfeat, gcol, outs, tag):
                tags = list(tag) if isinstance(tag, (list, tuple)) else [tag]
                for j, s_ in enumerate(srcs):
                    P.op('act', lambda e, s_=s_: e.activation(out=sqb[:, 0:n], in_=s_, func=AF.Square), r=tags, w=['sqb'])
                    P.op('pe', lambda e, j=j: e.matmul(ps[7][:, 0:n], lhsT=onesb[:], rhs=sqb[:, 0:n], start=(j == 0), stop=(j == len(srcs) - 1)),
                         r=['sqb', 'onesb'], w=['ps7'])
                P.op('act', lambda e: e.activation(out=rstd_b[:, 0:n], in_=ps[7][:, 0:n], func=AF.Sqrt, bias=epsT[:], scale=1.0 / nfeat),
                     r=['ps7', 'epsT'], w=['rstd_b'])
                P.op('dve', lambda e: e.reciprocal(out=rstd_b[:, 0:n], in_=rstd_b[:, 0:n]), r=['rstd_b'], w=['rstd_b'])
                for j, s_ in enumerate(srcs):
                    for (o_, ores) in outs[j]:
                        P.op('dve', lambda e, s_=s_, o_=o_, j=j: e.scalar_tensor_tensor(out=o_, in0=s_, scalar=gv[:, gcol + j:gcol + j + 1], in1=rstd_b[:, 0:n],
                                                                                       op0=ALU.mult, op1=ALU.mult), r=tags + ['gv', 'rstd_b'], w=[ores])

            if full:
                for j in range(4):
                    proj(j * 128, 128, 4 + j % 2)
                    P.op('act', lambda e, j=j: e.activation(out=T[0][j][:, 0:n], in_=ps[4 + j % 2][:, 0:n], func=AF.Copy), r=['ps%d' % (4 + j % 2)], w=['cqf', 'xc0', 'rr0', 'ii0', 'aa0'])
                fm_norm([T[0][j][:, 0:n] for j in range(4)], 512, GV_Q, [[(cqT[:, j, hcol:hcol + n], 'cqT')] for j in range(4)], ['cqf', 'xc0', 'rr0', 'ii0', 'aa0'])
            if DBG == 3: return
            for j in range(2):
                proj(512 + j * 128, 128, 4 + j % 2)
                P.op('act', lambda e, j=j: e.activation(out=ckv_f[:, j, 0:n], in_=ps[4 + j % 2][:, 0:n], func=AF.Copy), r=['ps%d' % (4 + j % 2)], w=['ckv'])
            fm_norm([ckv_f[:, j, 0:n] for j in range(2)], 256, GV_KV, [[(ckv_f[:, j, 0:n], 'ckv')] for j in range(2)], 'ckv')
            for j in range(2):
                P.op('dve', lambda e, j=j: e.tensor_copy(out=latT[:, j, pcol:pcol + n], in_=ckv_f[:, j, 0:n]), r=['ckv'], w=['latT'])
            if DBG == 4: return
            dma('sp', 'ropeT', ropeT[:, 0, 0:n], ropeC[:, pcol:pcol + n], w=['rope'])
            dma('sp', 'ropeT', ropeT[:, 1, 0:n], ropeS[:, pcol:pcol + n], w=['rope'])
            proj(768, 64, 4)
            P.op('act', lambda e: e.activation(out=kr_f[:, 0, 0:n], in_=ps[4][0:64, 0:n], func=AF.Copy), r=['ps4'], w=['krf0'])
            proj(0, 64, 5, wsrc=wkrp)
            P.op('dve', lambda e: e.tensor_tensor(out=kr_f[:, 1, 0:n], in0=ps[5][0:64, 0:n], in1=ropeT[:, 1, 0:n], op=ALU.mult), r=['ps5', 'rope'], w=['krf1'])
            P.op('dve', lambda e: e.tensor_tensor(out=kr_f[:, 0, 0:n], in0=kr_f[:, 0, 0:n], in1=ropeT[:, 0, 0:n], op=ALU.mult), r=['krf0', 'rope'], w=['krf0'])
            P.op('dve', lambda e: e.tensor_tensor(out=kr_f[:, 0, 0:n], in0=kr_f[:, 0, 0:n], in1=kr_f[:, 1, 0:n], op=ALU.add), r=['krf0', 'krf1'], w=['krf0'])
            P.op('dve', lambda e: e.tensor_copy(out=krT[:, pcol:pcol + n], in_=kr_f[:, 0, 0:n]), r=['krf0'], w=['krT'])
            if DBG == 5: return
            for i in range(nb):
                rows = min(128, n - i * 128)
                kb = (pcol + i * 128) // 128
                def trl(e, i=i, rows=rows):
                    e.transpose(ps[6][0:rows, 0:128], ckv_f[:, 0, i * 128:i * 128 + rows], identf[:])
                    e.transpose(ps[6][0:rows, 128:256], ckv_f[:, 1, i * 128:i * 128 + rows], identf[:])
                    return e.transpose(ps[6][0:rows, 256:320], kr_f[:, 0, i * 128:i * 128 + rows], identf[0:64, 0:64])
                P.op('pe', trl, r=['ckv', 'krf0', 'identf'], w=['ps6'])
                P.op('dve', lambda e, rows=rows, kb=kb: e.tensor_copy(out=lat_tok[0:rows, kb, 0:256], in_=ps[6][0:rows, 0:256]), r=['ps6'], w=['lat_tok'])
                if full and DBG != 7:
                    sb_ = stgc[0] % 2; stgc[0] += 1
                    P.op('act', lambda e, rows=rows, sb_=sb_: e.activation(out=stg[0:rows, sb_, :], in_=ps[6][0:rows, 0:320], func=AF.Copy), r=['ps6'], w=['stg%d' % sb_])
                    lo, ko = (lat_o, kr_o) if kind == 'O' else (lat_s, kr_s)
                    r0 = c0 + i * 128
                    if DBG != 8:
                        dma('sp', 'stg%d' % sb_, lo[r0:r0 + rows, :], stg[0:rows, sb_, 0:256], r=['stg%d' % sb_])
                    if DBG not in (8, 9):
                        dma('sp', 'stg%d' % sb_, ko[r0:r0 + rows, :], stg[0:rows, sb_, 256:320], r=['stg%d' % sb_])

            if DBG in (6, 7, 8, 9): return
            H = 48 if kind == 'S' else 3
            sh = 16 if kind == 'S' else 1
            if kind == 'O' and c0 == 0:
                P.op('dve', lambda e: e.tensor_scalar(out=hist[:], in0=hist[:], scalar1=flg[:, 0:1], scalar2=None, op0=ALU.mult), r=['hist', 'flg'], w=['hist'])
                P.op('dve', lambda e: e.tensor_scalar(out=state[:], in0=state[:], scalar1=flg[:, 0:1], scalar2=None, op0=ALU.mult), r=['state', 'flg'], w=['state'])
            def do_chunk(c):
                pp = c % 2
                xc, rr, ii, aa, uu = T[pp]; yy = aa; xbe = xbes[pp]; xcb = xcbs[pp]
                proj(832 + c * 128, 128, 4)
                hsrc = shist[:, c, :] if kind == 'S' else hist[:, c, :]
                P.op('dve', lambda e, hsrc=hsrc: e.tensor_copy(out=xbe[:, 0:H], in_=hsrc), r=['hist', 'shist'], w=['xbe%d' % pp])
                P.op('act', lambda e: e.activation(out=xbe[:, H:H + n], in_=ps[4][:, 0:n], func=AF.Copy), r=['ps4'], w=['xbe%d' % pp])
                P.op('dve', lambda e, c=c: e.tensor_scalar(out=xc[:, 0:n], in0=xbe[:, 0:n], scalar1=lruc[:, c, 0:1], scalar2=lruc[:, c, 4:5], op0=ALU.mult, op1=ALU.add),
                     r=['xbe%d' % pp, 'lruc'], w=['xc%d' % pp])
                for k in range(1, 4):
                    P.op('dve', lambda e, c=c, k=k: e.scalar_tensor_tensor(out=xc[:, 0:n], in0=xbe[:, k * sh:k * sh + n], scalar=lruc[:, c, k:k + 1], in1=xc[:, 0:n],
                                                                         op0=ALU.mult, op1=ALU.add), r=['xbe%d' % pp, 'lruc', 'xc%d' % pp], w=['xc%d' % pp])
                if kind != 'S':
                    P.op('dve', lambda e, c=c: e.tensor_copy(out=hist[:, c, :], in_=xbe[:, n:n + 3]), r=['xbe%d' % pp], w=['hist'])
                else:
                    P.op('dve', lambda e, c=c: e.tensor_copy(out=shist[:, c, :], in_=xbe[:, 64:112]), r=['xbe%d' % pp], w=['shist'])
                P.op('dve', lambda e: e.tensor_copy(out=xcb[:, 0:n], in_=xc[:, 0:n]), r=['xc%d' % pp], w=['xcb%d' % pp])
                P.op('pe', lambda e, c=c: e.matmul(ps[5][:, 0:n], lhsT=wg[:, 0, c, :], rhs=xcb[:, 0:n], start=True, stop=True), r=['wg', 'xcb%d' % pp], w=['ps5'])
                P.op('pe', lambda e, c=c: e.matmul(ps[6][:, 0:n], lhsT=wg[:, 1, c, :], rhs=xcb[:, 0:n], start=True, stop=True), r=['wg', 'xcb%d' % pp], w=['ps6'])
                P.op('act', lambda e, c=c: e.activation(out=rr[:, 0:n], in_=ps[5][:, 0:n], func=AF.Sigmoid, bias=lruc[:, c, 5:6], scale=1.0), r=['ps5', 'lruc'], w=['rr%d' % pp])
                P.op('act', lambda e, c=c: e.activation(out=ii[:, 0:n], in_=ps[6][:, 0:n], func=AF.Sigmoid, bias=lruc[:, c, 6:7], scale=1.0), r=['ps6', 'lruc'], w=['ii%d' % pp])
                P.op('act', lambda e, c=c: e.activation(out=aa[:, 0:n], in_=rr[:, 0:n], func=AF.Exp, scale=lruc[:, c, 7:8]), r=['rr%d' % pp, 'lruc'], w=['aa%d' % pp])
                P.op('act', lambda e, c=c: e.activation(out=rr[:, 0:n], in_=rr[:, 0:n], func=AF.Exp, scale=lruc[:, c, 8:9]), r=['rr%d' % pp, 'lruc'], w=['rr%d' % pp])
                P.op('act', lambda e: e.activation(out=rr[:, 0:n], in_=rr[:, 0:n], func=AF.Sqrt, bias=oneT[:], scale=-1.0), r=['rr%d' % pp, 'oneT'], w=['rr%d' % pp])
                P.op('dve', lambda e: e.tensor_tensor(out=uu[:, 0:n], in0=ii[:, 0:n], in1=xc[:, 0:n], op=ALU.mult), r=['ii%d' % pp, 'xc%d' % pp], w=['uu%d' % pp])
                P.op('dve', lambda e: e.tensor_tensor(out=uu[:, 0:n], in0=uu[:, 0:n], in1=rr[:, 0:n], op=ALU.mult), r=['uu%d' % pp, 'rr%d' % pp], w=['uu%d' % pp])
                if kind != 'S':
                    P.op('dve', lambda e, c=c: e.tensor_tensor_scan(out=uu[:, 0:n], data0=aa[:, 0:n], data1=uu[:, 0:n], initial=state[:, c, :], op0=ALU.mult, op1=ALU.add),
                         r=['aa%d' % pp, 'uu%d' % pp, 'state'], w=['uu%d' % pp])
                    P.op('dve', lambda e, c=c: e.tensor_copy(out=state[:, c, :], in_=uu[:, n - 1:n]), r=['uu%d' % pp], w=['state'])
                else:
                    for t in range(4):
                        prev = sstate[:, c, :] if t == 0 else uu[:, (t - 1) * 16:t * 16]
                        P.op('dve', lambda e, t=t, prev=prev: e.tensor_tensor(out=aa[:, t * 16:t * 16 + 16], in0=aa[:, t * 16:t * 16 + 16], in1=prev, op=ALU.mult),
                             r=['aa%d' % pp, 'uu%d' % pp, 'sstate'], w=['aa%d' % pp])
                        P.op('dve', lambda e, t=t: e.tensor_tensor(out=uu[:, t * 16:t * 16 + 16], in0=uu[:, t * 16:t * 16 + 16], in1=aa[:, t * 16:t * 16 + 16], op=ALU.add),
                             r=['aa%d' % pp, 'uu%d' % pp], w=['uu%d' % pp])
                    P.op('dve', lambda e, c=c: e.tensor_copy(out=sstate[:, c, :], in_=uu[:, 48:64]), r=['uu%d' % pp], w=['sstate'])
                if full:
                    proj(1856 + c * 128, 128, 7)
                    P.op('act', lambda e: e.activation(out=yy[:, 0:n], in_=ps[7][:, 0:n], func=AF.Copy), r=['ps7'], w=['aa%d' % pp])
                    P.op('act', lambda e: e.activation(out=ii[:, 0:n], in_=ps[7][:, 0:n], func=AF.Square), r=['ps7'], w=['ii%d' % pp])
                    P.op('dve', lambda e: e.tensor_scalar(out=ii[:, 0:n], in0=ii[:, 0:n], scalar1=0.044715, scalar2=1.0, op0=ALU.mult, op1=ALU.add), r=['ii%d' % pp], w=['ii%d' % pp])
                    P.op('dve', lambda e: e.tensor_tensor(out=ii[:, 0:n], in0=ii[:, 0:n], in1=yy[:, 0:n], op=ALU.mult), r=['ii%d' % pp, 'aa%d' % pp], w=['ii%d' % pp])
                    P.op('act', lambda e: e.activation(out=ii[:, 0:n], in_=ii[:, 0:n], func=AF.Sigmoid, scale=1.5957691216057308), r=['ii%d' % pp], w=['ii%d' % pp])
                    P.op('dve', lambda e: e.tensor_tensor(out=ii[:, 0:n], in0=ii[:, 0:n], in1=yy[:, 0:n], op=ALU.mult), r=['ii%d' % pp, 'aa%d' % pp], w=['ii%d' % pp])
                    P.op('dve', lambda e, c=c: e.tensor_tensor(out=mixT[:, 8 + c, hcol:hcol + n], in0=ii[:, 0:n], in1=uu[:, 0:n], op=ALU.mult), r=['ii%d' % pp, 'uu%d' % pp], w=['mixT'])
            for c_ in range(8):
                do_chunk(c_)
        for (kind_, c0_, n_) in tiles:
            do_tile(kind_, c0_, n_)
        dma('sp', 'o1', lru_o, state[:].rearrange("p c o -> p (c o)"), r=['state'])
        dma('sp', 'o2', conv_o, hist[:].rearrange("p c k -> p (c k)"), r=['hist'])
        dma('sp', 'o3', lru_s.rearrange("(c p) k -> p c k", p=128), sstate[:], r=['sstate'])
        dma('sp', 'o4', conv_s.rearrange("(c p) k -> p c k", p=128), shist[:], r=['shist'])
        P.emit(final=(STAGE == 1))
    if STAGE == 1:
        es_mid.close(); es.close(); return nc

    Sv = psall[:, 0:4, :].rearrange("p b c -> p (b c)")
    with ExitStack() as st2:
        wuvs = sb('wuvs', [128, 8, 2, 128], BF16, st2); smk = sb('smk', [32, 4], F32, st2)
        pts = sb('pts', [128, 16], I32, st2)
        qlS = sb('qlS', [128, 8, 2, 64], BF16, st2); qrSs = sb('qrSs', [64, 8, 64], BF16, st2)
        OTS = sb('OTS', [128, 2, 16, 32], BF16, st2)
        st2a = ExitStack()
        wqn = sb('wqn', [128, 4, 8, 128], BF16, st2a); wqr = sb('wqr', [128, 4, 8, 64], BF16, st2a); wqp = sb('wqp', [128, 4, 8, 64], BF16, st2a)
        wuk = sb('wuk', [128, 8, 256], BF16, st2a)
        trit = sb('trit', [128, 128], F32, st2a)
        qn = sb('qn', [128, 8, 128], BF16, st2a); qlT = sb('qlT', [128, 8, 2, 128], BF16, st2a); qrT = sb('qrT', [64, 8, 128], BF16, st2a)
        rtab = sb('rtab', [64, 2, 128], F32, st2a); qa = sb('qa', [64, 2, 4, 128], F32, st2a)
        Pm = sb('Pm', [128, 2048], BF16, st2a); PT = sb('PT', [128, 16, 128], BF16, st2a)
        stt = sb('stt', [128, 8], F32, st2a)
        On = sb('On', [128, 256], BF16, st2a); OT = sb('OT', [128, 8, 2, 128], BF16, st2a)
        w_uq3 = w_uq.rearrange("(k p) (h j) -> p k h j", p=128, h=8)
        wqrp3 = wqrp.rearrange("(k p) h j -> p k h j", p=128)
        for k in range(4):
            dma('pool', 'wq', wqn[:, k], w_uq3[:, k, :, 0:128], w=['wq'])
            dma('pool', 'wq', wqr[:, k], w_uq3[:, k, :, 128:192], w=['wq'])
            dma('pool', 'wq', wqp[:, k], wqrp3[:, k], w=['wq'])
        dma('pool', 'wq', wuk[:], wukT.rearrange("h n c -> n h c"), w=['wq'])
        for h in range(8):
            dma('pool', 'wuvs', wuvs[:, h], wuv[h].rearrange("(cc p) v -> p cc v", p=128), w=['wuvs'])
        dma('sp', 'trit', trit[:], tri, w=['trit']); dma('sp', 'smk', smk[:], smask, w=['smk'])

        def qproj(hc, nq, pcol):
            dma('sp', 'rtab', rtab[:, 0, 0:nq], ropeC[:, pcol:pcol + nq], w=['rtab'])
            dma('sp', 'rtab', rtab[:, 1, 0:nq], ropeS[:, pcol:pcol + nq], w=['rtab'])
            for hg in range(2):
                def mmn(e, hg=hg):
                    ins = None
                    for h4 in range(4):
                        for k in range(4):
                            ins = e.matmul(ps[6][:, h4 * 128:h4 * 128 + nq], lhsT=wqn[:, k, hg * 4 + h4, :], rhs=cqT[:, k, hc:hc + nq], start=(k == 0), stop=(k == 3))
                    return ins
                P.op('pe', mmn, r=['wq', 'cqT'], w=['ps6'])
                P.op('act', lambda e, hg=hg: e.activation(out=qn[:, hg * 4:hg * 4 + 4, 0:nq], in_=ps[6].rearrange("p (h t) -> p h t", h=4)[:, :, 0:nq], func=AF.Copy),
                     r=['ps6'], w=['qn'])
            for hp in range(4):
                def mml(e, hp=hp):
                    ins = None
                    for i2 in range(4):
                        h = hp * 2 + i2 // 2; cc = i2 % 2
                        ins = e.matmul(ps[7][:, i2 * 128:i2 * 128 + nq], lhsT=wuk[:, h, cc * 128:(cc + 1) * 128], rhs=qn[:, h, 0:nq], start=True, stop=True)
                    return ins
                P.op('pe', mml, r=['wq', 'qn'], w=['ps7'])
                P.op('dve', lambda e, hp=hp: e.tensor_copy(out=qlT[:, hp * 2:hp * 2 + 2, :, 0:nq],
                                                           in_=ps[7].rearrange("p (h c t) -> p h c t", h=2, c=2)[:, :, :, 0:nq]), r=['ps7'], w=['qlT'])
            for hg in range(2):
                for which, wt, bank in ((0, wqr, 6), (1, wqp, 7)):
                    def mmr(e, hg=hg, wt=wt, bank=bank):
                        ins = None
                        for h4 in range(4):
                            for k in range(4):
                                ins = e.matmul(ps[bank][0:64, h4 * 128:h4 * 128 + nq], lhsT=wt[:, k, hg * 4 + h4, :], rhs=cqT[:, k, hc:hc + nq], start=(k == 0), stop=(k == 3))
                        return ins
                    P.op('pe', mmr, r=['wq', 'cqT'], w=['ps%d' % bank])
                    P.op('dve', lambda e, which=which, bank=bank: e.tensor_tensor(
                        out=qa[:, which, :, 0:nq], in0=ps[bank][0:64, :].rearrange("p (h t) -> p h t", h=4)[:, :, 0:nq],
                        in1=rtab[:, which, 0:nq].unsqueeze(1).to_broadcast([64, 4, nq]), op=ALU.mult), r=['ps%d' % bank, 'rtab'], w=['qa%d' % which])
                P.op('dve', lambda e, hg=hg: e.tensor_tensor(out=qrT[:, hg * 4:hg * 4 + 4, 0:nq], in0=qa[:, 0, :, 0:nq], in1=qa[:, 1, :, 0:nq], op=ALU.add),
                     r=['qa0', 'qa1'], w=['qrT'])

        def do_block(j):
            hc = j * 128
            qproj(hc, 128, NPF + hc)
            nk = NPF + (j + 1) * 128
            nkc = nk // 128
            ngrp = (nk + 511) // 512
            for h in range(8):
                def sc(e, h=h):
                    ins = None
                    for g in range(ngrp):
                        k0 = g * 512; kw = min(512, nk - k0)
                        e.matmul(ps[g][:, 0:kw], lhsT=qlT[:, h, 0, :], rhs=latT[:, 0, k0:k0 + kw], start=True, stop=False)
                        e.matmul(ps[g][:, 0:kw], lhsT=qlT[:, h, 1, :], rhs=latT[:, 1, k0:k0 + kw], start=False, stop=False)
                        ins = e.matmul(ps[g][:, 0:kw], lhsT=qrT[:, h, :], rhs=krT[:, k0:k0 + kw], start=False, stop=True)
                    return ins
                SB = ['ps0', 'ps1', 'ps2', 'ps3']
                P.op('pe', sc, r=['qlT', 'qrT', 'latT', 'krT'], w=SB)
                P.op('dve', lambda e: e.tensor_scalar(out=Sv[:, 0:NPF], in0=Sv[:, 0:NPF], scalar1=flg[:, 1:2], scalar2=None, op0=ALU.add), r=SB + ['flg'], w=SB)
                P.op('dve', lambda e: e.tensor_tensor(out=Sv[:, nk - 128:nk], in0=Sv[:, nk - 128:nk], in1=trit[:], op=ALU.add), r=SB + ['trit'], w=SB)
                P.op('dve', lambda e: e.reduce_max(out=stt[:, 0:1], in_=Sv[:, 0:nk], axis=AX.X), r=SB, w=['stt0'])
                P.op('dve', lambda e: e.tensor_scalar(out=stt[:, 1:2], in0=stt[:, 0:1], scalar1=-SM_SCALE, scalar2=None, op0=ALU.mult), r=['stt0'], w=['stt1'])
                P.op('act', lambda e: e.activation(out=Pm[:, 0:nk], in_=Sv[:, 0:nk], func=AF.Exp, bias=stt[:, 1:2], scale=SM_SCALE), r=SB + ['stt1'], w=['Pm'])
                for half in range(2):
                    c_lo = half * 8; c_hi = min(nkc, c_lo + 8)
                    if c_hi <= c_lo: continue
                    pv4 = ps[4][:].bitcast(BF16)
                    def trp(e, c_lo=c_lo, c_hi=c_hi, pv4=pv4):
                        ins = None
                        for kc in range(c_lo, c_hi):
                            ins = e.transpose(pv4[:, (kc - c_lo) * 128:(kc - c_lo + 1) * 128], Pm[:, kc * 128:(kc + 1) * 128], identb[:])
                        return ins
                    P.op('pe', trp, r=['Pm', 'identb'], w=['ps4'])
                    P.op('dve', lambda e, c_lo=c_lo, c_hi=c_hi, pv4=pv4: e.tensor_copy(
                        out=PT[:, c_lo:c_hi, :], in_=pv4[:, 0:(c_hi - c_lo) * 128].rearrange("p (k t) -> p k t", t=128)), r=['ps4'], w=['PT'])
                def pvm(e):
                    ins = None
                    for kc in range(nkc):
                        ins = e.matmul(ps[5][:, 0:257], lhsT=PT[:, kc, :], rhs=lat_tok[:, kc, 0:257], start=(kc == 0), stop=(kc == nkc - 1))
                    return ins
                P.op('pe', pvm, r=['PT', 'lat_tok', 'lat_tok_ones'], w=['ps5'])
                P.op('dve', lambda e: e.reciprocal(out=stt[:, 2:3], in_=ps[5][:, 256:257]), r=['ps5'], w=['stt2'])
                P.op('act', lambda e: e.activation(out=On[:], in_=ps[5][:, 0:256], func=AF.Copy, scale=stt[:, 2:3]), r=['ps5', 'stt2'], w=['On'])
                pv6 = ps[6][:].bitcast(BF16)
                def tro(e, pv6=pv6):
                    e.transpose(pv6[:, 0:128], On[:, 0:128], identb[:])
                    return e.transpose(pv6[:, 128:256], On[:, 128:256], identb[:])
                P.op('pe', tro, r=['On', 'identb'], w=['ps6'])
                P.op('act', lambda e, h=h, pv6=pv6: e.activation(out=OT[:, h, :, :], in_=pv6[:, 0:256].rearrange("p (c t) -> p c t", c=2), func=AF.Copy), r=['ps6'], w=['OT'])
            for hg in range(2):
                def mmo(e, hg=hg):
                    ins = None
                    for h4 in range(4):
                        h = hg * 4 + h4
                        e.matmul(ps[7][:, h4 * 128:(h4 + 1) * 128], lhsT=wuvs[:, h, 0, :], rhs=OT[:, h, 0, :], start=True, stop=False)
                        ins = e.matmul(ps[7][:, h4 * 128:(h4 + 1) * 128], lhsT=wuvs[:, h, 1, :], rhs=OT[:, h, 1, :], start=False, stop=True)
                    return ins
                P.op('pe', mmo, r=['wuvs', 'OT'], w=['ps7'])
                P.op('act', lambda e, hg=hg, hc=hc: e.activation(out=mixT[:, hg * 4:hg * 4 + 4, hc:hc + 128], in_=ps[7].rearrange("p (h t) -> p h t", h=4), func=AF.Copy),
                     r=['ps7'], w=['mixT'])

        for j_ in range(8 if DBG != 21 else 1):
            do_block(j_)

        dma('sp', 'pts', pts[:], ptT, w=['pts'])
        P.op('dve', lambda e: e.tensor_single_scalar(out=pts[:], in_=pts[:], scalar=4, op=ALU.logical_shift_left), r=['pts'], w=['pts'])
        if DBG == 21:
            P.op('pool', lambda e: e.memset(OTS[:], 0.0), w=['OTS0', 'OTS1'])
        qproj(NO, 64, 2048)
        P.op('pool', lambda e: e.tensor_copy(out=qlS[:], in_=qlT[:, :, :, 0:64]), r=['qlT'], w=['qlS'])
        P.op('pool', lambda e: e.tensor_copy(out=qrSs[:], in_=qrT[:, :, 0:64]), r=['qrT'], w=['qrSs'])
        P.emit()
        st2a.close()
        st2b = st2
        NCH = 2
        Lg = sb('Lg', [128, NCH, 2, 8, 256], BF16, st2b); Kg = sb('Kg', [128, NCH, 2, 8, 64], BF16, st2b)
        KT = sb('KT', [128, NCH, 3, 512], BF16, st2b)
        qS = sb('qS', [128, NCH, 2, 32], BF16, st2b); qrS = sb('qrS', [64, NCH, 32], BF16, st2b)
        Ps = sb('Ps', [32, NCH, 512], BF16, st2b); PTs = sb('PTs', [128, NCH, 4, 32], BF16, st2b)
        sst = sb('sst', [32, NCH, 12], F32, st2b)
        Oa = sb('Oa', [32, NCH, 256], F32, st2b); Ob = sb('Ob', [32, NCH, 256], BF16, st2b)
        Ln = sb('Ln', [4, NCH, 256], BF16, st2b); PnT = sb('PnT', [4, NCH, 32], BF16, st2b)
        clv = cl.rearrange("n (a r) c -> (n a) (r c)", r=8)
        ckv = ck.rearrange("n (a r) c -> (n a) (r c)", r=8)

        def do_sample(s_, c):
            bk = 4 * c
            A, Bk, C_, D_ = bk, bk + 1, bk + 2, bk + 3
            rA, rB, rC, rD = 'ps%d' % A, 'ps%d' % Bk, 'ps%d' % C_, 'ps%d' % D_
            T_ = lambda n_: '%s_%d' % (n_, c)
            pvA = ps[A][:].bitcast(BF16); pvB = ps[Bk][:].bitcast(BF16)
            st_ = sst[:, c, :]
            def softmax_update(nkeys, srcS, first):
                P.op('dve', lambda e: e.reduce_max(out=st_[:, 1:2], in_=srcS, axis=AX.X), r=[rC], w=[T_('mx')])
                if first:
                    P.op('dve', lambda e: e.tensor_copy(out=st_[:, 2:3], in_=st_[:, 1:2]), r=[T_('mx')], w=[T_('mn')])
                else:
                    P.op('dve', lambda e: e.tensor_tensor(out=st_[:, 2:3], in0=st_[:, 0:1], in1=st_[:, 1:2], op=ALU.max), r=[T_('m'), T_('mx')], w=[T_('mn')])
                    P.op('dve', lambda e: e.tensor_tensor(out=st_[:, 3:4], in0=st_[:, 0:1], in1=st_[:, 2:3], op=ALU.subtract), r=[T_('m'), T_('mn')], w=[T_('d')])
                    P.op('act', lambda e: e.activation(out=st_[:, 4:5], in_=st_[:, 3:4], func=AF.Exp, scale=SM_SCALE), r=[T_('d')], w=[T_('corr')])
                P.op('dve', lambda e: e.tensor_scalar(out=st_[:, 5:6], in0=st_[:, 2:3], scalar1=-SM_SCALE, scalar2=None, op0=ALU.mult), r=[T_('mn')], w=[T_('nb')])
                P.op('dve', lambda e: e.tensor_copy(out=st_[:, 0:1], in_=st_[:, 2:3]), r=[T_('mn')], w=[T_('m')])
                P.op('act', lambda e: e.activation(out=Ps[:, c, 0:nkeys], in_=srcS, func=AF.Exp, bias=st_[:, 5:6], scale=SM_SCALE, accum_out=st_[:, 7:8]),
                     r=[rC, T_('nb')], w=[T_('Ps'), T_('lu')])
                if first:
                    P.op('dve', lambda e: e.tensor_copy(out=st_[:, 6:7], in_=st_[:, 7:8]), r=[T_('lu')], w=[T_('l')])
                else:
                    P.op('dve', lambda e: e.scalar_tensor_tensor(out=st_[:, 6:7], in0=st_[:, 6:7], scalar=st_[:, 4:5], in1=st_[:, 7:8], op0=ALU.mult, op1=ALU.add),
                         r=[T_('l'), T_('corr'), T_('lu')], w=[T_('l')])
            def acc_update(first):
                if first:
                    P.op('dve', lambda e: e.tensor_copy(out=Oa[:, c, :], in_=ps[D_][0:32, 0:256]), r=[rD], w=[T_('Oa')])
                else:
                    P.op('dve', lambda e: e.scalar_tensor_tensor(out=Oa[:, c, :], in0=Oa[:, c, :], scalar=st_[:, 4:5], in1=ps[D_][0:32, 0:256], op0=ALU.mult, op1=ALU.add),
                         r=[T_('Oa'), T_('corr'), rD], w=[T_('Oa')])
            for cc in range(2):
                P.op('dve', lambda e, cc=cc: e.tensor_copy(out=qS[:, c, cc, :].rearrange("p (h t) -> p h t", h=8), in_=qlS[:, :, cc, s_:64:16]), r=['qlS'], w=[T_('qS')])
            P.op('dve', lambda e: e.tensor_copy(out=qrS[:, c, :].rearrange("p (h t) -> p h t", h=8), in_=qrSs[:, :, s_:64:16]), r=['qrSs'], w=[T_('qrS')])
            yield
            for u in range(32):
                g, q4 = u // 2, u % 2
                gb = g % 2
                if q4 == 0:
                    P.op('pool', lambda e, g=g, gb=gb: e.indirect_dma_start(out=Lg[:, c, gb].rearrange("p r c -> p (r c)"), out_offset=None, in_=clv,
                         in_offset=bass.IndirectOffsetOnAxis(ap=pts[:, s_:s_ + 1], axis=0), element_offset=g * 2048), r=['pts'], w=[T_('Lg%d' % gb)], lane='Lg%d%d' % (c, gb))
                    P.op('pool', lambda e, g=g, gb=gb: e.indirect_dma_start(out=Kg[:, c, gb].rearrange("p r c -> p (r c)"), out_offset=None, in_=ckv,
                         in_offset=bass.IndirectOffsetOnAxis(ap=pts[:, s_:s_ + 1], axis=0), element_offset=g * 512), r=['pts'], w=[T_('Kg%d' % gb)], lane='Kg%d%d' % (c, gb))
                def trk(e, gb=gb, q4=q4):
                    ins = None
                    for r4 in range(4):
                        r_ = q4 * 4 + r4
                        e.transpose(pvA[:, r4 * 128:(r4 + 1) * 128], Lg[:, c, gb, r_, 0:128], identb[:])
                        e.transpose(pvA[:, 512 + r4 * 128:512 + (r4 + 1) * 128], Lg[:, c, gb, r_, 128:256], identb[:])
                        ins = e.transpose(pvB[0:64, r4 * 128:(r4 + 1) * 128], Kg[:, c, gb, r_, :], identb[:])
                    return ins
                P.op('pe', trk, r=[T_('Lg%d' % gb), T_('Kg%d' % gb), 'identb'], w=[rA, rB])
                yield
                P.op('dve', lambda e: e.tensor_copy(out=KT[:, c, 0:2, :], in_=pvA[:, 0:1024].rearrange("p (k t) -> p k t", k=2)), r=[rA], w=[T_('KT01')])
                P.op('act', lambda e: e.activation(out=KT[0:64, c, 2, :], in_=pvB[0:64, 0:512], func=AF.Copy), r=[rB], w=[T_('KT2')])
                def scs(e):
                    o_ = ps[C_][0:32, :]
                    e.matmul(o_, lhsT=qS[:, c, 0, :], rhs=KT[:, c, 0, :], start=True, stop=False)
                    e.matmul(o_, lhsT=qS[:, c, 1, :], rhs=KT[:, c, 1, :], start=False, stop=False)
                    return e.matmul(o_, lhsT=qrS[:, c, :], rhs=KT[0:64, c, 2, :], start=False, stop=True)
                P.op('pe', scs, r=[T_('qS'), T_('qrS'), T_('KT01'), T_('KT2')], w=[rC])
                yield
                softmax_update(512, ps[C_][0:32, :], first=(u == 0))
                def trps(e):
                    ins = None
                    for kc in range(4):
                        ins = e.transpose(pvB[:, 512 + kc * 32:512 + (kc + 1) * 32], Ps[:, c, kc * 128:(kc + 1) * 128], identb[0:32, 0:32])
                    return ins
                P.op('pe', trps, r=[T_('Ps'), 'identb'], w=[rB])
                yield
                P.op('act', lambda e: e.activation(out=PTs[:, c, :, :], in_=pvB[:, 512:640].rearrange("p (k q) -> p k q", q=32), func=AF.Copy), r=[rB], w=[T_('PTs')])
                def pvs(e, gb=gb, q4=q4):
                    ins = None
                    for kc in range(4):
                        ins = e.matmul(ps[D_][0:32, 0:256], lhsT=PTs[:, c, kc, :], rhs=Lg[:, c, gb, q4 * 4 + kc, :], start=(kc == 0), stop=(kc == 3))
                    return ins
                P.op('pe', pvs, r=[T_('PTs'), T_('Lg%d' % gb)], w=[rD])
                acc_update(first=(u == 0))
                yield
            def scn(e):
                o_ = ps[C_][0:32, 0:4]
                e.matmul(o_, lhsT=qS[:, c, 0, :], rhs=latT[:, 0, 2048 + s_:2048 + 64:16], start=True, stop=False)
                e.matmul(o_, lhsT=qS[:, c, 1, :], rhs=latT[:, 1, 2048 + s_:2048 + 64:16], start=False, stop=False)
                return e.matmul(o_, lhsT=qrS[:, c, :], rhs=krT[:, 2048 + s_:2048 + 64:16], start=False, stop=True)
            P.op('pe', scn, r=[T_('qS'), T_('qrS'), 'latT', 'krT'], w=[rC])
            P.op('dve', lambda e: e.tensor_tensor(out=ps[C_][0:32, 0:4], in0=ps[C_][0:32, 0:4], in1=smk[:], op=ALU.add), r=[rC, 'smk'], w=[rC])
            softmax_update(4, ps[C_][0:32, 0:4], first=False)
            yield
            P.op('pe', lambda e: e.matmul(ps[C_][0:4, 0:256], lhsT=identb[0:64, s_:64:16], rhs=lat_tok[0:64, 16, 0:256], start=True, stop=True),
                 r=['identb', 'lat_tok'], w=[rC])
            P.op('act', lambda e: e.activation(out=Ln[:, c, :], in_=ps[C_][0:4, 0:256], func=AF.Copy), r=[rC], w=[T_('Ln')])
            P.op('pe', lambda e: e.transpose(pvB[0:4, 704:736], Ps[:, c, 0:4], identb[0:32, 0:32]), r=[T_('Ps'), 'identb'], w=[rB])
            P.op('act', lambda e: e.activation(out=PnT[:, c, :], in_=pvB[0:4, 704:736], func=AF.Copy), r=[rB], w=[T_('PnT')])
            P.op('pe', lambda e: e.matmul(ps[D_][0:32, 0:256], lhsT=PnT[:, c, :], rhs=Ln[:, c, :], start=True, stop=True), r=[T_('PnT'), T_('Ln')], w=[rD])
            acc_update(first=False)
            yield
            P.op('dve', lambda e: e.reciprocal(out=st_[:, 8:9], in_=st_[:, 6:7]), r=[T_('l')], w=[T_('ri')])
            P.op('act', lambda e: e.activation(out=Ob[:, c, :], in_=Oa[:, c, :], func=AF.Copy, scale=st_[:, 8:9]), r=[T_('Oa'), T_('ri')], w=[T_('Ob')])
            def trob(e):
                e.transpose(pvB[:, 640:672], Ob[:, c, 0:128], identb[0:32, 0:32])
                return e.transpose(pvB[:, 672:704], Ob[:, c, 128:256], identb[0:32, 0:32])
            P.op('pe', trob, r=[T_('Ob'), 'identb'], w=[rB])
            P.op('act', lambda e: e.activation(out=OTS[:, :, s_, :], in_=pvB[:, 640:704].rearrange("p (c q) -> p c q", c=2), func=AF.Copy), r=[rB], w=['OTS%d' % c])
            yield

        NSAMP = 16 if DBG != 21 else 2
        for s0 in range(0, NSAMP, NCH):
            gens = [do_sample(s0 + c_, c_) for c_ in range(NCH)]
            alive = list(gens)
            while alive:
                for g_ in list(alive):
                    try:
                        next(g_)
                    except StopIteration:
                        alive.remove(g_)
        for h in range(8):
            def mmos(e, h=h):
                e.matmul(ps[7][:, 0:64], lhsT=wuvs[:, h, 0, :], rhs=OTS[:, 0, :, h * 4:(h + 1) * 4], start=True, stop=False)
                return e.matmul(ps[7][:, 0:64], lhsT=wuvs[:, h, 1, :], rhs=OTS[:, 1, :, h * 4:(h + 1) * 4], start=False, stop=True)
            P.op('pe', mmos, r=['wuvs', 'OTS0', 'OTS1'], w=['ps7'])
            P.op('act', lambda e, h=h: e.activation(out=mixT[:, h, NO:NO + 64].rearrange("p (t s) -> p s t", t=4),
                                                     in_=ps[7][:, 0:64].rearrange("p (s t) -> p s t", t=4), func=AF.Copy), r=['ps7'], w=['mixT'])
        P.emit(final=(STAGE == 2))
    es_mid.close()
    if STAGE == 2:
        es.close(); return nc

    TILES = [(0, 512), (512, 512), (1024, 64)]
    with ExitStack() as st3:
        sq2 = sb('sq2', [128, 512], BF16, st3)
        rsb = sb('rsb', [128, 512], F32, st3)
        tmpf = sb('tmpf', [128, 512], F32, st3); tmpg = sb('tmpg', [128, 512], F32, st3)
        wbuf = sb('wbuf', [128, 3, 16, 128], BF16, st3)
        wbc = [0]

        def norm_stats(srcs, nfeat, n, tag):
            for j, s_ in enumerate(srcs):
                P.op('act', lambda e, s_=s_: e.activation(out=sq2[:, 0:n], in_=s_, func=AF.Square), r=[tag], w=['sq2'])
                P.op('pe', lambda e, j=j: e.matmul(ps[7][:, 0:n], lhsT=onesb[:], rhs=sq2[:, 0:n], start=(j == 0), stop=(j == len(srcs) - 1)),
                     r=['sq2', 'onesb'], w=['ps7'])
            P.op('act', lambda e: e.activation(out=rsb[:, 0:n], in_=ps[7][:, 0:n], func=AF.Sqrt, bias=epsT[:], scale=1.0 / nfeat), r=['ps7', 'epsT'], w=['rsb'])
            P.op('dve', lambda e: e.reciprocal(out=rsb[:, 0:n], in_=rsb[:, 0:n]), r=['rsb'], w=['rsb'])

        def load_w(src_ap, nk, ncol=128):
            b = wbc[0] % 3; wbc[0] += 1
            wt = wbuf[:, b, 0:nk, 0:ncol]
            dma('pool', 'wbuf%d' % b, wt, src_ap.rearrange("(k p) c -> p k c", p=128), w=['wbuf%d' % b])
            return wt, 'wbuf%d' % b

        for (c0, n) in TILES:
            for (k0, gcol) in ((0, GV_OM), (8, GV_OL)):
                norm_stats([mixT[:, k0 + k, c0:c0 + n] for k in range(8)], 1024, n, 'mixT')
                for k in range(8):
                    P.op('dve', lambda e, k=k, k0=k0, gcol=gcol, c0=c0, n=n: e.scalar_tensor_tensor(
                        out=mixT[:, k0 + k, c0:c0 + n], in0=mixT[:, k0 + k, c0:c0 + n], scalar=gv[:, gcol + k:gcol + k + 1], in1=rsb[:, 0:n],
                        op0=ALU.mult, op1=ALU.mult), r=['mixT', 'gv', 'rsb'], w=['mixT'])
        for m in range(16):
            wt, wres = load_w(w_o[:, m * 128:(m + 1) * 128], 16)
            for ti, (c0, n) in enumerate(TILES):
                bank = 4 + ti % 2
                def mmw(e, wt=wt, c0=c0, n=n, bank=bank):
                    ins = None
                    for k in range(16):
                        ins = e.matmul(ps[bank][:, 0:n], lhsT=wt[:, k, :], rhs=mixT[:, k, c0:c0 + n], start=(k == 0), stop=(k == 15))
                    return ins
                P.op('pe', mmw, r=[wres, 'mixT'], w=['ps%d' % bank])
                P.op('dve', lambda e, m=m, c0=c0, n=n, bank=bank: e.tensor_tensor(out=hT[:, m, c0:c0 + n], in0=hT[:, m, c0:c0 + n], in1=ps[bank][:, 0:n], op=ALU.add),
                     r=['hT', 'ps%d' % bank], w=['hT'])

        def fm_rmsnorm_to_mix(gcol, extra=None):
            for (c0, n) in TILES:
                norm_stats([hT[:, k, c0:c0 + n] for k in range(16)], D, n, 'hT')
                for k in range(16):
                    P.op('dve', lambda e, k=k, c0=c0, n=n: e.scalar_tensor_tensor(out=tmpf[:, 0:n], in0=hT[:, k, c0:c0 + n], scalar=gv[:, gcol + k:gcol + k + 1],
                                                                                 in1=rsb[:, 0:n], op0=ALU.mult, op1=ALU.mult), r=['hT', 'gv', 'rsb'], w=['tmpf'])
                    P.op('pool', lambda e, k=k, c0=c0, n=n: e.tensor_copy(out=mixT[:, k, c0:c0 + n], in_=tmpf[:, 0:n]), r=['tmpf'], w=['mixT'])
                    if extra is not None: extra(k, c0, n)

        if DBG != 31:
          with ExitStack() as st4:
            wr32 = sb('wr32', [128, 16, 36], F32, st4)
            lgT = sb('lgT', [36, NT], F32, st4)
            brow = sb('brow', [128, 36], F32, st4)
            tl = sb('tl', [128, 36], F32, st4); ohg = sb('ohg', [128, 4], F32, st4); em = sb('em', [128, 32], F32, st4)
            m8 = sb('m8', [128, 8], F32, st4); rs_ = sb('rs_', [128, 8], F32, st4); cmb = sb('cmb', [128, 32], F32, st4)
            combT = sb('combT', [32, NT], F32, st4)
            esel = sb('esel', [32, 128], F32, st4); ones32 = sb('ones32', [32, 128], F32, st4)
            cbe = sb('cbe', [128, NT], F32, st4)
            gu = sb('gu', [128, 3, 2, 16, 128], BF16, st4)
            wd = sb('wd', [128, 2, 4, 2048], BF16, st4)
            actT = sb('actT', [128, 4, NT], BF16, st4)
            dma('sp', 'wr32', wr32[:, :, 0:4], w_group.rearrange("(k p) c -> p k c", p=128), w=['wr32'])
            dma('sp', 'wr32', wr32[:, :, 4:36], w_router.rearrange("(k p) c -> p k c", p=128), w=['wr32'])
            dma('sp', 'brow', brow[:, 0:4], b_group.partition_broadcast(128), w=['brow'])
            dma('sp', 'brow', brow[:, 4:36], b_router.partition_broadcast(128), w=['brow'])
            P.op('pool', lambda e: e.memset(ones32[:], 1.0), w=['ones32'])
            cur = {}
            def router_hook(k, c0, n):
                bank = 6
                P.op('pe', lambda e, k=k, n=n: e.matmul(ps[6][0:36, 0:n], lhsT=wr32[:, k, :], rhs=tmpf[:, 0:n], start=(k == 0), stop=(k == 15)),
                     r=['wr32', 'tmpf'], w=['ps6'])
                if k == 15:
                    P.op('act', lambda e, c0=c0, n=n: e.activation(out=lgT[:, c0:c0 + n], in_=ps[6][0:36, 0:n], func=AF.Copy), r=['ps6'], w=['lgT'])
            fm_rmsnorm_to_mix(GV_FFN, router_hook)
            for bi in range(9):
                t0_ = bi * 128; rows = min(128, NT - t0_)
                P.op('pe', lambda e, t0_=t0_, rows=rows: e.transpose(ps[6][0:rows, 0:36], lgT[:, t0_:t0_ + rows], identf[0:36, 0:36]), r=['lgT', 'identf'], w=['ps6'])
                R_ = slice(0, rows)
                P.op('dve', lambda e, R_=R_: e.tensor_tensor(out=tl[R_, :], in0=ps[6][R_, 0:36], in1=brow[R_, :], op=ALU.add), r=['ps6', 'brow'], w=['tl'])
                P.op('dve', lambda e, R_=R_: e.reduce_max(out=rs_[R_, 0:1], in_=tl[R_, 0:4], axis=AX.X), r=['tl'], w=['rs0'])
                P.op('dve', lambda e, R_=R_: e.tensor_scalar(out=ohg[R_, :], in0=tl[R_, 0:4], scalar1=rs_[R_, 0:1], scalar2=None, op0=ALU.is_ge), r=['tl', 'rs0'], w=['ohg'])
                P.op('dve', lambda e, R_=R_: e.tensor_scalar(out=rs_[R_, 1:2], in0=rs_[R_, 0:1], scalar1=-1.0, scalar2=None, op0=ALU.mult), r=['rs0'], w=['rs1'])
                P.op('act', lambda e, R_=R_: e.activation(out=tl[R_, 0:4], in_=tl[R_, 0:4], func=AF.Exp, bias=rs_[R_, 1:2], scale=1.0, accum_out=rs_[R_, 2:3]),
                     r=['tl', 'rs1'], w=['tl', 'rs2'])
                P.op('dve', lambda e, R_=R_: e.tensor_scalar(out=ohg[R_, :], in0=ohg[R_, :], scalar1=-1.0, scalar2=1e30, op0=ALU.add, op1=ALU.mult), r=['ohg'], w=['ohg'])
                P.op('dve', lambda e, R_=R_, rows=rows: e.tensor_tensor(out=em[R_, :].rearrange("p (g x) -> p g x", g=4), in0=tl[R_, 4:36].rearrange("p (g x) -> p g x", g=4),
                                                             in1=ohg[R_, :].unsqueeze(2).to_broadcast([rows, 4, 8]), op=ALU.add), r=['tl', 'ohg'], w=['em'])
                P.op('dve', lambda e, R_=R_: e.max(out=m8[R_, :], in_=em[R_, :]), r=['em'], w=['m8'])
                P.op('dve', lambda e, R_=R_: e.tensor_scalar(out=cmb[R_, :], in0=em[R_, :], scalar1=m8[R_, 1:2], scalar2=None, op0=ALU.is_ge), r=['em', 'm8'], w=['cmb'])
                P.op('dve', lambda e, R_=R_: e.tensor_scalar(out=rs_[R_, 3:4], in0=m8[R_, 0:1], scalar1=-1.0, scalar2=None, op0=ALU.mult), r=['m8'], w=['rs3'])
                P.op('act', lambda e, R_=R_: e.activation(out=em[R_, :], in_=em[R_, :], func=AF.Exp, bias=rs_[R_, 3:4], scale=1.0), r=['em', 'rs3'], w=['em'])
                P.op('act', lambda e, R_=R_: e.activation(out=rs_[R_, 4:5], in_=m8[R_, 1:2], func=AF.Exp, bias=rs_[R_, 3:4], scale=1.0), r=['m8', 'rs3'], w=['rs4'])
                P.op('dve', lambda e, R_=R_: e.tensor_scalar(out=rs_[R_, 4:5], in0=rs_[R_, 4:5], scalar1=1.0, scalar2=rs_[R_, 2:3], op0=ALU.add, op1=ALU.mult), r=['rs4', 'rs2'], w=['rs4'])
                P.op('dve', lambda e, R_=R_: e.reciprocal(out=rs_[R_, 5:6], in_=rs_[R_, 4:5]), r=['rs4'], w=['rs5'])
                P.op('dve', lambda e, R_=R_: e.scalar_tensor_tensor(out=cmb[R_, :], in0=em[R_, :], scalar=rs_[R_, 5:6], in1=cmb[R_, :], op0=ALU.mult, op1=ALU.mult),
                     r=['em', 'rs5', 'cmb'], w=['cmb'])
                P.op('pe', lambda e, rows=rows: e.transpose(ps[7][0:32, 0:rows], cmb[0:rows, :], identf[0:rows, 0:rows]), r=['cmb', 'identf'], w=['ps7'])
                P.op('act', lambda e, t0_=t0_, rows=rows: e.activation(out=combT[:, t0_:t0_ + rows], in_=ps[7][0:32, 0:rows], func=AF.Copy), r=['ps7'], w=['combT'])
            NEXP = 32 if DBG != 32 else 2
            guc = [0]; wdc = [0]
            for ex in range(NEXP):
                P.op('dve', lambda e, ex=ex: e.tensor_scalar(out=esel[:], in0=ones32[:], scalar1=identf[0:32, ex:ex + 1], scalar2=None, op0=ALU.mult), r=['ones32', 'identf'], w=['esel'])
                for ti, (c0, n) in enumerate(TILES):
                    P.op('pe', lambda e, c0=c0, n=n: e.matmul(ps[6][:, 0:n], lhsT=esel[:], rhs=combT[:, c0:c0 + n], start=True, stop=True), r=['esel', 'combT'], w=['ps6'])
                    P.op('act', lambda e, c0=c0, n=n: e.activation(out=cbe[:, c0:c0 + n], in_=ps[6][:, 0:n], func=AF.Copy), r=['ps6'], w=['cbe'])
                wb_ = wdc[0] % 2; wdc[0] += 1
                for fc in range(4):
                    dma('pool', 'wd%d' % wb_, wd[:, wb_, fc, :], w_down[ex, fc * 128:(fc + 1) * 128, :], w=['wd%d' % wb_])
                for fc in range(4):
                    gb = guc[0] % 3; guc[0] += 1
                    dma('pool', 'gu%d' % gb, gu[:, gb, 0], w_gate[ex, :, fc * 128:(fc + 1) * 128].rearrange("(k p) c -> p k c", p=128), w=['gu%d' % gb])
                    dma('pool', 'gu%d' % gb, gu[:, gb, 1], w_up[ex, :, fc * 128:(fc + 1) * 128].rearrange("(k p) c -> p k c", p=128), w=['gu%d' % gb])
                    for ti, (c0, n) in enumerate(TILES):
                        for which in range(2):
                            bank = 2 * (ti % 2) + which
                            def mmg(e, gb=gb, which=which, c0=c0, n=n, bank=bank):
                                ins = None
                                for k in range(16):
                                    ins = e.matmul(ps[bank][:, 0:n], lhsT=gu[:, gb, which, k, :], rhs=mixT[:, k, c0:c0 + n], start=(k == 0), stop=(k == 15))
                                return ins
                            P.op('pe', mmg, r=['gu%d' % gb, 'mixT'], w=['ps%d' % bank])
                        b0 = 2 * (ti % 2)
                        P.op('act', lambda e, n=n, b0=b0: e.activation(out=tmpf[:, 0:n], in_=ps[b0][:, 0:n], func=AF.Silu), r=['ps%d' % b0], w=['tmpf'])
                        P.op('dve', lambda e, n=n, b0=b0: e.tensor_tensor(out=tmpf[:, 0:n], in0=tmpf[:, 0:n], in1=ps[b0 + 1][:, 0:n], op=ALU.mult), r=['tmpf', 'ps%d' % (b0 + 1)], w=['tmpf'])
                        P.op('dve', lambda e, fc=fc, c0=c0, n=n: e.tensor_tensor(out=actT[:, fc, c0:c0 + n], in0=tmpf[:, 0:n], in1=cbe[:, c0:c0 + n], op=ALU.mult),
                             r=['tmpf', 'cbe'], w=['actT'])
                for m in range(16):
                    for ti, (c0, n) in enumerate(TILES):
                        bank = 4 + (m * 3 + ti) % 4
                        def mmd(e, wb_=wb_, m=m, c0=c0, n=n, bank=bank):
                            ins = None
                            for fc in range(4):
                                ins = e.matmul(ps[bank][:, 0:n], lhsT=wd[:, wb_, fc, m * 128:(m + 1) * 128], rhs=actT[:, fc, c0:c0 + n], start=(fc == 0), stop=(fc == 3))
                            return ins
                        P.op('pe', mmd, r=['wd%d' % wb_, 'actT'], w=['ps%d' % bank])
                        P.op('dve', lambda e, m=m, c0=c0, n=n, bank=bank: e.tensor_tensor(out=hT[:, m, c0:c0 + n], in0=hT[:, m, c0:c0 + n], in1=ps[bank][:, 0:n], op=ALU.add),
                             r=['hT', 'ps%d' % bank], w=['hT'])
            P.emit()

        with ExitStack() as st5:
            pT = sb('pT', [128, 2, NT], BF16, st5)
            pst = sb('pst', [128, 256], F32, st5)
            wpp = sb('wpp', [128, 2, 2, 128], BF16, st5)
            fm_rmsnorm_to_mix(GV_PLE)
            for bi in range(9):
                t0_ = bi * 128; rows = min(128, NT - t0_)
                srcp = po[t0_:t0_ + rows, :] if bi < 8 else psm[0:rows, :]
                dma('sp', 'pst', pst[0:rows, :], srcp, w=['pst'])
                def trp_(e, rows=rows):
                    e.transpose(ps[6][:, 0:rows], pst[0:rows, 0:128], identf[0:rows, 0:rows])
                    return e.transpose(ps[6][:, 128:128 + rows], pst[0:rows, 128:256], identf[0:rows, 0:rows])
                P.op('pe', trp_, r=['pst', 'identf'], w=['ps6'])
                P.op('act', lambda e, t0_=t0_, rows=rows: e.activation(out=pT[:, :, t0_:t0_ + rows], in_=ps[6][:, 0:256].rearrange("p (c t) -> p c t", c=2)[:, :, 0:rows], func=AF.Copy),
                     r=['ps6'], w=['pT'])
            for m in range(16):
                wt, wres = load_w(w_pg[:, m * 128:(m + 1) * 128], 16)
                pb_ = m % 2
                dma('pool', 'wpp%d' % pb_, wpp[:, pb_], w_pp[:, m * 128:(m + 1) * 128].rearrange("(k p) c -> p k c", p=128), w=['wpp%d' % pb_])
                for ti, (c0, n) in enumerate(TILES):
                    b0 = 2 * (ti % 2)
                    def mmg2(e, wt=wt, c0=c0, n=n, b0=b0):
                        ins = None
                        for k in range(16):
                            ins = e.matmul(ps[b0][:, 0:n], lhsT=wt[:, k, :], rhs=mixT[:, k, c0:c0 + n], start=(k == 0), stop=(k == 15))
                        return ins
                    P.op('pe', mmg2, r=[wres, 'mixT'], w=['ps%d' % b0])
                    def mmp2(e, pb_=pb_, c0=c0, n=n, b0=b0):
                        e.matmul(ps[b0 + 1][:, 0:n], lhsT=wpp[:, pb_, 0, :], rhs=pT[:, 0, c0:c0 + n], start=True, stop=False)
                        return e.matmul(ps[b0 + 1][:, 0:n], lhsT=wpp[:, pb_, 1, :], rhs=pT[:, 1, c0:c0 + n], start=False, stop=True)
                    P.op('pe', mmp2, r=['wpp%d' % pb_, 'pT'], w=['ps%d' % (b0 + 1)])
                    P.op('act', lambda e, m=m, n=n, b0=b0: e.activation(out=tmpg[:, 0:n], in_=ps[b0][:, 0:n], func=AF.Sigmoid, bias=gv[:, GV_BPG + m:GV_BPG + m + 1], scale=1.0),
                         r=['ps%d' % b0, 'gv'], w=['tmpg'])
                    P.op('dve', lambda e, n=n, b0=b0: e.tensor_tensor(out=tmpg[:, 0:n], in0=tmpg[:, 0:n], in1=ps[b0 + 1][:, 0:n], op=ALU.mult), r=['tmpg', 'ps%d' % (b0 + 1)], w=['tmpg'])
                    P.op('dve', lambda e, m=m, c0=c0, n=n: e.tensor_tensor(out=hT[:, m, c0:c0 + n], in0=hT[:, m, c0:c0 + n], in1=tmpg[:, 0:n], op=ALU.add), r=['hT', 'tmpg'], w=['hT'])
            P.emit()

        with ExitStack() as st6:
            yst = sb('yst', [128, 2, D], F32, st6)
            yc = [0]
            for (c0, n) in TILES:
                norm_stats([hT[:, k, c0:c0 + n] for k in range(16)], D, n, 'hT')
                nb_ = (n + 127) // 128
                for i in range(nb_):
                    rows = min(128, n - i * 128)
                    yb = yc[0] % 2; yc[0] += 1
                    for q4 in range(4):
                        for k4 in range(4):
                            k = q4 * 4 + k4
                            P.op('dve', lambda e, k=k, k4=k4, c0=c0, i=i, rows=rows: e.scalar_tensor_tensor(
                                out=tmpf[:, k4 * 128:k4 * 128 + rows], in0=hT[:, k, c0 + i * 128:c0 + i * 128 + rows], scalar=gv2[:, k:k + 1],
                                in1=rsb[:, i * 128:i * 128 + rows], op0=ALU.mult, op1=ALU.mult), r=['hT', 'gv', 'rsb'], w=['tmpf'])
                        bank = 4 + q4 % 2
                        def try_(e, rows=rows, bank=bank):
                            ins = None
                            for k4 in range(4):
                                ins = e.transpose(ps[bank][0:rows, k4 * 128:(k4 + 1) * 128], tmpf[:, k4 * 128:k4 * 128 + rows], identf[:])
                            return ins
                        P.op('pe', try_, r=['tmpf', 'identf'], w=['ps%d' % bank])
                        P.op('act', lambda e, q4=q4, rows=rows, bank=bank, yb=yb: e.activation(out=yst[0:rows, yb, q4 * 512:(q4 + 1) * 512], in_=ps[bank][0:rows, :], func=AF.Copy),
                             r=['ps%d' % bank], w=['yst%d' % yb])
                    r0 = c0 + i * 128
                    dst = y_o[r0:r0 + rows, :] if c0 < NO else y_s[0:rows, :]
                    dma('sp', 'yst%d' % yb, dst, yst[0:rows, yb, :], r=['yst%d' % yb])
            P.emit(final=True)
    es.close()
    return nc


def _rope_tables(pos):
    inv = 10000.0 ** (-np.arange(32, dtype=np.float32) / 32)
    ang = pos.astype(np.float32)[None, :] * inv[:, None]
    c = np.cos(ang).astype(np.float32); s = np.sin(ang).astype(np.float32)
    return np.concatenate([c, c], 0), np.concatenate([-s, s], 0)


def make_in_maps(inp):
    f = lambda a: np.ascontiguousarray(np.asarray(a, dtype=np.float32))
    x_prompt = np.asarray(inp['x_prompt']); x_sample = np.asarray(inp['x_sample'])
    perm = (np.arange(64) + 32) % 64
    w_in = np.asarray(inp['w_in'])[0]; w_uq = np.asarray(inp['w_uq'])[0]; w_ukv = np.asarray(inp['w_ukv'])[0]
    shared = dict(
        cl=f(inp['cache_latent'][0]), ck=f(inp['cache_krope'][0]),
        g_mix=f(inp['g_mix'][0]), w_in=f(w_in), wkrp=f(w_in[:, 768:832][:, perm]),
        g_q=f(inp['g_q'][0]), w_uq=f(w_uq), wqrp=f(w_uq.reshape(512, 8, 192)[:, :, 128:][:, :, perm]),
        g_kv=f(inp['g_kv'][0]), wukT=f(w_ukv[:, :, :128].transpose(1, 2, 0)), wuv=f(w_ukv[:, :, 128:].transpose(1, 0, 2)),
        w_conv=f(inp['w_conv'][0]), b_conv=f(inp['b_conv'][0]), w_rg=f(inp['w_rg'][0]), b_rg=f(inp['b_rg'][0]),
        w_ig=f(inp['w_ig'][0]), b_ig=f(inp['b_ig'][0]), lam=f(inp['lru_lambda'][0]),
        g_om=f(inp['g_out_mla'][0]), g_ol=f(inp['g_out_lru'][0]), w_o=f(inp['w_o'][0]), g_ffn=f(inp['g_ffn'][0]),
        w_group=f(inp['w_group'][0]), b_group=f(inp['b_group'][0]), w_router=f(inp['w_router'][0]), b_router=f(inp['b_router'][0]),
        w_gate=f(inp['w_gate'][0]), w_up=f(inp['w_up'][0]), w_down=f(inp['w_down'][0]),
        g_ple=f(inp['g_ple'][0]), w_pg=f(inp['w_ple_gate'][0]), b_pg=f(inp['b_ple_gate'][0]), w_pp=f(inp['w_ple_proj'][0]),
        g_fin=f(inp['g_final']),
        tri=np.where(np.arange(128)[None, :] <= np.arange(128)[:, None], 0.0, NEG).astype(np.float32),
        smask=np.where(np.arange(4)[None, :] <= (np.arange(32) % 4)[:, None], 0.0, NEG).astype(np.float32),
    )
    pm = lambda a: np.asarray(a, np.float32).reshape(-1, 128).T
    gvh = np.zeros((128, 104), np.float32)
    for off, key in [(0, 'g_mix'), (16, 'g_q'), (20, 'g_kv'), (22, 'g_out_mla'), (30, 'g_out_lru'), (38, 'g_ffn'), (54, 'g_ple')]:
        v = pm(np.asarray(inp[key])[0]); gvh[:, off:off + v.shape[1]] = v
    gvh[:, 70:86] = pm(inp['g_final'])
    gvh[:, 86:102] = pm(np.asarray(inp['b_ple_gate'])[0])
    lruch = np.zeros((128, 8, 8), np.float32)
    for k_ in range(4): lruch[:, :, k_] = pm(np.asarray(inp['w_conv'])[0, k_])
    for j_, key in enumerate(['b_conv', 'b_rg', 'b_ig', 'lru_lambda']): lruch[:, :, 4 + j_] = pm(np.asarray(inp[key])[0])
    past_len = inp['page_table'].shape[1] * inp['cache_latent'].shape[2]
    pos_s = past_len + np.repeat(np.arange(4), 16)
    in_maps = []
    for c in range(8):
        b, half = c // 2, c % 2
        own = slice(half * 1024, half * 1024 + 1024)
        ss = slice(16 * c, 16 * c + 16)
        tm = lambda a: np.ascontiguousarray(np.swapaxes(a, 0, 1).reshape((64,) + a.shape[2:]))
        pos = np.concatenate([np.arange(0, 1024), np.arange(half * 1024, half * 1024 + 1024), pos_s])
        rc, rs = _rope_tables(pos)
        m = dict(shared)
        m.update(
            xo=f(x_prompt[b, own]), xp=f(x_prompt[b, 0:1024]) if half else np.zeros((1024, D), np.float32),
            xs=f(tm(x_sample[ss])), po=f(inp['p_prompt'][0, b, own]), psm=f(tm(np.asarray(inp['p_sample'])[0, ss])),
            ptT=np.ascontiguousarray(np.asarray(inp['page_table'])[ss].T.astype(np.int32)),
            slT=f(np.asarray(inp['state_lru'])[0, ss].T), scT=f(np.asarray(inp['state_conv'])[0, ss].transpose(2, 1, 0).reshape(1024, 48)),
            gvh=gvh, lruch=lruch,
            flags=np.tile(np.array([[1.0 if half else 0.0, 0.0 if half else NEG]], np.float32), (128, 1)),
            ropeC=rc, ropeS=rs,
        )
        in_maps.append(m)
    return in_maps


def assemble(res):
    B, S, DB, T = 4, 2048, 128, 4
    y_p = np.zeros((B, S, D), np.float32); y_s = np.zeros((DB, T, D), np.float32)
    nl_p = np.zeros((1, B, S, 256), np.float32); nk_p = np.zeros((1, B, S, 64), np.float32)
    nlru_p = np.zeros((1, B, 1024), np.float32); nconv_p = np.zeros((1, B, 3, 1024), np.float32)
    nl_s = np.zeros((1, DB, T, 256), np.float32); nk_s = np.zeros((1, DB, T, 64), np.float32)
    nlru_s = np.zeros((1, DB, 1024), np.float32); nconv_s = np.zeros((1, DB, 3, 1024), np.float32)
    utm = lambda a: np.swapaxes(a.reshape((4, 16) + a.shape[1:]), 0, 1)
    for c in range(8):
        b, half = c // 2, c % 2
        own = slice(half * 1024, half * 1024 + 1024); ss = slice(16 * c, 16 * c + 16)
        r = res[c]
        y_p[b, own] = r['y_o']; y_s[ss] = utm(r['y_s'])
        nl_p[0, b, own] = r['lat_o']; nk_p[0, b, own] = r['kr_o']
        if half:
            nlru_p[0, b] = r['lru_o'].T.reshape(1024); nconv_p[0, b] = r['conv_o'].reshape(128, 8, 3).transpose(2, 1, 0).reshape(3, 1024)
        nl_s[0, ss] = utm(r['lat_s']); nk_s[0, ss] = utm(r['kr_s'])
        nlru_s[0, ss] = r['lru_s'].T
        nconv_s[0, ss] = r['conv_s'].reshape(1024, 3, 16).transpose(2, 1, 0)
    return (y_p, y_s, nl_p, nk_p, nlru_p, nconv_p, nl_s, nk_s, nlru_s, nconv_s)


def kernel(**inp):
    nc = build_nc(int(np.asarray(inp['cache_latent']).shape[1]))
    in_maps = make_in_maps(inp)
    names = set(DECLARED)
    in_maps = [{k: v for k, v in m.items() if k in names} for m in in_maps]
    res = run_bass_kernel_spmd(nc, in_maps, core_ids=list(range(8))).results
    return assemble(res)
```
